# Optimizing a Trainium2 kernel written in Bass

```python
import math
import jax, jax.numpy as jnp
from jax import lax
import numpy as np

D_MODEL = 1024
BATCH = 8
SEQ = 2048
DEPTH = 2

GRID_W = 64
CTX_LEN = 256
N_MIXERS = 2
M_HEADS = 4
M_DV = D_MODEL // M_HEADS
M_DK = M_DV // 2
M_CHUNK = 128
M_PROJ = 2 * M_HEADS * M_DK + 2 * M_HEADS * M_DV + 4 * M_HEADS
F_GROUPS = 8
F_GROUP_DIM = D_MODEL // F_GROUPS
FFN_DIM = 2816
CONV_W = 3
EPS = 1e-6

kernel_name = "hybrid_mlstm_fourier_dit_block"


def rmsnorm(x, g):
    x32 = x.astype(jnp.float32)
    y = x32 * lax.rsqrt(jnp.mean(x32 * x32, axis=-1, keepdims=True) + EPS)
    return (y * g.astype(jnp.float32)).astype(x.dtype)


def modulate(h, shift, scale):
    return h * (1 + scale[..., None, :]) + shift[..., None, :]


def _ctx_stream_needed(layer):
    return any(j % N_MIXERS == 0 for j in range(layer + 1, DEPTH))


def mlstm_project(h, w_in, b_in):
    B, T, _ = h.shape
    hk, hv = M_HEADS * M_DK, M_HEADS * M_DV
    p = h @ w_in + b_in
    q, k, v, o, gates = jnp.split(p, [hk, 2 * hk, 2 * hk + hv, 2 * hk + 2 * hv], axis=-1)

    def heads(t, d):
        return t.reshape(B, T, M_HEADS, d).transpose(0, 2, 1, 3).astype(jnp.float32)

    q = heads(q, M_DK)
    k = heads(k, M_DK) * (M_DK ** -0.5)
    v = heads(v, M_DV)
    g = gates.astype(jnp.float32).reshape(B, T, 4, M_HEADS).transpose(2, 0, 3, 1)
    fwd = (g[0], jax.nn.log_sigmoid(g[1]))
    bwd = (g[2], jax.nn.log_sigmoid(g[3]))
    return q, k, v, o, fwd, bwd


def mlstm_zero_state(B):
    return (jnp.zeros((B, M_HEADS, M_DK, M_DV), jnp.float32),
            jnp.zeros((B, M_HEADS, M_DK), jnp.float32),
            jnp.zeros((B, M_HEADS), jnp.float32))


def mlstm_chunk_scan(q, k, v, logi, logf, state0, with_outputs):
    B, H, T, _ = q.shape
    L = M_CHUNK
    nc = T // L

    def chunks(t):
        return jnp.moveaxis(t.reshape((B, H, nc, L) + t.shape[3:]), 2, 0)

    tri = jnp.tril(jnp.ones((L, L), dtype=bool))

    def step(carry, inp):
        C, n, m = carry
        qc, kc, vc, li, lf = inp
        b = jnp.cumsum(lf, axis=-1)
        bL = b[..., -1]
        g_s = bL[..., None] - b + li
        m_new = jnp.maximum(bL + m, jnp.max(g_s, axis=-1))
        a = jnp.exp(bL + m - m_new)
        ws = jnp.exp(g_s - m_new[..., None])
        C_new = a[..., None, None] * C + jnp.einsum('bhs,bhsd,bhse->bhde', ws, kc, vc)
        n_new = a[..., None] * n + jnp.einsum('bhs,bhsd->bhd', ws, kc)
        if not with_outputs:
            return (C_new, n_new, m_new), None
        dmat = jnp.where(tri, b[..., :, None] - b[..., None, :] + li[..., None, :], -jnp.inf)
        inter = b + m[..., None]
        m_t = jnp.maximum(inter, jnp.max(dmat, axis=-1))
        w_inter = jnp.exp(inter - m_t)
        s = jnp.einsum('bhtd,bhsd->bhts', qc, kc) * jnp.exp(dmat - m_t[..., None])
        num = w_inter[..., None] * jnp.einsum('bhtd,bhde->bhte', qc, C) + jnp.einsum('bhts,bhse->bhte', s, vc)
        den = w_inter * jnp.einsum('bhtd,bhd->bht', qc, n) + jnp.sum(s, axis=-1)
        h = num / jnp.maximum(jnp.abs(den), jnp.exp(-m_t))[..., None]
        return (C_new, n_new, m_new), h

    state, hs = lax.scan(step, state0, (chunks(q), chunks(k), chunks(v), chunks(logi), chunks(logf)))
    if with_outputs:
        hs = jnp.moveaxis(hs, 0, 2).reshape(B, H, T, M_DV)
    return hs, state


def mlstm_direction(q, k, v, gates, state0, reverse, with_outputs):
    logi, logf = gates
    if reverse:
        q, k, v = q[:, :, ::-1], k[:, :, ::-1], v[:, :, ::-1]
        logi, logf = logi[..., ::-1], logf[..., ::-1]
    h, state = mlstm_chunk_scan(q, k, v, logi, logf, state0, with_outputs)
    if reverse and with_outputs:
        h = h[:, :, ::-1]
    return h, state


def mlstm_readout(h, o, norm_g, w_out):
    B, H, T, _ = h.shape
    hn = h * lax.rsqrt(jnp.mean(h * h, axis=-1, keepdims=True) + EPS)
    hn = hn * norm_g.astype(jnp.float32).reshape(H, 1, M_DV)
    hn = hn.transpose(0, 2, 1, 3).reshape(B, T, H * M_DV)
    y = (hn * jax.nn.sigmoid(o.astype(jnp.float32))).astype(o.dtype)
    return y @ w_out


def mlstm_mixer(hx, hc, w_in, b_in, norm_g, w_out, ctx_outputs):
    B = hx.shape[0]
    s0 = mlstm_zero_state(B)
    qc, kc, vc, oc, gfc, gbc = mlstm_project(hc, w_in, b_in)
    hcf, st_f = mlstm_direction(qc, kc, vc, gfc, s0, False, ctx_outputs)
    hcb, st_b = mlstm_direction(qc, kc, vc, gbc, s0, True, ctx_outputs)
    qx, kx, vx, ox, gfx, gbx = mlstm_project(hx, w_in, b_in)
    hxf, _ = mlstm_direction(qx, kx, vx, gfx, st_f, False, True)
    hxb, _ = mlstm_direction(qx, kx, vx, gbx, st_b, True, True)
    yx = mlstm_readout(hxf + hxb, ox, norm_g, w_out)
    yc = mlstm_readout(hcf + hcb, oc, norm_g, w_out) if ctx_outputs else None
    return yx, yc


def fourier_mixer(h, w_out, b_out):
    B, T, D = h.shape
    hg = h.astype(jnp.float32).reshape(B, T, F_GROUPS, F_GROUP_DIM)
    y = jnp.real(jnp.fft.fft2(hg, axes=(1, 3), norm="ortho")).reshape(B, T, D).astype(h.dtype)
    return y @ w_out + b_out


def dwconv3(g, w, b, axis):
    n = g.shape[axis]
    pad = [(0, 0)] * g.ndim
    pad[axis] = (1, 1)
    gp = jnp.pad(g, pad)
    return (lax.slice_in_dim(gp, 0, n, axis=axis) * w[0]
            + lax.slice_in_dim(gp, 1, n + 1, axis=axis) * w[1]
            + lax.slice_in_dim(gp, 2, n + 2, axis=axis) * w[2] + b)


def conv_ffn(h, up_w, conv_w, conv_b, down_w, on_grid):
    u, g = jnp.split(h @ up_w, 2, axis=-1)
    if on_grid:
        B, T, F = g.shape
        rows = T // GRID_W
        g = dwconv3(g.reshape(B, rows, GRID_W, F), conv_w, conv_b, axis=2).reshape(B, T, F)
    else:
        g = dwconv3(g, conv_w, conv_b, axis=1)
    return (jax.nn.silu(g) * u) @ down_w


def setup_inputs(seed: int = 0) -> dict:
    key = jax.random.key(seed)
    ks = jax.random.split(key, 24)
    D, F = D_MODEL, FFN_DIM
    NA = (DEPTH + N_MIXERS - 1) // N_MIXERS
    NB = DEPTH // N_MIXERS
    nrm = jax.random.normal
    x = nrm(ks[0], (BATCH, SEQ, D), jnp.float32)
    c = nrm(ks[1], (BATCH, D), jnp.float32)
    ctx = nrm(ks[2], (BATCH, CTX_LEN, D), jnp.float32)
    c_ctx = nrm(ks[3], (D,), jnp.float32)
    ada_w = nrm(ks[4], (DEPTH, D, 6 * D), jnp.float32) * D ** -0.5
    ada_b = 0.02 * nrm(ks[5], (DEPTH, 6 * D), jnp.float32)
    pre_mix_g = 1.0 + 0.05 * nrm(ks[6], (DEPTH, D), jnp.float32)
    post_mix_g = 1.0 + 0.05 * nrm(ks[7], (DEPTH, D), jnp.float32)
    pre_ffn_g = 1.0 + 0.05 * nrm(ks[8], (DEPTH, D), jnp.float32)
    post_ffn_g = 1.0 + 0.05 * nrm(ks[9], (DEPTH, D), jnp.float32)
    ffn_up_w = nrm(ks[10], (DEPTH, D, 2 * F), jnp.float32) * D ** -0.5
    ffn_conv_w = nrm(ks[11], (DEPTH, CONV_W, F), jnp.float32) * CONV_W ** -0.5
    ffn_conv_b = 0.02 * nrm(ks[12], (DEPTH, F), jnp.float32)
    ffn_down_w = nrm(ks[13], (DEPTH, F, D), jnp.float32) * F ** -0.5
    m_in_w = nrm(ks[14], (NA, D, M_PROJ), jnp.float32) * D ** -0.5
    lin_b = 0.02 * nrm(ks[15], (NA, M_PROJ - 4 * M_HEADS), jnp.float32)
    f_base = jnp.linspace(3.0, 6.0, M_HEADS, dtype=jnp.float32)
    gn = 0.1 * nrm(ks[16], (NA, 4, M_HEADS), jnp.float32)
    gate_b = gn + jnp.stack([jnp.zeros_like(f_base), f_base, jnp.zeros_like(f_base), f_base])[None]
    m_in_b = jnp.concatenate([lin_b, gate_b.reshape(NA, 4 * M_HEADS)], axis=-1)
    m_norm_g = 1.0 + 0.05 * nrm(ks[17], (NA, M_HEADS * M_DV), jnp.float32)
    m_out_w = nrm(ks[18], (NA, M_HEADS * M_DV, D), jnp.float32) * (M_HEADS * M_DV) ** -0.5
    f_out_w = nrm(ks[19], (NB, D, D), jnp.float32) * D ** -0.5
    f_out_b = 0.02 * nrm(ks[20], (NB, D), jnp.float32)
    return {"x": x, "c": c, "ctx": ctx, "c_ctx": c_ctx,
            "ada_w": ada_w, "ada_b": ada_b,
            "pre_mix_g": pre_mix_g, "post_mix_g": post_mix_g,
            "pre_ffn_g": pre_ffn_g, "post_ffn_g": post_ffn_g,
            "ffn_up_w": ffn_up_w, "ffn_conv_w": ffn_conv_w, "ffn_conv_b": ffn_conv_b,
            "ffn_down_w": ffn_down_w,
            "m_in_w": m_in_w, "m_in_b": m_in_b, "m_norm_g": m_norm_g, "m_out_w": m_out_w,
            "f_out_w": f_out_w, "f_out_b": f_out_b}


def reference(x, c, ctx, c_ctx, ada_w, ada_b, pre_mix_g, post_mix_g, pre_ffn_g, post_ffn_g,
              ffn_up_w, ffn_conv_w, ffn_conv_b, ffn_down_w,
              m_in_w, m_in_b, m_norm_g, m_out_w, f_out_w, f_out_b):
    cx = jax.nn.silu(c)
    cc = jax.nn.silu(c_ctx)
    h_ctx = ctx
    for i in range(DEPTH):
        is_mlstm = (i % N_MIXERS) == 0
        j = i // N_MIXERS
        ctx_out = _ctx_stream_needed(i)
        sh1, sc1, g1, sh2, sc2, g2 = jnp.split(cx @ ada_w[i] + ada_b[i], 6, axis=-1)
        if is_mlstm or ctx_out:
            csh1, csc1, cg1, csh2, csc2, cg2 = jnp.split(cc @ ada_w[i] + ada_b[i], 6, axis=-1)
        hx = modulate(rmsnorm(x, pre_mix_g[i]), sh1, sc1)
        if is_mlstm:
            hc = modulate(rmsnorm(h_ctx, pre_mix_g[i]), csh1, csc1)
            yx, yc = mlstm_mixer(hx, hc, m_in_w[j], m_in_b[j], m_norm_g[j], m_out_w[j], ctx_out)
        else:
            yx = fourier_mixer(hx, f_out_w[j], f_out_b[j])
            if ctx_out:
                hc = modulate(rmsnorm(h_ctx, pre_mix_g[i]), csh1, csc1)
                yc = fourier_mixer(hc, f_out_w[j], f_out_b[j])
        x = x + g1[..., None, :] * rmsnorm(yx, post_mix_g[i])
        if ctx_out:
            h_ctx = h_ctx + cg1[..., None, :] * rmsnorm(yc, post_mix_g[i])
        hx = modulate(rmsnorm(x, pre_ffn_g[i]), sh2, sc2)
        yx = conv_ffn(hx, ffn_up_w[i], ffn_conv_w[i], ffn_conv_b[i], ffn_down_w[i], True)
        x = x + g2[..., None, :] * rmsnorm(yx, post_ffn_g[i])
        if ctx_out:
            hc = modulate(rmsnorm(h_ctx, pre_ffn_g[i]), csh2, csc2)
            yc = conv_ffn(hc, ffn_up_w[i], ffn_conv_w[i], ffn_conv_b[i], ffn_down_w[i], False)
            h_ctx = h_ctx + cg2[..., None, :] * rmsnorm(yc, post_ffn_g[i])
    return x
```

```python
from contextlib import ExitStack
import numpy as np
import ml_dtypes
import concourse.bass as bass
import concourse.mybir as mybir
from concourse.bass_utils import run_bass_kernel_spmd

F32 = mybir.dt.float32
BF16 = mybir.dt.bfloat16
AF = mybir.ActivationFunctionType
ALU = mybir.AluOpType
AX = mybir.AxisListType

D = 1024
T = 2048
TC = 256
TT = T + TC
NCH = TT // 128
H = 4
DK = 128
DV = 256
FF = 2816
NF = FF // 128
EPS = 1e-6
BIG = 30000.0


class Buf:
    __slots__ = ("name", "w", "r", "dsem", "dcount", "excl")

    def __init__(self, name, excl=False):
        self.name = name
        self.excl = excl
        self.w = None
        self.r = {}
        self.dsem = None
        self.dcount = 0


class Sched:
    def __init__(self, nc, stack):
        self.nc = nc
        self.stack = stack
        self.eng = {}
        self.sems = {}
        for name, h in (("pe", nc.tensor), ("act", nc.scalar), ("dve", nc.vector),
                        ("pool", nc.gpsimd), ("sp", nc.sync)):
            sem = stack.enter_context(nc.semaphore("s_" + name))
            self.eng[name] = {"h": h, "sem": sem, "count": 0, "waited": {}}
            self.sems[name] = sem
        self.dma_bufs = {}
        self.nsem = 5
        self.nops = {k: 0 for k in self.eng}
        self.stopped = False
        self.rec = None

    def _wait(self, ename, tok):
        key, val = tok
        if key in self.dma_bufs:
            val = max(val, self.dma_bufs[key].dcount)
        if key == "pe" and ename == "pe":
            return
        e = self.eng[ename]
        if e["waited"].get(key, 0) >= val:
            return
        e["waited"][key] = val
        e["h"].wait_ge(self.sems[key], val)

    def _deps(self, ename, reads, writes):
        for b in reads:
            if b.w is not None:
                self._wait(ename, b.w)
            if b.excl:
                for k, v in b.r.items():
                    if k != ename:
                        self._wait(ename, (k, v))
        for b in writes:
            if b.w is not None:
                self._wait(ename, b.w)
            for k, v in b.r.items():
                self._wait(ename, (k, v))

    def _mark(self, tok, reads, writes):
        k, v = tok
        for b in reads:
            if b.r.get(k, 0) < v:
                b.r[k] = v
        for b in writes:
            b.w = tok
            b.r = {}

    def op(self, ename, fn, reads=(), writes=()):
        if self.stopped:
            return
        if self.rec is not None:
            self.rec.append(("op", ename, fn, list(reads), list(writes), {}))
            return
        e = self.eng[ename]
        self._deps(ename, reads, writes)
        inst = fn(e["h"])
        e["count"] += 1
        self.nops[ename] += 1
        inst.then_inc(e["sem"], 1)
        self._mark((ename, e["count"]), reads, writes)

    def dma(self, qname, out_ap, in_ap, reads=(), writes=(), **kw):
        if self.stopped:
            return
        if self.rec is not None:
            self.rec.append(("dma", qname, (out_ap, in_ap), list(reads), list(writes), kw))
            return
        e = self.eng[qname]
        self._deps(qname, reads, writes)
        dst = writes[0]
        if dst.dsem is None:
            dst.dsem = "d_" + dst.name
            self.sems[dst.dsem] = self.stack.enter_context(self.nc.semaphore(dst.dsem))
            self.dma_bufs[dst.dsem] = dst
            self.nsem += 1
        dst.dcount += 16
        e["h"].dma_start(out=out_ap, in_=in_ap, **kw).then_inc(self.sems[dst.dsem], 16)
        self._mark((dst.dsem, dst.dcount), reads, writes)

    def replay(self, rec, n):
        for _ in range(n):
            if not rec:
                return
            kind, en, x, r, w, kw = rec.pop(0)
            if kind == "barrier":
                self.barrier()
            elif kind == "op":
                self.op(en, x, r, w)
            else:
                self.dma(en, x[0], x[1], r, w, **kw)

    def barrier(self):
        if self.stopped:
            return
        if self.rec is not None:
            self.rec.append(("barrier", None, None, [], [], {}))
            return
        toks = [(n, e["count"]) for n, e in self.eng.items() if e["count"] > 0]
        toks += [(k, b.dcount) for k, b in self.dma_bufs.items()]
        for n in self.eng:
            for t in toks:
                if not (t[0] == "pe" and n == "pe"):
                    self._wait(n, t)

    def finish(self, out_bufs):
        for b in out_bufs:
            if b.w is not None:
                self._wait("sp", b.w)


class _Stop(Exception):
    pass


def V(base, dims):
    return bass.AP(base.tensor, base.offset, [base.ap[0]] + [list(d) for d in dims])


VOFF = {}
_o = 0
for _n, _w in (("adab0", 48), ("adab1", 48), ("premix", 16), ("postmix", 16), ("preffn", 16),
               ("postffn", 16), ("bq", 4), ("bk", 4), ("fb", 8), ("convw", 132), ("convb", 44),
               ("gbi", 1), ("gbf", 1), ("rsf", 1), ("rsb", 1), ("ngc", 8)):
    VOFF[_n] = _o
    _o += _w
NV = _o

C32 = {"maskf": 0, "maskb": 128, "id8": 256, "sel": 264}
NC32 = 264 + 8 * 128
CBF = {"ident": 0, "ones": 128, "dftd": 256}
NCBF = 512


def pchunk(w):
    K, N = w.shape
    return np.ascontiguousarray(w.reshape(K // 128, 128, N).transpose(1, 0, 2))


def colvec(v):
    return np.ascontiguousarray(v.reshape(-1, 128).T)


def host_shared(inp):
    f32 = np.float32
    sh = {}
    ada_w = np.asarray(inp["ada_w"], f32)
    sh["adaw"] = np.ascontiguousarray(ada_w.reshape(2, 8, 128, 6 * D).transpose(0, 2, 1, 3))
    vecs = np.zeros((128, NV), f32)
    ada_b = np.asarray(inp["ada_b"], f32)
    for l in range(2):
        vecs[:, VOFF["adab%d" % l]:VOFF["adab%d" % l] + 48] = colvec(ada_b[l])
    for nm, key in (("premix", "pre_mix_g"), ("postmix", "post_mix_g"), ("preffn", "pre_ffn_g"),
                    ("postffn", "post_ffn_g")):
        a = np.asarray(inp[key], f32)
        for l in range(2):
            vecs[:, VOFF[nm] + 8 * l:VOFF[nm] + 8 * l + 8] = colvec(a[l])
    mb = np.asarray(inp["m_in_b"], f32)[0]
    vecs[:, VOFF["bq"]:VOFF["bq"] + 4] = colvec(mb[0:512])
    vecs[:, VOFF["bk"]:VOFF["bk"] + 4] = colvec(mb[512:1024])
    vecs[:, VOFF["fb"]:VOFF["fb"] + 8] = colvec(np.asarray(inp["f_out_b"], f32)[0])
    cw = np.asarray(inp["ffn_conv_w"], f32)
    cb = np.asarray(inp["ffn_conv_b"], f32)
    for l in range(2):
        for j in range(3):
            o = VOFF["convw"] + (l * 3 + j) * 22
            vecs[:, o:o + 22] = colvec(cw[l, j])
        o = VOFF["convb"] + l * 22
        vecs[:, o:o + 22] = colvec(cb[l])
    gperm = [0, 1, 2, 3, 8, 9, 10, 11, 4, 5, 6, 7, 12, 13, 14, 15]
    gb = mb[3072:3088][gperm]
    vecs[0:8, VOFF["gbi"]] = gb[0:8]
    vecs[0:8, VOFF["gbf"]] = gb[8:16]
    vecs[0:4, VOFF["rsf"]] = 1.0
    vecs[4:8, VOFF["rsb"]] = 1.0
    vecs[:, VOFF["ngc"]:VOFF["ngc"] + 8] = colvec(np.asarray(inp["m_norm_g"], f32)[0])
    sh["vecs"] = vecs
    ng = np.asarray(inp["m_norm_g"], f32)[0]
    rv = np.zeros((H, 640), f32)
    for h in range(H):
        rv[h, 0:128] = mb[512 + h * 128:512 + (h + 1) * 128]
        rv[h, 128:384] = mb[1024 + h * 256:1024 + (h + 1) * 256]
        rv[h, 384:640] = mb[2048 + h * 256:2048 + (h + 1) * 256]
    sh["rowv"] = rv
    miw = np.asarray(inp["m_in_w"], f32)[0]
    whp = np.zeros((128, H, 8, 768), f32)
    for h in range(H):
        cols = np.concatenate([np.arange(h * 128, (h + 1) * 128), 512 + np.arange(h * 128, (h + 1) * 128),
                               1024 + np.arange(h * 256, (h + 1) * 256),
                               2048 + np.arange(h * 256, (h + 1) * 256)])
        whp[:, h] = pchunk(miw[:, cols])
    sh["whp"] = whp
    sh["wgp"] = pchunk(miw[:, 3072 + np.array(gperm)])
    sh["wmo"] = pchunk(np.asarray(inp["m_out_w"], f32)[0])
    sh["wfo"] = pchunk(np.asarray(inp["f_out_w"], f32)[0])
    up = np.asarray(inp["ffn_up_w"], f32)
    upp = np.zeros((2, 128, NF, 8, 256), f32)
    for l in range(2):
        pc = pchunk(up[l])
        upp[l, :, :, :, 0:128] = pc[:, :, 0:FF].reshape(128, 8, NF, 128).transpose(0, 2, 1, 3)
        upp[l, :, :, :, 128:256] = pc[:, :, FF:2 * FF].reshape(128, 8, NF, 128).transpose(0, 2, 1, 3)
    sh["upp"] = upp
    dn = np.asarray(inp["ffn_down_w"], f32)
    sh["dnp"] = np.stack([pchunk(dn[l]) for l in range(2)])
    c32 = np.zeros((128, NC32), f32)
    s_i = np.arange(128)[:, None]
    t_i = np.arange(128)[None, :]
    c32[:, C32["maskf"]:C32["maskf"] + 128] = np.where(s_i <= t_i, 0.0, BIG)
    c32[:, C32["maskb"]:C32["maskb"] + 128] = np.where(s_i >= t_i, 0.0, BIG)
    c32[0:8, C32["id8"]:C32["id8"] + 8] = np.eye(8)
    sel = np.zeros((8, 8, 128), f32)
    for j in range(8):
        sel[j, j, :] = 1.0
    c32[0:8, C32["sel"]:] = sel.reshape(8, 8 * 128)
    sh["c32"] = c32
    cbf = np.zeros((128, NCBF), f32)
    cbf[:, 0:128] = np.eye(128)
    cbf[:, 128:256] = 1.0
    dd = np.arange(128)
    ang = 2.0 * np.pi * np.outer(dd, dd) / 128.0
    cbf[:, 256:384] = np.cos(ang) / 512.0
    cbf[:, 384:512] = np.sin(ang) / 512.0
    sh["cbf"] = cbf.astype(ml_dtypes.bfloat16)
    p_i = np.arange(128)[:, None, None, None]
    par = np.arange(2)[None, :, None, None]
    ii = np.arange(8)[None, None, :, None]
    k1 = np.arange(1024)[None, None, None, :]
    tt = 2 * (ii * 128 + p_i) + par
    ph = (tt * k1) % 2048
    a2 = 2.0 * np.pi * ph.astype(np.float64) / 2048.0
    dft2 = np.stack([np.cos(a2), -np.sin(a2)], axis=1).astype(f32)
    sh["dft2"] = dft2.astype(ml_dtypes.bfloat16)
    return sh


def host_percore(inp, b):
    f32 = np.float32
    x = np.asarray(inp["x"], f32)[b]
    ctx = np.asarray(inp["ctx"], f32)[b]
    xc = np.ascontiguousarray(np.concatenate([ctx.T, x.T], axis=1))
    cv = np.zeros((128, 16), f32)
    cv[:, 0:8] = colvec(np.asarray(inp["c"], f32)[b])
    cv[:, 8:16] = colvec(np.asarray(inp["c_ctx"], f32))
    return {"xc": xc, "cv": cv}


def build(dbg=None):
    dbg = dbg or set()
    nc = bass.Bass("TRN2", target_bir_lowering=False)
    dram_in = lambda n, s, dt=F32: nc.dram_tensor(n, list(s), dt, kind="ExternalInput").ap()
    xc_d = dram_in("xc", [D, TT])
    cv_d = dram_in("cv", [128, 16])
    adaw_d = dram_in("adaw", [2, 128, 8, 6 * D])
    vecs_d = dram_in("vecs", [128, NV])
    rowv_d = dram_in("rowv", [H, 640])
    whp_d = dram_in("whp", [128, H, 8, 768])
    wgp_d = dram_in("wgp", [128, 8, 16])
    wmo_d = dram_in("wmo", [128, 8, D])
    wfo_d = dram_in("wfo", [128, 8, D])
    upp_d = dram_in("upp", [2, 128, NF, 8, 256])
    dnp_d = dram_in("dnp", [2, 128, NF, D])
    c32_d = dram_in("c32", [128, NC32])
    cbf_d = dram_in("cbf", [128, NCBF], BF16)
    dft2_d = dram_in("dft2", [128, 2, 2, 8, 1024], BF16)
    out_d = nc.dram_tensor("out", [D, T], F32, kind="ExternalOutput").ap()
    yt_d = nc.dram_tensor("yt_scr", [128, 8, T], BF16, kind="ExternalOutput").ap()
    dbg_out = {}

    xc_v = xc_d.rearrange("(k p) t -> p k t", p=128)
    out_v = out_d.rearrange("(k p) t -> p k t", p=128)

    with ExitStack() as st:
        S = Sched(nc, st)

        uid = [0]

        def sb(name, shape, dt, stack=st):
            uid[0] += 1
            return stack.enter_context(nc.sbuf_tensor("sb%d_%s" % (uid[0], name), list(shape), dt))

        XT = sb("XT", [128, 8, T], F32)
        XTf = XT[:].rearrange("p k t -> p (k t)")
        XTB = [Buf("XT%d" % i) for i in range(4)]
        vecs = sb("vecs", [128, NV], F32); VECS = Buf("vecs")
        c32 = sb("c32", [128, NC32], F32); C32B = Buf("c32")
        cbf = sb("cbf", [128, NCBF], BF16); CBFB = Buf("cbf")
        cv = sb("cv", [128, 16], F32); CVB = Buf("cv")
        scv = sb("scv", [128, 16], F32); SCVB = Buf("scv")
        mod = sb("mod", [128, 2, 48], F32); MODB = Buf("mod")
        cmod = sb("cmod", [128, 16], F32); CMODB = Buf("cmod")
        der = sb("der", [128, 2, 4, 8], F32); DERB = Buf("der")
        cder = sb("cder", [128, 8], F32); CDERB = Buf("cder")
        ident = cbf[:, 0:128]
        ones = cbf[:, 128:256]
        dftd = cbf[:, 256:512]

        PS = []
        psbs = [st.enter_context(nc.psum_tensor("psb%d" % i, [128, 1024], BF16)) for i in range(2)]
        NPS = 6
        for i in range(NPS):
            t = st.enter_context(nc.psum_tensor("ps%d" % i, [128, 512], F32))
            PS.append((t, Buf("ps%d" % i, excl=True)))
        ps_i = [0]

        def ps_next():
            r = PS[ps_i[0] % NPS]
            ps_i[0] += 1
            return r

        def vcol(name, i=0, n=1):
            return vecs[:, VOFF[name] + i:VOFF[name] + i + n]

        def stop(name):
            if name in dbg:
                S.barrier()
                S.stopped = True

        def dump(name, ap_sb, shape, dt, bufs):
            if name not in dbg:
                return
            d = nc.dram_tensor("dbg_" + name, list(shape), dt, kind="ExternalOutput").ap()
            B = Buf("dbg_" + name)
            S.dma("sp", d, ap_sb, reads=bufs, writes=[B])
            dbg_out[name] = B

        try:
            S.dma("sp", vecs[:], vecs_d[:, :], writes=[VECS])
            S.dma("sp", cv[:], cv_d[:, :], writes=[CVB])
            S.dma("sp", c32[:], c32_d[:, :], writes=[C32B])
            S.dma("sp", cbf[:], cbf_d[:, :], writes=[CBFB])

            scvb = sb("scvb", [128, 16], BF16); SCVBB = Buf("scvb")
            S.op("act", lambda e: e.activation(out=scv[:], in_=cv[:], func=AF.Silu), reads=[CVB], writes=[SCVB])
            S.op("dve", lambda e: e.tensor_copy(scvb[:], scv[:]), reads=[SCVB], writes=[SCVBB])
            ada_state = {"next_dma": 0, "next_pe": 0, "bufs": None}
            ada_items = [(l, nb) for l in range(2) for nb in range(24)]

            def ada_dma(n):
                ada, ADAB, modrow, MRB = ada_state["bufs"]
                if n >= ada_state.get("limit", len(ada_items)):
                    return
                l, nb = ada_items[n]
                S.dma("pool", ada[n % 2][:], adaw_d[l, :, :, nb * 256:(nb + 1) * 256], writes=[ADAB[n % 2]])

            def ada_pe(n):
                ada, ADAB, modrow, MRB = ada_state["bufs"]
                l, nb = ada_items[n]
                bi = n % 2
                pt, PB = ps_next()

                def mm(e, pt=pt, bi=bi):
                    last = None
                    for kk in range(8):
                        last = e.matmul(pt[0:2, 0:256], V(scvb[:, kk:kk + 1], [[8, 2]]), ada[bi][:, kk, :],
                                        start=(kk == 0), stop=(kk == 7))
                    return last
                S.op("pe", mm, reads=[ADAB[bi], SCVBB], writes=[PB])
                mr, MB_ = modrow[n % 2], MRB[n % 2]
                S.op("act", lambda e, pt=pt, mr=mr: e.copy(mr[:, :], pt[0:2, 0:256]), reads=[PB], writes=[MB_])
                pt2, PB2 = ps_next()

                def mmT(e, pt2=pt2, mr=mr):
                    last = None
                    for c4 in range(2):
                        last = e.matmul(pt2[:, c4 * 2:c4 * 2 + 2], mr[:, c4 * 128:(c4 + 1) * 128],
                                        c32[0:2, C32["id8"]:C32["id8"] + 2], start=True, stop=True)
                    return last
                S.op("pe", mmT, reads=[MB_, C32B], writes=[PB2])
                S.op("dve", lambda e, pt2=pt2, l=l, nb=nb: e.tensor_tensor(
                    out=mod[:, l, nb * 2:nb * 2 + 2], in0=V(pt2[:, 0:1], [[2, 2]]),
                    in1=vcol("adab%d" % l, nb * 2, 2), op=ALU.add), reads=[PB2, VECS], writes=[MODB])
                if l == 0 and nb < 8:
                    S.op("dve", lambda e, pt2=pt2, nb=nb: e.tensor_tensor(
                        out=cmod[:, nb * 2:nb * 2 + 2], in0=V(pt2[:, 1:2], [[2, 2]]),
                        in1=vcol("adab0", nb * 2, 2), op=ALU.add), reads=[PB2, VECS], writes=[CMODB])

            def ada_more(k):
                for _ in range(k):
                    n = ada_state["next_pe"]
                    if n >= ada_state.get("limit", len(ada_items)):
                        break
                    ada_pe(n)
                    ada_state["next_pe"] = n + 1
                    ada_dma(n + 2)
                for l in range(2):
                    if ada_state["next_pe"] >= 24 * (l + 1) and not ada_state.get("done%d" % l):
                        ada_state["done%d" % l] = True
                        S.op("dve", lambda e, l=l: e.tensor_tensor(
                            out=der[:, l, 1, :], in0=mod[:, l, 16:24], in1=vcol("postmix", 8 * l, 8), op=ALU.mult),
                            reads=[MODB, VECS], writes=[DERB])
                        S.op("dve", lambda e, l=l: e.scalar_tensor_tensor(
                            out=der[:, l, 2, :], in0=mod[:, l, 32:40], scalar=1.0, in1=vcol("preffn", 8 * l, 8),
                            op0=ALU.add, op1=ALU.mult), reads=[MODB, VECS], writes=[DERB])
                        S.op("dve", lambda e, l=l: e.tensor_tensor(
                            out=der[:, l, 3, :], in0=mod[:, l, 40:48], in1=vcol("postffn", 8 * l, 8), op=ALU.mult),
                            reads=[MODB, VECS], writes=[DERB])
                        if l == 1:
                            S.op("dve", lambda e: e.scalar_tensor_tensor(
                                out=der[:, 1, 0, :], in0=mod[:, 1, 8:16], scalar=1.0, in1=vcol("premix", 8, 8),
                                op0=ALU.add, op1=ALU.mult), reads=[MODB, VECS], writes=[DERB])

            def rstd_from_sq(sq, SQB, nb, sd, SDB, rstd, RSB, ndiv=float(D)):
                pt, PB = ps_next()

                def mm(e):
                    last = None
                    for k in range(8):
                        last = e.matmul(pt[:, 0:nb], ones, sq[:, k, 0:nb], start=(k == 0), stop=(k == 7))
                    return last
                S.op("pe", mm, reads=[SQB, CBFB], writes=[PB])
                S.op("act", lambda e: e.activation(out=sd[:, 0:nb], in_=pt[:, 0:nb], func=AF.Ln,
                                                   bias=EPS, scale=1.0 / ndiv), reads=[PB], writes=[SDB])
                S.op("act", lambda e: e.activation(out=rstd[:, 0:nb], in_=sd[:, 0:nb], func=AF.Exp, scale=-0.5),
                     reads=[SDB], writes=[RSB])

            with ExitStack() as ph:
                ada = [sb("ada%d" % i, [128, 8, 256], BF16, ph) for i in range(2)]
                ADAB = [Buf("ada%d" % i) for i in range(2)]
                modrow = [sb("modrow%d" % i, [2, 256], F32, ph) for i in range(2)]
                MRB = [Buf("modrow%d" % i) for i in range(2)]
                ada_state["bufs"] = (ada, ADAB, modrow, MRB)
                ada_state["limit"] = 24
                for n in range(2):
                    ada_dma(n)
                ada_more(8)
                S.op("dve", lambda e: e.scalar_tensor_tensor(
                    out=der[:, 0, 0, :], in0=mod[:, 0, 8:16], scalar=1.0, in1=vcol("premix", 0, 8),
                    op0=ALU.add, op1=ALU.mult), reads=[MODB, VECS], writes=[DERB])
                S.op("dve", lambda e: e.scalar_tensor_tensor(
                    out=cder[:], in0=cmod[:, 8:16], scalar=1.0, in1=vcol("premix", 0, 8),
                    op0=ALU.add, op1=ALU.mult), reads=[CMODB, VECS], writes=[CDERB])
                hxT = sb("hxT", [128, 8, TT], BF16, ph)
                blocks = [(0, 256)] + [(256 + 512 * i, 512) for i in range(4)]
                HXB = [Buf("hx%d" % i) for i in range(5)]

                with ExitStack() as p1:
                    xb = [sb("xb%d" % i, [128, 8, 512], F32, p1) for i in range(2)]
                    XBB = [Buf("xb%d" % i) for i in range(2)]
                    sqs = [sb("sq1_%d" % i, [128, 8, 512], BF16, p1) for i in range(2)]
                    SQBS = [Buf("sq1_%d" % i) for i in range(2)]
                    sds = [sb("sd1_%d" % i, [128, 512], F32, p1) for i in range(2)]
                    SDBS = [Buf("sd1_%d" % i) for i in range(2)]
                    rstds = [sb("rstd1_%d" % i, [128, 512], F32, p1) for i in range(2)]
                    RSBS = [Buf("rstd1_%d" % i) for i in range(2)]
                    tmp = [sb("tmp1_%d" % i, [128, 512], F32, p1) for i in range(4)]
                    TMPB = [Buf("tmp1_%d" % i) for i in range(4)]
                    for bi, (t0, nb) in enumerate(blocks):
                        x_ = xb[bi % 2]; XB_ = XBB[bi % 2]
                        sq, SQB, sd, SDB, rstd, RSB = sqs[bi % 2], SQBS[bi % 2], sds[bi % 2], SDBS[bi % 2], rstds[bi % 2], RSBS[bi % 2]
                        S.dma("sp", x_[:, :, 0:nb], xc_v[:, :, t0:t0 + nb], writes=[XB_])
                        S.op("act", lambda e, x_=x_, nb=nb, sq=sq: e.activation(out=sq[:, :, 0:nb], in_=x_[:, :, 0:nb],
                                                                         func=AF.Square), reads=[XB_], writes=[SQB])
                        rstd_from_sq(sq, SQB, nb, sd, SDB, rstd, RSB)
                        for k in range(8):
                            tm = tmp[k % 4]; TB = TMPB[k % 4]
                            if bi == 0:
                                a_col, b_col, AB, BB = cder[:, k:k + 1], cmod[:, k:k + 1], CDERB, CMODB
                            else:
                                a_col, b_col, AB, BB = der[:, 0, 0, k:k + 1], mod[:, 0, k:k + 1], DERB, MODB
                            S.op("dve", lambda e, x_=x_, k=k, nb=nb, tm=tm, a_col=a_col, rstd=rstd: e.scalar_tensor_tensor(
                                out=tm[:, 0:nb], in0=x_[:, k, 0:nb], scalar=a_col, in1=rstd[:, 0:nb],
                                op0=ALU.mult, op1=ALU.mult), reads=[XB_, RSB, AB], writes=[TB])
                            if k % 2 == 0:
                                S.op("act", lambda e, k=k, nb=nb, t0=t0, tm=tm, b_col=b_col: e.activation(
                                    out=hxT[:, k, t0:t0 + nb], in_=tm[:, 0:nb], func=AF.Identity, bias=b_col, scale=1.0),
                                    reads=[TB, BB], writes=[HXB[bi]])
                            else:
                                S.op("dve", lambda e, k=k, nb=nb, t0=t0, tm=tm, b_col=b_col: e.tensor_scalar(
                                    out=hxT[:, k, t0:t0 + nb], in0=tm[:, 0:nb], scalar1=b_col, scalar2=None, op0=ALU.add),
                                    reads=[TB, BB], writes=[HXB[bi]])
                    S.barrier()
                dump("hxT", hxT[:], [128, 8, TT], BF16, HXB)

                stop("stop1")

                RW = [XTf[0:8, i * TT:(i + 1) * TT] for i in range(7)]
                RB = [Buf("row%d" % i) for i in range(7)]
                ucng = sb("ucng", [128, NCH, 16], F32, ph); UCB = Buf("ucng")
                tots = sb("tots", [8, 4], F32, ph); TOTB = Buf("tots")
                rsf = vecs[0:8, VOFF["rsf"]:VOFF["rsf"] + 1]
                rsb = vecs[0:8, VOFF["rsb"]:VOFF["rsb"] + 1]
                with ExitStack() as p2:
                    wg = sb("wg", [128, 8, 16], BF16, ph); WGB = Buf("wg")
                    S.rec = []
                    S.dma("pool", wg[:], wgp_d[:, :, :], writes=[WGB])
                    gblocks = [(i * 512, min(512, TT - i * 512)) for i in range(5)]
                    for (t0, nb) in gblocks:
                        bsel = [HXB[0], HXB[1]] if t0 == 0 else ([HXB[(t0 - 256) // 512 + 1]] + ([HXB[(t0 - 256) // 512 + 2]] if t0 + nb > 256 + ((t0 - 256) // 512 + 1) * 512 else []))
                        for gi in range(2):
                            pt, PB = ps_next()

                            def mm(e, pt=pt, t0=t0, nb=nb, gi=gi):
                                last = None
                                for k in range(8):
                                    last = e.matmul(pt[0:8, 0:nb], wg[:, k, gi * 8:gi * 8 + 8], hxT[:, k, t0:t0 + nb],
                                                    start=(k == 0), stop=(k == 7))
                                return last
                            S.op("pe", mm, reads=[WGB] + HXB, writes=[PB])
                            bcol = vecs[0:8, VOFF["gbi" if gi == 0 else "gbf"]:VOFF["gbi" if gi == 0 else "gbf"] + 1]
                            S.op("act", lambda e, pt=pt, t0=t0, nb=nb, gi=gi, bcol=bcol: e.activation(
                                out=RW[gi][:, t0:t0 + nb], in_=pt[0:8, 0:nb], func=AF.Identity, bias=bcol, scale=1.0),
                                reads=[PB, VECS], writes=[RB[gi]])
                    S.op("act", lambda e: e.activation(out=RW[1], in_=RW[1], func=AF.Exp, scale=-1.0),
                         reads=[RB[1]], writes=[RB[1]])
                    S.op("act", lambda e: e.activation(out=RW[1], in_=RW[1], func=AF.Ln, bias=1.0, scale=1.0),
                         reads=[RB[1]], writes=[RB[1]])
                    S.op("pool", lambda e: e.memset(RW[3], 0.0), writes=[RB[3]])
                    for (a, b) in ((0, TC), (TC, TT)):
                        S.op("dve", lambda e, a=a, b=b: e.tensor_tensor_scan(
                            out=RW[2][:, a:b], data0=RW[1][:, a:b], data1=RW[3][:, a:b], initial=0.0,
                            op0=ALU.add, op1=ALU.add), reads=[RB[1], RB[3]], writes=[RB[2]])
                    S.op("dve", lambda e: e.tensor_copy(tots[:, 0:1], RW[2][:, TC - 1:TC]), reads=[RB[2]], writes=[TOTB])
                    S.op("dve", lambda e: e.tensor_tensor(out=tots[:, 1:2], in0=RW[2][:, TC - 1:TC], in1=RW[2][:, TT - 1:TT],
                                                          op=ALU.add), reads=[RB[2]], writes=[TOTB])
                    S.op("dve", lambda e: e.tensor_copy(RW[4][:, 0:TC], RW[2][:, 0:TC]), reads=[RB[2]], writes=[RB[4]])
                    S.op("dve", lambda e: e.tensor_scalar(out=RW[4][:, TC:TT], in0=RW[2][:, TC:TT], scalar1=tots[:, 0:1],
                                                          scalar2=None, op0=ALU.add), reads=[RB[2], TOTB], writes=[RB[4]])
                    S.op("dve", lambda e: e.tensor_tensor(out=RW[5], in0=RW[1], in1=RW[2], op=ALU.subtract),
                         reads=[RB[1], RB[2]], writes=[RB[5]])
                    S.op("dve", lambda e: e.tensor_scalar(out=RW[5][:, 0:TC], in0=RW[5][:, 0:TC], scalar1=tots[:, 0:1],
                                                          scalar2=None, op0=ALU.add), reads=[RB[5], TOTB], writes=[RB[5]])
                    S.op("dve", lambda e: e.tensor_scalar(out=RW[5][:, TC:TT], in0=RW[5][:, TC:TT], scalar1=tots[:, 1:2],
                                                          scalar2=None, op0=ALU.add), reads=[RB[5], TOTB], writes=[RB[5]])
                    S.op("dve", lambda e: e.tensor_scalar(out=RW[4], in0=RW[4], scalar1=rsf, scalar2=None, op0=ALU.mult),
                         reads=[RB[4], VECS], writes=[RB[4]])
                    S.op("dve", lambda e: e.scalar_tensor_tensor(out=RW[4], in0=RW[5], scalar=rsb, in1=RW[4],
                                                                 op0=ALU.mult, op1=ALU.add),
                         reads=[RB[5], RB[4], VECS], writes=[RB[4]])
                    S.op("dve", lambda e: e.tensor_tensor(out=RW[0], in0=RW[0], in1=RW[4], op=ALU.add),
                         reads=[RB[0], RB[4]], writes=[RB[0]])
                    S.op("dve", lambda e: e.tensor_tensor_scan(out=RW[5], data0=RW[0], data1=RW[0], initial=0.0,
                                                               op0=ALU.max, op1=ALU.max), reads=[RB[0]], writes=[RB[5]])
                    cur, CURB = RW[0], RB[0]
                    pp = 0
                    sh = 1
                    while sh < TT - TC:
                        nxt, NXTB = RW[2 + pp], RB[2 + pp]
                        for (a, b) in ((0, TC), (TC, TT)):
                            n = b - a
                            if sh < n:
                                S.op("dve", lambda e, a=a, b=b, sh=sh, cur=cur, nxt=nxt: e.tensor_tensor(
                                    out=nxt[:, a:b - sh], in0=cur[:, a:b - sh], in1=cur[:, a + sh:b], op=ALU.max),
                                    reads=[CURB], writes=[NXTB])
                                S.op("pool", lambda e, a=a, b=b, sh=sh, cur=cur, nxt=nxt: e.tensor_copy(
                                    nxt[:, b - sh:b], cur[:, b - sh:b]), reads=[CURB], writes=[NXTB])
                            else:
                                S.op("pool", lambda e, a=a, b=b, cur=cur, nxt=nxt: e.tensor_copy(
                                    nxt[:, a:b], cur[:, a:b]), reads=[CURB], writes=[NXTB])
                        cur, CURB = nxt, NXTB
                        pp ^= 1
                        sh *= 2
                    sm, SMB = cur, CURB
                    S.op("dve", lambda e: e.tensor_scalar(out=sm[:, 0:TC], in0=sm[:, 0:TC], scalar1=0.0, scalar2=None,
                                                          op0=ALU.max), reads=[SMB], writes=[SMB])
                    S.op("dve", lambda e: e.tensor_copy(tots[:, 2:3], sm[:, 0:1]), reads=[SMB], writes=[TOTB])
                    S.op("dve", lambda e: e.tensor_scalar(out=sm[:, TC:TT], in0=sm[:, TC:TT], scalar1=tots[:, 2:3],
                                                          scalar2=None, op0=ALU.max), reads=[SMB, TOTB], writes=[SMB])
                    S.op("dve", lambda e: e.tensor_scalar(out=RW[5], in0=RW[5], scalar1=rsf, scalar2=None, op0=ALU.mult),
                         reads=[RB[5], VECS], writes=[RB[5]])
                    S.op("dve", lambda e: e.scalar_tensor_tensor(out=RW[5], in0=sm, scalar=rsb, in1=RW[5],
                                                                 op0=ALU.mult, op1=ALU.add),
                         reads=[SMB, RB[5], VECS], writes=[RB[5]])
                    S.op("dve", lambda e: e.tensor_tensor(out=RW[4], in0=RW[4], in1=RW[5], op=ALU.subtract),
                         reads=[RB[4], RB[5]], writes=[RB[4]])
                    pt, PB = ps_next()

                    def mmT(e, pt=pt):
                        last = None
                        for c in range(NCH):
                            e.matmul(pt[:, c * 16:c * 16 + 8], RW[0][:, c * 128:(c + 1) * 128],
                                     c32[0:8, C32["id8"]:C32["id8"] + 8], start=True, stop=True)
                            last = e.matmul(pt[:, c * 16 + 8:c * 16 + 16], RW[4][:, c * 128:(c + 1) * 128],
                                            c32[0:8, C32["id8"]:C32["id8"] + 8], start=True, stop=True)
                        return last
                    S.op("pe", mmT, reads=[RB[0], RB[4], C32B], writes=[PB])
                    S.op("dve", lambda e, pt=pt: e.tensor_copy(ucng[:].rearrange("p c j -> p (c j)"), pt[:, 0:NCH * 16]),
                         reads=[PB], writes=[UCB])
                    S.op("dve", lambda e: e.tensor_copy(RW[0], RW[5]), reads=[RB[5], RB[0]], writes=[RB[0]])
                    S.barrier()
                gate_rec = S.rec
                S.rec = None
                if "stop2a" in dbg or "ucng" in dbg:
                    S.replay(gate_rec, 10 ** 6)
                dump("ucng", ucng[:], [128, NCH, 16], F32, [UCB])
                stop("stop2a")
                MROW, MROWB = RW[0], RB[0]

                MbD = [XTf[:, TT + d * 2 * TT:2 * TT + d * 2 * TT] for d in range(2)]
                zzD = [XTf[:, 2 * TT + d * 2 * TT:3 * TT + d * 2 * TT] for d in range(2)]
                MBB = [Buf("Mb%d" % d) for d in range(2)]
                ZB = [Buf("zz%d" % d) for d in range(2)]
                Hh = XTf[:, 5 * TT:5 * TT + 4096]; HHB = Buf("Hh")
                Mb3D = [m.rearrange("p (c t) -> p c t", t=128) for m in MbD]
                zz3D = [z.rearrange("p (c t) -> p c t", t=128) for z in zzD]
                Hh3 = Hh.rearrange("p (c e) -> p c e", e=256)
                with ExitStack() as p3:
                    wh = [sb("wh%d" % i, [128, 8, 768], BF16, p3) for i in range(1)]
                    WHB = [Buf("wh%d" % i) for i in range(1)]
                    rowb = sb("rowb", [128, 640], F32, p3); ROWB = Buf("rowb")
                    QT = sb("QT", [128, TT], BF16, p3); QTB = Buf("QT")
                    KT = sb("KT", [128, TT], BF16, p3); KTB = Buf("KT")
                    KV = sb("KV", [128, NCH, 385], BF16, p3); KVB = Buf("KV")
                    Osig = sb("Osig", [128, 16, 256], BF16, p3); OSB = Buf("Osig")
                    otmp = [sb("otmp%d" % i, [128, 256], F32, p3) for i in range(2)]
                    OTB = [Buf("otmp%d" % i) for i in range(2)]
                    QW = [sb("QW%d" % d, [128, TT], BF16, p3) for d in range(2)]
                    QWB = [Buf("QW%d" % d) for d in range(2)]
                    Dj = [sb("Dj%d" % d, [128, NCH, 128], BF16, p3) for d in range(2)]
                    DJB = [Buf("Dj%d" % d) for d in range(2)]
                    KS = [sb("KS%d" % d, [128, NCH, 128], BF16, p3) for d in range(2)]
                    KSB = [Buf("KS%d" % d) for d in range(2)]
                    Cst = [[sb("Cst%d_%d" % (d, i), [128, 257], F32, p3) for i in range(2)] for d in range(2)]
                    CSTB = [[Buf("Cst%d_%d" % (d, i)) for i in range(2)] for d in range(2)]
                    Cbf = [[sb("Cbf%d_%d" % (d, i), [128, 257], BF16, p3) for i in range(2)] for d in range(2)]
                    CBFB2 = [[Buf("Cbf%d_%d" % (d, i)) for i in range(2)] for d in range(2)]
                    sm18 = [sb("sm18_%d" % d, [128, 6, NCH], F32, p3) for d in range(2)]
                    SM18 = [Buf("sm18_%d" % d) for d in range(2)]
                    Sp = [sb("Sp%d" % i, [128, 128], BF16, p3) for i in range(4)]
                    SPB = [Buf("Sp%d" % i) for i in range(4)]
                    dsm = sb("dsm", [128, 4, 4], F32, p3); DSMB = [Buf("dsm%d" % i) for i in range(4)]
                    ssq = sb("ssq", [128, 3, 16], F32, p3); SSQB = Buf("ssq")
                    yh, YHB = Osig, OSB
                    ytb = [sb("ytb%d" % i, [128, 512], BF16, p3) for i in range(2)]
                    YTBB = [Buf("ytb%d" % i) for i in range(2)]
                    PSBH = [Buf("psb0", excl=True), Buf("psb1", excl=True)]
                    YTD = Buf("ytd")
                    qscale = float(DK) ** -0.5
                    dctr = [0]
                    spctr = [0]
                    S.dma("pool", wh[0][:], whp_d[:, 0, :, :], writes=[WHB[0]])
                    S.op("pool", lambda e: e.memset(KV[:, :, 384:385], 1.0), writes=[KVB])
                    junk = sb("junk", [128, 256], BF16, p3); JUNKB = Buf("junk")
                    pending_readout = []
                    pending_tr = []
                    ro_thunks = []

                    def readout(h):
                        for c in range(16):
                            S.op("act", lambda e, c=c: e.activation(out=junk[:, :], in_=Hh3[:, c, :], func=AF.Square,
                                                                    accum_out=ssq[:, 0, c:c + 1]),
                                 reads=[HHB], writes=[JUNKB, SSQB])
                        S.op("act", lambda e: e.activation(out=ssq[:, 1, :], in_=ssq[:, 0, :], func=AF.Sqrt, bias=EPS,
                                                           scale=1.0 / DV), reads=[SSQB], writes=[SSQB])
                        S.op("dve", lambda e: e.reciprocal(out=ssq[:, 2, :], in_=ssq[:, 1, :]), reads=[SSQB], writes=[SSQB])
                        for c in range(16):
                            ro_thunks.append(lambda c=c: S.op("dve", lambda e: e.scalar_tensor_tensor(
                                out=yh[:, c, :], in0=Hh3[:, c, :], scalar=ssq[:, 2, c:c + 1], in1=Osig[:, c, :],
                                op0=ALU.mult, op1=ALU.mult), reads=[HHB, SSQB, OSB], writes=[OSB]))
                        pending_tr.append(h)

                    def readout_tr(h):
                        tctr = 0
                        for i in range(2):
                            for cg in range(4):
                                hb = tctr % 2
                                tctr += 1

                                def tr(e, i=i, cg=cg, hb=hb):
                                    last = None
                                    for q in range(4):
                                        last = e.transpose(psbs[hb][:, q * 128:(q + 1) * 128],
                                                           yh[:, cg * 4 + q, i * 128:(i + 1) * 128], ident)
                                    return last
                                S.op("pe", tr, reads=[YHB, CBFB], writes=[PSBH[hb]])
                                S.op("act", lambda e, hb=hb: e.copy(ytb[hb][:, :], psbs[hb][:, 0:512]),
                                     reads=[PSBH[hb]], writes=[YTBB[hb]])
                                S.dma("sp", yt_d[:, 2 * h + i, cg * 512:(cg + 1) * 512], ytb[hb][:, :],
                                      reads=[YTBB[hb]], writes=[YTD])

                    orders = [list(range(NCH)), [1, 0] + list(range(NCH - 1, 1, -1))]
                    mcols = [C32["maskf"], C32["maskb"]]
                    for h in range(H):
                        whh, WHH = wh[0], WHB[0]
                        S.dma("sp", rowb[:], bass.AP(rowv_d.tensor, rowv_d[h:h + 1, :].offset, [[0, 128], [1, 640]]),
                              writes=[ROWB])
                        def emit_mb_prep(h=h):
                            for d in range(2):
                                j = d * 4 + h
                                Mb, Mb3 = MbD[d], Mb3D[d]
                                RN, RC, AL, WS, EE, T18 = [sm18[d][:, i, :] for i in range(6)]
                                SMB = SM18[d]
                                for (t0, nb) in gblocks:
                                    pt, PB = ps_next()
                                    S.op("pe", lambda e, pt=pt, t0=t0, nb=nb, j=j: e.matmul(
                                        pt[:, 0:nb], c32[0:8, C32["sel"] + j * 128:C32["sel"] + (j + 1) * 128],
                                        MROW[:, t0:t0 + nb], start=True, stop=True), reads=[C32B, MROWB], writes=[PB])
                                    S.op("act", lambda e, pt=pt, t0=t0, nb=nb, Mb=Mb: e.copy(Mb[:, t0:t0 + nb], pt[:, 0:nb]),
                                         reads=[PB], writes=[MBB[d]])
                                ucj = ucng[:, :, j]
                                ngj = ucng[:, :, 8 + j]
                                if d == 0:
                                    S.op("dve", lambda e, RN=RN, Mb=Mb: e.tensor_copy(RN, V(Mb[:, 127:128], [[128, NCH]])),
                                         reads=[MBB[d]], writes=[SMB])
                                    S.op("pool", lambda e, RC=RC: e.memset(RC[:, 0:1], 0.0), writes=[SMB])
                                    S.op("dve", lambda e, RN=RN, RC=RC: e.tensor_copy(RC[:, 1:NCH], RN[:, 0:NCH - 1]),
                                         reads=[SMB], writes=[SMB])
                                else:
                                    S.op("dve", lambda e, RN=RN, Mb=Mb: e.tensor_copy(RN, V(Mb[:, 0:1], [[128, NCH]])),
                                         reads=[MBB[d]], writes=[SMB])
                                    S.op("pool", lambda e, RC=RC: e.memset(RC[:, 1:2], 0.0), writes=[SMB])
                                    S.op("dve", lambda e, RN=RN, RC=RC: e.tensor_copy(RC[:, 0:1], RN[:, 1:2]), reads=[SMB], writes=[SMB])
                                    S.op("dve", lambda e, RN=RN, RC=RC: e.tensor_copy(RC[:, 2:17], RN[:, 3:18]), reads=[SMB], writes=[SMB])
                                    S.op("dve", lambda e, RN=RN, RC=RC: e.tensor_copy(RC[:, 17:18], RN[:, 0:1]), reads=[SMB], writes=[SMB])
                                S.op("dve", lambda e, AL=AL, RC=RC, RN=RN: e.tensor_tensor(out=AL, in0=RC, in1=RN, op=ALU.subtract),
                                     reads=[SMB], writes=[SMB])
                                S.op("dve", lambda e, WS=WS, ucj=ucj, RN=RN: e.tensor_tensor(out=WS, in0=ucj, in1=RN, op=ALU.subtract),
                                     reads=[SMB, UCB], writes=[SMB])
                                S.op("act", lambda e, AL=AL: e.activation(out=AL, in_=AL, func=AF.Exp), reads=[SMB], writes=[SMB])
                                S.op("act", lambda e, WS=WS: e.activation(out=WS, in_=WS, func=AF.Exp), reads=[SMB], writes=[SMB])
                                S.op("act", lambda e, EE=EE, ngj=ngj: e.activation(out=EE, in_=ngj, func=AF.Exp), reads=[UCB], writes=[SMB])

                        if h > 0:
                            emit_mb_prep()
                        for (t0, nb) in gblocks:
                            for qi in range(2):
                                pt, PB = ps_next()

                                def mm(e, pt=pt, t0=t0, nb=nb, qi=qi, whh=whh):
                                    last = None
                                    for k in range(8):
                                        last = e.matmul(pt[:, 0:nb], whh[:, k, qi * 128:(qi + 1) * 128], hxT[:, k, t0:t0 + nb],
                                                        start=(k == 0), stop=(k == 7))
                                    return last
                                S.op("pe", mm, reads=[WHH] + HXB, writes=[PB])
                                if qi == 0:
                                    S.op("dve", lambda e, pt=pt, t0=t0, nb=nb, h=h: e.tensor_scalar(
                                        out=QT[:, t0:t0 + nb], in0=pt[:, 0:nb], scalar1=vcol("bq", h), scalar2=qscale,
                                        op0=ALU.add, op1=ALU.mult), reads=[PB, VECS], writes=[QTB])
                                else:
                                    S.op("act", lambda e, pt=pt, t0=t0, nb=nb, h=h: e.activation(
                                        out=KT[:, t0:t0 + nb], in_=pt[:, 0:nb], func=AF.Identity, bias=vcol("bk", h),
                                        scale=1.0), reads=[PB, VECS], writes=[KTB])
                                if h == 0:
                                    S.replay(gate_rec, 3)
                        while pending_readout:
                            readout(pending_readout.pop(0))
                        thunks = []
                        for d in range(2):
                            j = d * 4 + h
                            Mb3, zz, zz3 = Mb3D[d], zzD[d], zz3D[d]
                            RN, RC, AL, WS, EE, T18 = [sm18[d][:, i, :] for i in range(6)]
                            ucj = ucng[:, :, j]
                            thunks.append(lambda d=d, zz3=zz3, Mb3=Mb3, RC=RC: S.op("dve", lambda e: e.tensor_tensor(
                                out=zz3, in0=Mb3, in1=V(RC[:, 0:1], [[1, NCH], [0, 128]]), op=ALU.subtract),
                                reads=[MBB[d], SM18[d]], writes=[ZB[d]]))
                            thunks.append(lambda d=d, zz=zz: S.op("act", lambda e: e.activation(
                                out=zz, in_=zz, func=AF.Exp, scale=-1.0), reads=[ZB[d]], writes=[ZB[d]]))
                            thunks.append(lambda d=d, zz=zz: S.op("dve", lambda e: e.tensor_tensor(
                                out=QW[d][:, :], in0=QT[:, :], in1=zz, op=ALU.mult), reads=[QTB, ZB[d]], writes=[QWB[d]]))
                            thunks.append(lambda d=d, zz3=zz3, Mb3=Mb3, ucj=ucj: S.op("dve", lambda e: e.tensor_tensor(
                                out=zz3, in0=Mb3, in1=V(ucj[:, 0:1], [[16, NCH], [0, 128]]), op=ALU.subtract),
                                reads=[MBB[d], UCB, ZB[d]], writes=[ZB[d]]))
                            thunks.append(lambda d=d, zz3=zz3: S.op("dve", lambda e: e.tensor_tensor(
                                out=zz3, in0=zz3, in1=V(c32[:, mcols[d]:mcols[d] + 1], [[0, NCH], [1, 128]]), op=ALU.add),
                                reads=[ZB[d], C32B], writes=[ZB[d]]))
                            thunks.append(lambda d=d, zz=zz: S.op("act", lambda e: e.activation(
                                out=Dj[d][:].rearrange("p c t -> p (c t)"), in_=zz, func=AF.Exp, scale=-1.0),
                                reads=[ZB[d]], writes=[DJB[d]]))
                        thunks = [t for pair in zip(thunks[0:6], thunks[6:12]) for t in pair]
                        for c in range(NCH):
                            pt, PB = ps_next()

                            def mm(e, pt=pt, c=c, whh=whh):
                                last = None
                                for k in range(8):
                                    last = e.matmul(pt[:, 0:384], hxT[:, k, c * 128:(c + 1) * 128], whh[:, k, 128:512],
                                                    start=(k == 0), stop=(k == 7))
                                return last
                            S.op("pe", mm, reads=[WHH] + HXB, writes=[PB])
                            S.op("dve", lambda e, pt=pt, c=c: e.tensor_tensor(
                                out=KV[:, c, 0:384], in0=pt[:, 0:384], in1=rowb[:, 0:384], op=ALU.add),
                                reads=[PB, ROWB], writes=[KVB])
                            if h == 0:
                                S.replay(gate_rec, 3)
                            elif ro_thunks:
                                ro_thunks.pop(0)()
                            elif thunks:
                                thunks.pop(0)()
                        while ro_thunks:
                            ro_thunks.pop(0)()
                        while pending_tr:
                            readout_tr(pending_tr.pop(0))
                        for c in range(2, NCH):
                            pt2, PB2 = ps_next()

                            def mm2(e, pt2=pt2, c=c, whh=whh):
                                last = None
                                for k in range(8):
                                    last = e.matmul(pt2[:, 0:256], hxT[:, k, c * 128:(c + 1) * 128], whh[:, k, 512:768],
                                                    start=(k == 0), stop=(k == 7))
                                return last
                            S.op("pe", mm2, reads=[WHH] + HXB, writes=[PB2])
                            ot, OB_ = otmp[c % 2], OTB[c % 2]
                            S.op("dve", lambda e, pt2=pt2, ot=ot: e.tensor_tensor(
                                out=ot[:, :], in0=pt2[:, 0:256], in1=rowb[:, 384:640], op=ALU.add),
                                reads=[PB2, ROWB], writes=[OB_])
                            S.op("act", lambda e, ot=ot, c=c: e.activation(out=Osig[:, c - 2, :], in_=ot[:, :],
                                                                           func=AF.Sigmoid), reads=[OB_], writes=[OSB])
                            if h == 0:
                                S.replay(gate_rec, 3)
                            elif thunks:
                                thunks.pop(0)()
                        if h == 0:
                            S.replay(gate_rec, 10 ** 6)
                            emit_mb_prep()
                        while thunks:
                            thunks.pop(0)()
                        for d in range(2):
                            WS = sm18[d][:, 3, :]
                            S.op("dve", lambda e, d=d, WS=WS: e.tensor_tensor(
                                out=KS[d][:], in0=KV[:, :, 0:128], in1=V(WS[:, 0:1], [[1, NCH], [0, 128]]), op=ALU.mult),
                                reads=[KVB, SM18[d]], writes=[KSB[d]])
                        if h + 1 < H:
                            S.dma("pool", wh[0][:], whp_d[:, h + 1, :, :], writes=[WHB[0]])
                        ada_more(1)
                        if h == 0:
                            stop("stop2p")
                        touched = set()
                        cur = [0, 0]
                        for idx in range(NCH):
                            if idx in (5, 9, 13):
                                ada_more(1)
                            work = []
                            for d in range(2):
                                c = orders[d][idx]
                                AL, EE = sm18[d][:, 2, :], sm18[d][:, 4, :]
                                it = {"d": d, "c": c, "AL": AL, "EE": EE}
                                if c >= 2:
                                    pts, PSB_ = ps_next()
                                    S.op("pe", lambda e, pts=pts, c=c: e.matmul(
                                        pts[:, 0:128], KT[:, c * 128:(c + 1) * 128], QT[:, c * 128:(c + 1) * 128],
                                        start=True, stop=True), reads=[KTB, QTB], writes=[PSB_])
                                    it["pts"], it["PSB"] = pts, PSB_
                                if idx < NCH - 1:
                                    ptu, PUB = ps_next()
                                    S.op("pe", lambda e, ptu=ptu, c=c, d=d: e.matmul(
                                        ptu[:, 0:257], KS[d][:, c, :], KV[:, c, 128:385], start=True, stop=True),
                                        reads=[KSB[d], KVB], writes=[PUB])
                                    it["ptu"], it["PUB"] = ptu, PUB
                                work.append(it)
                            for it in work:
                                d, c = it["d"], it["c"]
                                if c >= 2:
                                    pts, PSB_ = it["pts"], it["PSB"]
                                    si = spctr[0] % 4
                                    spctr[0] += 1
                                    sp_, SB_ = Sp[si], SPB[si]
                                    S.op("dve", lambda e, pts=pts, c=c, sp_=sp_, d=d: e.tensor_tensor(
                                        out=sp_[:, :], in0=pts[:, 0:128], in1=Dj[d][:, c, :], op=ALU.mult),
                                        reads=[PSB_, DJB[d]], writes=[SB_])
                                    pto, POB = ps_next()
                                    cb_, CB_ = Cbf[d][cur[d]], CBFB2[d][cur[d]]

                                    def mmo(e, pto=pto, c=c, sp_=sp_, d=d, cb_=cb_):
                                        e.matmul(pto[:, 0:257], sp_[:, :], KV[:, c, 128:385], start=True, stop=False)
                                        return e.matmul(pto[:, 0:257], QW[d][:, c * 128:(c + 1) * 128], cb_[:, :],
                                                        start=False, stop=True)
                                    S.op("pe", mmo, reads=[SB_, KVB, QWB[d], CB_], writes=[POB])
                                    it["pto"], it["POB"] = pto, POB
                            for it in work:
                                d, c = it["d"], it["c"]
                                if idx < NCH - 1:
                                    ptu, PUB = it["ptu"], it["PUB"]
                                    co, cn = cur[d], 1 - cur[d]
                                    if idx == 0:
                                        S.op("dve", lambda e, ptu=ptu, d=d, cn=cn: e.tensor_copy(Cst[d][cn][:, :], ptu[:, 0:257]),
                                             reads=[PUB], writes=[CSTB[d][cn]])
                                    else:
                                        S.op("dve", lambda e, ptu=ptu, d=d, co=co, cn=cn, c=c, AL=it["AL"]: e.scalar_tensor_tensor(
                                            out=Cst[d][cn][:, :], in0=Cst[d][co][:, :], scalar=AL[:, c:c + 1], in1=ptu[:, 0:257],
                                            op0=ALU.mult, op1=ALU.add), reads=[PUB, CSTB[d][co], SM18[d]], writes=[CSTB[d][cn]])
                                    S.op("act", lambda e, d=d, cn=cn: e.copy(Cbf[d][cn][:, :], Cst[d][cn][:, :]),
                                         reads=[CSTB[d][cn]], writes=[CBFB2[d][cn]])
                                    cur[d] = cn
                                if c >= 2:
                                    pto, POB = it["pto"], it["POB"]
                                    di = dctr[0] % 4
                                    dctr[0] += 1
                                    dd_, DB_ = dsm[:, di, :], DSMB[di]
                                    EE = it["EE"]
                                    S.op("act", lambda e, pto=pto, dd_=dd_: e.activation(out=dd_[:, 0:1], in_=pto[:, 256:257],
                                                                                         func=AF.Abs), reads=[POB], writes=[DB_])
                                    S.op("dve", lambda e, dd_=dd_, c=c, EE=EE: e.tensor_tensor(
                                        out=dd_[:, 1:2], in0=dd_[:, 0:1], in1=EE[:, c:c + 1], op=ALU.max),
                                        reads=[DB_, SM18[d]], writes=[DB_])
                                    S.op("dve", lambda e, dd_=dd_: e.reciprocal(out=dd_[:, 2:3], in_=dd_[:, 1:2]),
                                         reads=[DB_], writes=[DB_])
                                    if c not in touched:
                                        touched.add(c)
                                        S.op("dve", lambda e, pto=pto, dd_=dd_, c=c: e.tensor_scalar(
                                            out=Hh3[:, c - 2, :], in0=pto[:, 0:256], scalar1=dd_[:, 2:3], scalar2=None,
                                            op0=ALU.mult), reads=[POB, DB_], writes=[HHB])
                                    else:
                                        S.op("dve", lambda e, pto=pto, dd_=dd_, c=c: e.scalar_tensor_tensor(
                                            out=Hh3[:, c - 2, :], in0=pto[:, 0:256], scalar=dd_[:, 2:3], in1=Hh3[:, c - 2, :],
                                            op0=ALU.mult, op1=ALU.add), reads=[POB, DB_, HHB], writes=[HHB])
                        if h == 0:
                            stop("stop2o")
                        pending_readout.append(h)
                        if h == 0:
                            stop("stop2h")
                    while pending_readout:
                        readout(pending_readout.pop(0))
                    while ro_thunks:
                        ro_thunks.pop(0)()
                    while pending_tr:
                        readout_tr(pending_tr.pop(0))
                    stop("stop2z")
                    S.barrier()

            def mk_pnr(ph_, tag, shared=None, nbuf=1):
                sets = []
                for i in range(nbuf):
                    yo_ = sb("yo%s%d" % (tag, i), [128, 8, 512], F32, ph_)
                    YOBS_ = [Buf("yo%s%d_%d" % (tag, i, j)) for j in range(8)]
                    if shared is None:
                        sq_ = sb("sqo%s%d" % (tag, i), [128, 8, 512], BF16, ph_); SQB_ = Buf("sqo%s%d" % (tag, i))
                        sd_ = sb("sdo%s%d" % (tag, i), [128, 512], F32, ph_); SDB_ = Buf("sdo%s%d" % (tag, i))
                        rstd_ = sb("rso%s%d" % (tag, i), [128, 512], F32, ph_); RSB_ = Buf("rso%s%d" % (tag, i))
                    else:
                        sq_, SQB_, sd_, SDB_, rstd_, RSB_ = shared
                    sets.append((yo_, YOBS_, sq_, SQB_, sd_, SDB_, rstd_, RSB_))

                def part1(blk, wsb, WBs, rhs_fn, rhs_bufs_fn, nk, bias_name=None, split_tail=0):
                    yo, YOBS, sq, SQB, sd, SDB, rstd, RSB = sets[blk % nbuf]
                    for dc in range(8):
                        pt, PB = ps_next()
                        segs = [(0, nk)]
                        if dc == 0 and split_tail:
                            segs = [(0, nk - split_tail), (nk - split_tail, nk)]
                        for (k0, k1) in segs:
                            def mm(e, pt=pt, dc=dc, k0=k0, k1=k1):
                                last = None
                                for k in range(k0, k1):
                                    last = e.matmul(pt[:, :], wsb[:, k, dc * 128:(dc + 1) * 128], rhs_fn(k),
                                                    start=(k == 0), stop=(k == nk - 1))
                                return last
                            S.op("pe", mm, reads=WBs + rhs_bufs_fn(k0, k1), writes=[PB])
                        YOB = YOBS[dc]
                        if bias_name is None:
                            S.op("dve", lambda e, pt=pt, dc=dc: e.tensor_copy(yo[:, dc, :], pt[:, :]), reads=[PB], writes=[YOB])
                            S.op("act", lambda e, pt=pt, dc=dc: e.activation(out=sq[:, dc, :], in_=pt[:, :], func=AF.Square),
                                 reads=[PB], writes=[SQB])
                        else:
                            S.op("dve", lambda e, pt=pt, dc=dc: e.tensor_scalar(
                                out=yo[:, dc, :], in0=pt[:, :], scalar1=vcol(bias_name, dc), scalar2=None, op0=ALU.add),
                                reads=[PB, VECS], writes=[YOB])
                            S.op("act", lambda e, pt=pt, dc=dc: e.activation(out=sq[:, dc, :], in_=pt[:, :], func=AF.Square,
                                                                             bias=vcol(bias_name, dc), scale=1.0),
                                 reads=[PB, VECS], writes=[SQB])

                def part2(blk, gate_idx, layer, xr, XRB):
                    yo, YOBS, sq, SQB, sd, SDB, rstd, RSB = sets[blk % nbuf]
                    rstd_from_sq(sq, SQB, 512, sd, SDB, rstd, RSB)
                    for dc in range(8):
                        YOB = YOBS[dc]
                        S.op("dve", lambda e, dc=dc: e.scalar_tensor_tensor(
                            out=yo[:, dc, :], in0=yo[:, dc, :], scalar=der[:, layer, gate_idx, dc:dc + 1], in1=rstd[:, :],
                            op0=ALU.mult, op1=ALU.mult), reads=[YOB, DERB, RSB], writes=[YOB])
                        S.op("dve", lambda e, dc=dc: e.tensor_tensor(
                            out=XT[:, dc, blk * 512:(blk + 1) * 512], in0=yo[:, dc, :], in1=xr(dc), op=ALU.add),
                            reads=[YOB] + XRB, writes=[XTB[blk]])

                def run(blk, wsb, WBs, rhs_fn, rhs_bufs, nk, gate_idx, layer, xr, XRB, bias_name=None):
                    part1(blk, wsb, WBs, rhs_fn, lambda k0, k1: rhs_bufs, nk, bias_name)
                    part2(blk, gate_idx, layer, xr, XRB)
                run.part1 = part1
                run.part2 = part2
                return run

            with ExitStack() as ph:
                wmo = sb("wmo", [128, 8, D], BF16, ph); WMOB = Buf("wmo")
                S.dma("pool", wmo[:], wmo_d[:, :, :], writes=[WMOB])
                for k in range(8):
                    S.op("dve", lambda e, k=k: e.tensor_scalar(out=wmo[:, k, :], in0=wmo[:, k, :], scalar1=vcol("ngc", k),
                                                                scalar2=None, op0=ALU.mult), reads=[WMOB, VECS], writes=[WMOB])
                ytl = [sb("ytl%d" % i, [128, 8, 512], BF16, ph) for i in range(2)]
                YTLB = [Buf("ytl%d" % i) for i in range(2)]
                xrs = [sb("xrs%d" % i, [128, 8, 512], F32, ph) for i in range(2)]
                XRSB = [Buf("xrs%d" % i) for i in range(2)]

                ada3 = [sb("ada3_%d" % i, [128, 8, 256], BF16, ph) for i in range(2)]
                ADAB3 = [Buf("ada3_%d" % i) for i in range(2)]
                modrow3 = [sb("modrow3_%d" % i, [2, 256], F32, ph) for i in range(2)]
                MRB3 = [Buf("modrow3_%d" % i) for i in range(2)]
                ada_state["bufs"] = (ada3, ADAB3, modrow3, MRB3)
                ada_state["limit"] = len(ada_items)
                ada_dma(24)
                ada_dma(25)
                ada_more(4)
                pnr = mk_pnr(ph, "3", nbuf=2)
                for blk in range(4):
                    yb, YB_ = ytl[blk % 2], YTLB[blk % 2]
                    xb_, XB_ = xrs[blk % 2], XRSB[blk % 2]
                    S.dma("sp", yb[:], yt_d[:, :, blk * 512:(blk + 1) * 512], reads=[YTD], writes=[YB_])
                    pnr.part1(blk, wmo, [WMOB], lambda k, yb=yb: yb[:, k, :], lambda k0, k1, YB_=YB_: [YB_], 8)
                    if blk > 0:
                        pb = blk - 1
                        pnr.part2(pb, 1, 0, lambda dc, xq=xrs[pb % 2]: xq[:, dc, :], [XRSB[pb % 2]])
                    S.dma("sp", xb_[:], xc_v[:, :, TC + blk * 512:TC + (blk + 1) * 512], writes=[XB_])
                    ada_more(5)
                pnr.part2(3, 1, 0, lambda dc, xq=xrs[1]: xq[:, dc, :], [XRSB[1]])
                S.barrier()
            dump("x1", XT[:], [128, 8, T], F32, XTB)
            stop("stop3")


            def pre_norm_sq(blk, sq, SQB):
                S.op("act", lambda e: e.activation(out=sq[:, :, :], in_=XT[:, :, blk * 512:(blk + 1) * 512],
                                                   func=AF.Square), reads=[XTB[blk]], writes=[SQB])

            def pre_norm_rest(blk, layer, a_idx, b_off, sq, SQB, sd, SDB, rstd, RSB, tmp, TMPB, dst_fn, DSTB_fn):
                rstd_from_sq(sq, SQB, 512, sd, SDB, rstd, RSB)
                for k in range(8):
                    tm, TB = tmp[k % 2], TMPB[k % 2]
                    S.op("dve", lambda e, k=k, tm=tm: e.scalar_tensor_tensor(
                        out=tm[:, :], in0=XT[:, k, blk * 512:(blk + 1) * 512], scalar=der[:, layer, a_idx, k:k + 1],
                        in1=rstd[:, :], op0=ALU.mult, op1=ALU.mult), reads=[XTB[blk], RSB, DERB], writes=[TB])
                    if k % 2 == 0:
                        S.op("act", lambda e, k=k, tm=tm: e.activation(
                            out=dst_fn(k), in_=tm[:, :], func=AF.Identity, bias=mod[:, layer, b_off + k:b_off + k + 1],
                            scale=1.0), reads=[TB, MODB], writes=DSTB_fn(k))
                    else:
                        S.op("dve", lambda e, k=k, tm=tm: e.tensor_scalar(
                            out=dst_fn(k), in0=tm[:, :], scalar1=mod[:, layer, b_off + k:b_off + k + 1], scalar2=None,
                            op0=ALU.add), reads=[TB, MODB], writes=DSTB_fn(k))

            def pre_norm_block(blk, layer, a_idx, b_off, sq, SQB, sd, SDB, rstd, RSB, tmp, TMPB, dst_fn, DSTB_fn):
                pre_norm_sq(blk, sq, SQB)
                pre_norm_rest(blk, layer, a_idx, b_off, sq, SQB, sd, SDB, rstd, RSB, tmp, TMPB, dst_fn, DSTB_fn)

            OUTB = Buf("outd")

            def ffn(l):
                with ExitStack() as ph:
                    wdn = sb("wdn", [128, NF, D], BF16, ph)
                    WDNB = [Buf("wdn%d_%d" % (l, i)) for i in range(2)]
                    h2T = [sb("h2T%d" % i, [128, 8, 512], BF16, ph) for i in range(2)]
                    H2B = [Buf("h2T%d_%d" % (l, i)) for i in range(2)]
                    aT = sb("aT", [128, NF, 512], BF16, ph); ATBS = [Buf("aT%d_%d" % (l, i)) for i in range(NF)]
                    wup = [sb("wup%d" % i, [128, 8, 256], BF16, ph) for i in range(4)]
                    WUPB = [Buf("wup%d_%d" % (l, i)) for i in range(4)]
                    sq = sb("sqf", [128, 8, 512], BF16, ph); SQB = Buf("sqf%d" % l)
                    sd = sb("sdf", [128, 512], F32, ph); SDB = Buf("sdf%d" % l)
                    rstd = sb("rsf", [128, 512], F32, ph); RSB = Buf("rsf%d" % l)
                    gc = [sb("gc%d" % i, [128, 512], F32, ph) for i in range(2)]
                    GCB = [Buf("gc%d_%d" % (l, i)) for i in range(2)]
                    tmp, TMPB = gc, GCB
                    sg = [sb("sg%d" % i, [128, 512], F32, ph) for i in range(2)]
                    SGB = [Buf("sg%d_%d" % (l, i)) for i in range(2)]
                    pnr = mk_pnr(ph, "f", shared=(sq, SQB, sd, SDB, rstd, RSB))
                    wctr = 0
                    for blk in range(4):
                        hb, HB_ = h2T[blk % 2], H2B[blk % 2]
                        if blk == 0:
                            pre_norm_block(0, l, 2, 24, sq, SQB, sd, SDB, rstd, RSB, tmp, TMPB,
                                           lambda k, hb=hb: hb[:, k, :], lambda k, HB_=HB_: [HB_])
                        for f in range(NF):
                            if f == 2 and blk > 0:
                                pnr.part2(blk - 1, 3, l, lambda dc, b_=blk - 1: XT[:, dc, b_ * 512:(b_ + 1) * 512], [XTB[blk - 1]])
                                if l == 1:
                                    b_ = blk - 1
                                    S.dma("sp", out_v[:, :, b_ * 512:(b_ + 1) * 512], XT[:, :, b_ * 512:(b_ + 1) * 512],
                                          reads=[XTB[b_]], writes=[OUTB])
                            if f == 9 and blk < 3:
                                pre_norm_sq(blk + 1, sq, SQB)
                            if f == 12 and blk < 3:
                                hn, HN_ = h2T[(blk + 1) % 2], H2B[(blk + 1) % 2]
                                pre_norm_rest(blk + 1, l, 2, 24, sq, SQB, sd, SDB, rstd, RSB, tmp, TMPB,
                                              lambda k, hn=hn: hn[:, k, :], lambda k, HN_=HN_: [HN_])
                            wb, WB_ = wup[wctr % 4], WUPB[wctr % 4]
                            wctr += 1
                            S.dma("pool", wb[:], upp_d[l, :, f, :, :], writes=[WB_])
                            if blk == 0 and f in (12, 17):
                                hf = 0 if f == 12 else 1
                                S.dma("pool", wdn[:, hf * 11:(hf + 1) * 11, :], dnp_d[l, :, hf * 11:(hf + 1) * 11, :],
                                      writes=[WDNB[hf]])
                            if True:
                                ptu, PUB = ps_next()
                                ptg, PGB = ps_next()

                                def mmu(e, ptu=ptu, wb=wb, hb=hb):
                                    last = None
                                    for k in range(8):
                                        last = e.matmul(ptu[:, :], wb[:, k, 0:128], hb[:, k, :], start=(k == 0), stop=(k == 7))
                                    return last

                                def mmg(e, ptg=ptg, wb=wb, hb=hb):
                                    last = None
                                    for k in range(8):
                                        last = e.matmul(ptg[:, :], wb[:, k, 128:256], hb[:, k, :], start=(k == 0), stop=(k == 7))
                                    return last
                                S.op("pe", mmg, reads=[WB_, HB_], writes=[PGB])
                                S.op("pe", mmu, reads=[WB_, HB_], writes=[PUB])
                                g_, GB_ = gc[f % 2], GCB[f % 2]
                                s_, SB_ = sg[f % 2], SGB[f % 2]
                                w0 = vcol("convw", (l * 3 + 0) * 22 + f)
                                w1 = vcol("convw", (l * 3 + 1) * 22 + f)
                                w2 = vcol("convw", (l * 3 + 2) * 22 + f)
                                cb_ = vcol("convb", l * 22 + f)
                                S.op("act", lambda e, ptg=ptg, g_=g_, w1=w1, cb_=cb_: e.activation(
                                    out=g_[:, :], in_=ptg[:, :], func=AF.Identity, bias=cb_, scale=w1),
                                    reads=[PGB, VECS], writes=[GB_])
                                g3 = g_[:, :].rearrange("p (r c) -> p r c", c=64)
                                p3 = ptg[:, :].rearrange("p (r c) -> p r c", c=64)
                                S.op("dve", lambda e, g3=g3, p3=p3, w0=w0: e.scalar_tensor_tensor(
                                    out=g3[:, :, 1:64], in0=p3[:, :, 0:63], scalar=w0, in1=g3[:, :, 1:64],
                                    op0=ALU.mult, op1=ALU.add), reads=[PGB, GB_, VECS], writes=[GB_])
                                S.op("dve", lambda e, g3=g3, p3=p3, w2=w2: e.scalar_tensor_tensor(
                                    out=g3[:, :, 0:63], in0=p3[:, :, 1:64], scalar=w2, in1=g3[:, :, 0:63],
                                    op0=ALU.mult, op1=ALU.add), reads=[PGB, GB_, VECS], writes=[GB_])
                                S.op("act", lambda e, g_=g_, s_=s_: e.activation(out=s_[:, :], in_=g_[:, :], func=AF.Silu),
                                     reads=[GB_], writes=[SB_])
                                S.op("dve", lambda e, s_=s_, ptu=ptu, f=f: e.tensor_tensor(
                                    out=aT[:, f, :], in0=s_[:, :], in1=ptu[:, :], op=ALU.mult),
                                    reads=[SB_, PUB], writes=[ATBS[f]])
                        pnr.part1(blk, wdn, WDNB, lambda k: aT[:, k, :], lambda k0, k1: ATBS[k0:k1], NF, split_tail=3)
                    pnr.part2(3, 3, l, lambda dc: XT[:, dc, 3 * 512:4 * 512], [XTB[3]])
                    S.barrier()

            ffn(0)
            dump("x2", XT[:], [128, 8, T], F32, XTB)
            stop("stop4")

            with ExitStack() as ph:
                hT = sb("hT", [128, 8, T], BF16, ph)
                HTB = [Buf("hT%d" % g) for g in range(8)]
                wfo = sb("wfo", [128, 8, D], BF16, ph); WFOB = Buf("wfo")
                S.dma("pool", wfo[:], wfo_d[:, :, :], writes=[WFOB])
                with ExitStack() as p5:
                    dft = sb("dft", [128, 2, 2, 8, 1024], BF16, p5)
                    DFTB = [[Buf("dft%d%d" % (a_, b_)) for b_ in range(2)] for a_ in range(2)]
                    for a_ in range(2):
                        for b_ in range(2):
                            S.dma("sp", dft[:, a_, b_, :, :], dft2_d[:, a_, b_, :, :], writes=[DFTB[a_][b_]])
                    PQ = [sb("PQ%d" % i, [128, 2, 8, 256], BF16, p5) for i in range(2)]
                    PQB = [Buf("PQ%d" % i) for i in range(2)]
                    Esb = [sb("Esb%d" % i, [128, 512], F32, p5) for i in range(2)]
                    ESB = [Buf("Esb%d" % i) for i in range(2)]
                    sd = sb("sd5", [128, 512], F32, p5); SDB = Buf("sd5")
                    rstd4 = PQ[0][:].rearrange("p a i c -> p (a i c)").bitcast(F32).rearrange("p (b t) -> p b t", t=512)
                    RSB4 = [PQB[0]] * 4
                    for blk in range(4):
                        k0 = 4 + 2 * (blk % 2)
                        sq = hT[:, k0:k0 + 2, :].rearrange("p a t -> p (a t)").rearrange("p (k t) -> p k t", t=512)
                        SQW = [HTB[k0], HTB[k0 + 1]]
                        S.op("act", lambda e, blk=blk, sq=sq: e.activation(out=sq, in_=XT[:, :, blk * 512:(blk + 1) * 512],
                                                                          func=AF.Square), reads=[XTB[blk]], writes=SQW)
                        pt, PB = ps_next()

                        def mmss(e, pt=pt, sq=sq):
                            last = None
                            for k in range(8):
                                last = e.matmul(pt[:, :], ones, sq[:, k, :], start=(k == 0), stop=(k == 7))
                            return last
                        S.op("pe", mmss, reads=SQW + [CBFB], writes=[PB])
                        S.op("act", lambda e, pt=pt: e.activation(out=sd[:, :], in_=pt[:, :], func=AF.Ln, bias=EPS,
                                                                  scale=1.0 / D), reads=[PB], writes=[SDB])
                        S.op("act", lambda e, blk=blk: e.activation(out=rstd4[:, blk, :], in_=sd[:, :], func=AF.Exp,
                                                                   scale=-0.5), reads=[SDB], writes=[RSB4[blk]])
                    tctr5 = [0]
                    pn_thunks = []

                    def pn_pair(k, blk):
                        tm, TB = Esb[tctr5[0] % 2], ESB[tctr5[0] % 2]
                        tctr5[0] += 1
                        S.op("dve", lambda e: e.scalar_tensor_tensor(
                            out=tm[:, :], in0=XT[:, k, blk * 512:(blk + 1) * 512], scalar=der[:, 1, 0, k:k + 1],
                            in1=rstd4[:, blk, :], op0=ALU.mult, op1=ALU.mult),
                            reads=[XTB[blk], RSB4[blk], DERB], writes=[TB])
                        if tctr5[0] % 2 == 0:
                            S.op("act", lambda e: e.activation(
                                out=hT[:, k, blk * 512:(blk + 1) * 512], in_=tm[:, :], func=AF.Identity,
                                bias=mod[:, 1, k:k + 1], scale=1.0), reads=[TB, MODB], writes=[HTB[k]])
                        else:
                            S.op("dve", lambda e: e.tensor_scalar(
                                out=hT[:, k, blk * 512:(blk + 1) * 512], in0=tm[:, :], scalar1=mod[:, 1, k:k + 1],
                                scalar2=None, op0=ALU.add), reads=[TB, MODB], writes=[HTB[k]])
                    for blk in range(4):
                        pn_pair(0, blk)
                    for k in range(1, 8):
                        for blk in range(4):
                            pn_thunks.append(lambda k=k, blk=blk: pn_pair(k, blk))
                    ectr = 0
                    for g in range(8):
                        pq, PQB_ = PQ[1], PQB[1]
                        for par in range(2):
                            for ip in range(4):
                                pt, PB = ps_next()

                                def mm0(e, pt=pt, g=g, par=par, ip=ip):
                                    last = None
                                    for q in range(2):
                                        i = 2 * ip + q
                                        t0 = 2 * i * 128 + par
                                        last = e.matmul(pt[:, q * 256:(q + 1) * 256], V(hT[:, g, t0:t0 + 1], [[2, 128]]),
                                                        dftd, start=True, stop=True)
                                    return last
                                S.op("pe", mm0, reads=[HTB[g], CBFB], writes=[PB])
                                if pn_thunks and ip % 2 == 1:
                                    pn_thunks.pop(0)()
                                dst = pq[:, par, 2 * ip:2 * ip + 2, :].rearrange("p a b -> p (a b)")
                                if ip % 2 == 0:
                                    S.op("act", lambda e, pt=pt, dst=dst: e.copy(dst, pt[:, :]), reads=[PB], writes=[PQB_])
                                else:
                                    S.op("dve", lambda e, pt=pt, dst=dst: e.tensor_copy(dst, pt[:, :]), reads=[PB], writes=[PQB_])
                        for kb in range(2):
                            pte, PEB = ps_next()
                            pto, POB = ps_next()
                            for par, ptx, PXB in ((0, pte, PEB), (1, pto, POB)):
                                def mm1(e, ptx=ptx, par=par, kb=kb, pq=pq):
                                    last = None
                                    for i in range(8):
                                        e.matmul(ptx[:, :], pq[:, par, i, 0:128], dft[:, 0, par, i, kb * 512:(kb + 1) * 512],
                                                 start=(i == 0), stop=False)
                                        last = e.matmul(ptx[:, :], pq[:, par, i, 128:256],
                                                        dft[:, 1, par, i, kb * 512:(kb + 1) * 512], start=False, stop=(i == 7))
                                    return last
                                S.op("pe", mm1, reads=[PQB_, DFTB[0][par], DFTB[1][par]], writes=[PXB])
                            es, ESB_ = Esb[ectr % 2], ESB[ectr % 2]
                            ectr += 1
                            S.op("act", lambda e, es=es, pte=pte: e.copy(es[:, :], pte[:, :]), reads=[PEB], writes=[ESB_])
                            S.op("dve", lambda e, es=es, pto=pto, g=g, kb=kb: e.tensor_tensor(
                                out=hT[:, g, kb * 512:(kb + 1) * 512], in0=es[:, :], in1=pto[:, :], op=ALU.add),
                                reads=[ESB_, POB], writes=[HTB[g]])
                            S.op("dve", lambda e, es=es, pto=pto, g=g, kb=kb: e.tensor_tensor(
                                out=hT[:, g, 1024 + kb * 512:1024 + (kb + 1) * 512], in0=es[:, :], in1=pto[:, :],
                                op=ALU.subtract), reads=[ESB_, POB], writes=[HTB[g]])
                    S.barrier()
                dump("yfftT", hT[:], [128, 8, T], BF16, HTB)
                with ExitStack() as p6:
                    pnr = mk_pnr(p6, "6", nbuf=2)
                    for blk in range(4):
                        pnr.part1(blk, wfo, [WFOB], lambda k, blk=blk: hT[:, k, blk * 512:(blk + 1) * 512],
                                  lambda k0, k1: HTB, 8, bias_name="fb")
                        if blk > 0:
                            pb = blk - 1
                            pnr.part2(pb, 1, 1, lambda dc, pb=pb: XT[:, dc, pb * 512:(pb + 1) * 512], [XTB[pb]])
                    pnr.part2(3, 1, 1, lambda dc: XT[:, dc, 3 * 512:4 * 512], [XTB[3]])
                    S.barrier()
            dump("x3", XT[:], [128, 8, T], F32, XTB)
            stop("stop6")

            ffn(1)
            for blk in range(3, 4):
                S.dma("sp", out_v[:, :, blk * 512:(blk + 1) * 512], XT[:, :, blk * 512:(blk + 1) * 512],
                      reads=[XTB[blk]], writes=[OUTB])
            dbg_out["__out"] = OUTB
        except _Stop:
            pass
        S.stopped = False
        S.finish(list(dbg_out.values()))
    return nc, dbg_out


_CACHE = {}


def kernel(**inputs):
    sh = host_shared(inputs)
    in_maps = []
    for b in range(8):
        m = dict(sh)
        m.update(host_percore(inputs, b))
        in_maps.append(m)
    if "nc" not in _CACHE:
        _CACHE["nc"] = build()[0]
    res = run_bass_kernel_spmd(_CACHE["nc"], in_maps, core_ids=list(range(8)))
    out = np.stack([np.ascontiguousarray(res.results[b]["out"].T) for b in range(8)])
    return out.astype(np.float32)
```

```python
from contextlib import ExitStack
import numpy as np
import ml_dtypes
import concourse.bass as bass
import concourse.mybir as mybir
from concourse.bass_utils import run_bass_kernel_spmd

F32 = mybir.dt.float32
BF16 = mybir.dt.bfloat16
AF = mybir.ActivationFunctionType
ALU = mybir.AluOpType
AX = mybir.AxisListType

D = 1024
T = 2048
TC = 256
TT = T + TC
NCH = TT // 128
H = 4
DK = 128
DV = 256
FF = 2816
NF = FF // 128
EPS = 1e-6
BIG = 30000.0


class Buf:
    __slots__ = ("name", "w", "r", "dsem", "dcount", "excl")

    def __init__(self, name, excl=False):
        self.name = name
        self.excl = excl
        self.w = None
        self.r = {}
        self.dsem = None
        self.dcount = 0


class Sched:
    def __init__(self, nc, stack):
        self.nc = nc
        self.stack = stack
        self.eng = {}
        self.sems = {}
        for name, h in (("pe", nc.tensor), ("act", nc.scalar), ("dve", nc.vector),
                        ("pool", nc.gpsimd), ("sp", nc.sync)):
            sem = stack.enter_context(nc.semaphore("s_" + name))
            self.eng[name] = {"h": h, "sem": sem, "count": 0, "waited": {}}
            self.sems[name] = sem
        self.dma_bufs = {}
        self.nsem = 5
        self.nops = {k: 0 for k in self.eng}
        self.stopped = False
        self.rec = None

    def _wait(self, ename, tok):
        key, val = tok
        if key in self.dma_bufs:
            val = max(val, self.dma_bufs[key].dcount)
        if key == "pe" and ename == "pe":
            return
        e = self.eng[ename]
        if e["waited"].get(key, 0) >= val:
            return
        e["waited"][key] = val
        e["h"].wait_ge(self.sems[key], val)

    def _deps(self, ename, reads, writes):
        for b in reads:
            if b.w is not None:
                self._wait(ename, b.w)
            if b.excl:
                for k, v in b.r.items():
                    if k != ename:
                        self._wait(ename, (k, v))
        for b in writes:
            if b.w is not None:
                self._wait(ename, b.w)
            for k, v in b.r.items():
                self._wait(ename, (k, v))

    def _mark(self, tok, reads, writes):
        k, v = tok
        for b in reads:
            if b.r.get(k, 0) < v:
                b.r[k] = v
        for b in writes:
            b.w = tok
            b.r = {}

    def op(self, ename, fn, reads=(), writes=()):
        if self.stopped:
            return
        if self.rec is not None:
            self.rec.append(("op", ename, fn, list(reads), list(writes), {}))
            return
        e = self.eng[ename]
        self._deps(ename, reads, writes)
        inst = fn(e["h"])
        e["count"] += 1
        self.nops[ename] += 1
        inst.then_inc(e["sem"], 1)
        self._mark((ename, e["count"]), reads, writes)

    def dma(self, qname, out_ap, in_ap, reads=(), writes=(), **kw):
        if self.stopped:
            return
        if self.rec is not None:
            self.rec.append(("dma", qname, (out_ap, in_ap), list(reads), list(writes), kw))
            return
        e = self.eng[qname]
        self._deps(qname, reads, writes)
        dst = writes[0]
        if dst.dsem is None:
            dst.dsem = "d_" + dst.name
            self.sems[dst.dsem] = self.stack.enter_context(self.nc.semaphore(dst.dsem))
            self.dma_bufs[dst.dsem] = dst
            self.nsem += 1
        dst.dcount += 16
        e["h"].dma_start(out=out_ap, in_=in_ap, **kw).then_inc(self.sems[dst.dsem], 16)
        self._mark((dst.dsem, dst.dcount), reads, writes)

    def replay(self, rec, n):
        i = 0
        last_pe = False
        while rec and (i < n or last_pe):
            kind, en, x, r, w, kw = rec.pop(0)
            i += 1
            last_pe = (kind == "op" and en == "pe")
            if kind == "barrier":
                self.barrier()
            elif kind == "op":
                self.op(en, x, r, w)
            else:
                self.dma(en, x[0], x[1], r, w, **kw)

    def barrier(self):
        if self.stopped:
            return
        if self.rec is not None:
            self.rec.append(("barrier", None, None, [], [], {}))
            return
        toks = [(n, e["count"]) for n, e in self.eng.items() if e["count"] > 0]
        toks += [(k, b.dcount) for k, b in self.dma_bufs.items()]
        for n in self.eng:
            for t in toks:
                if not (t[0] == "pe" and n == "pe"):
                    self._wait(n, t)

    def finish(self, out_bufs):
        for b in out_bufs:
            if b.w is not None:
                self._wait("sp", b.w)


class _Stop(Exception):
    pass


def V(base, dims):
    return bass.AP(base.tensor, base.offset, [base.ap[0]] + [list(d) for d in dims])


VOFF = {}
_o = 0
for _n, _w in (("adab0", 48), ("adab1", 48), ("premix", 16), ("postmix", 16), ("preffn", 16),
               ("postffn", 16), ("bq", 4), ("bk", 4), ("fb", 8), ("convw", 132), ("convb", 44),
               ("gbi", 1), ("gbf", 1), ("rsf", 1), ("rsb", 1), ("ngc", 8)):
    VOFF[_n] = _o
    _o += _w
NV = _o

C32 = {"maskf": 0, "maskb": 128, "id8": 256, "sel": 264}
NC32 = 264 + 8 * 128
CBF = {"ident": 0, "ones": 128, "dftd": 256}
NCBF = 512


def pchunk(w):
    K, N = w.shape
    return np.ascontiguousarray(w.reshape(K // 128, 128, N).transpose(1, 0, 2))


def colvec(v):
    return np.ascontiguousarray(v.reshape(-1, 128).T)


def host_shared(inp):
    f32 = np.float32
    sh = {}
    ada_w = np.asarray(inp["ada_w"], f32)
    sh["adaw"] = np.ascontiguousarray(ada_w.reshape(2, 8, 128, 6 * D).transpose(0, 2, 1, 3))
    vecs = np.zeros((128, NV), f32)
    ada_b = np.asarray(inp["ada_b"], f32)
    for l in range(2):
        vecs[:, VOFF["adab%d" % l]:VOFF["adab%d" % l] + 48] = colvec(ada_b[l])
    for nm, key in (("premix", "pre_mix_g"), ("postmix", "post_mix_g"), ("preffn", "pre_ffn_g"),
                    ("postffn", "post_ffn_g")):
        a = np.asarray(inp[key], f32)
        for l in range(2):
            vecs[:, VOFF[nm] + 8 * l:VOFF[nm] + 8 * l + 8] = colvec(a[l])
    mb = np.asarray(inp["m_in_b"], f32)[0]
    vecs[:, VOFF["bq"]:VOFF["bq"] + 4] = colvec(mb[0:512])
    vecs[:, VOFF["bk"]:VOFF["bk"] + 4] = colvec(mb[512:1024])
    vecs[:, VOFF["fb"]:VOFF["fb"] + 8] = colvec(np.asarray(inp["f_out_b"], f32)[0])
    cw = np.asarray(inp["ffn_conv_w"], f32)
    cb = np.asarray(inp["ffn_conv_b"], f32)
    for l in range(2):
        for j in range(3):
            o = VOFF["convw"] + (l * 3 + j) * 22
            vecs[:, o:o + 22] = colvec(cw[l, j])
        o = VOFF["convb"] + l * 22
        vecs[:, o:o + 22] = colvec(cb[l])
    gperm = [0, 1, 2, 3, 8, 9, 10, 11, 4, 5, 6, 7, 12, 13, 14, 15]
    gb = mb[3072:3088][gperm]
    vecs[0:8, VOFF["gbi"]] = gb[0:8]
    vecs[0:8, VOFF["gbf"]] = gb[8:16]
    vecs[0:4, VOFF["rsf"]] = 1.0
    vecs[4:8, VOFF["rsb"]] = 1.0
    vecs[:, VOFF["ngc"]:VOFF["ngc"] + 8] = colvec(np.asarray(inp["m_norm_g"], f32)[0])
    sh["vecs"] = vecs
    ng = np.asarray(inp["m_norm_g"], f32)[0]
    rv = np.zeros((H, 640), f32)
    for h in range(H):
        rv[h, 0:128] = mb[512 + h * 128:512 + (h + 1) * 128]
        rv[h, 128:384] = mb[1024 + h * 256:1024 + (h + 1) * 256]
        rv[h, 384:640] = mb[2048 + h * 256:2048 + (h + 1) * 256]
    sh["rowv"] = rv
    miw = np.asarray(inp["m_in_w"], f32)[0]
    whp = np.zeros((128, H, 8, 768), f32)
    for h in range(H):
        cols = np.concatenate([np.arange(h * 128, (h + 1) * 128), 512 + np.arange(h * 128, (h + 1) * 128),
                               1024 + np.arange(h * 256, (h + 1) * 256),
                               2048 + np.arange(h * 256, (h + 1) * 256)])
        whp[:, h] = pchunk(miw[:, cols])
    sh["whp"] = whp
    sh["wgp"] = pchunk(miw[:, 3072 + np.array(gperm)])
    sh["wmo"] = pchunk(np.asarray(inp["m_out_w"], f32)[0])
    sh["wfo"] = pchunk(np.asarray(inp["f_out_w"], f32)[0])
    up = np.asarray(inp["ffn_up_w"], f32)
    upp = np.zeros((2, 128, NF, 8, 256), f32)
    for l in range(2):
        pc = pchunk(up[l])
        upp[l, :, :, :, 0:128] = pc[:, :, 0:FF].reshape(128, 8, NF, 128).transpose(0, 2, 1, 3)
        upp[l, :, :, :, 128:256] = pc[:, :, FF:2 * FF].reshape(128, 8, NF, 128).transpose(0, 2, 1, 3)
    sh["upp"] = upp
    dn = np.asarray(inp["ffn_down_w"], f32)
    sh["dnp"] = np.stack([pchunk(dn[l]) for l in range(2)])
    c32 = np.zeros((128, NC32), f32)
    s_i = np.arange(128)[:, None]
    t_i = np.arange(128)[None, :]
    c32[:, C32["maskf"]:C32["maskf"] + 128] = np.where(s_i <= t_i, 0.0, BIG)
    c32[:, C32["maskb"]:C32["maskb"] + 128] = np.where(s_i >= t_i, 0.0, BIG)
    c32[0:8, C32["id8"]:C32["id8"] + 8] = np.eye(8)
    sel = np.zeros((8, 8, 128), f32)
    for j in range(8):
        sel[j, j, :] = 1.0
    c32[0:8, C32["sel"]:] = sel.reshape(8, 8 * 128)
    sh["c32"] = c32
    cbf = np.zeros((128, NCBF), f32)
    cbf[:, 0:128] = np.eye(128)
    cbf[:, 128:256] = 1.0
    dd = np.arange(128)
    ang = 2.0 * np.pi * np.outer(dd, dd) / 128.0
    cbf[:, 256:384] = np.cos(ang) / 512.0
    cbf[:, 384:512] = np.sin(ang) / 512.0
    sh["cbf"] = cbf.astype(ml_dtypes.bfloat16)
    p_i = np.arange(128)[:, None, None, None]
    par = np.arange(2)[None, :, None, None]
    ii = np.arange(8)[None, None, :, None]
    k1 = np.arange(1024)[None, None, None, :]
    tt = 2 * (ii * 128 + p_i) + par
    ph = (tt * k1) % 2048
    a2 = 2.0 * np.pi * ph.astype(np.float64) / 2048.0
    dft2 = np.stack([np.cos(a2), -np.sin(a2)], axis=1).astype(f32)
    sh["dft2"] = dft2.astype(ml_dtypes.bfloat16)
    return sh


def host_percore(inp, b):
    f32 = np.float32
    x = np.asarray(inp["x"], f32)[b]
    ctx = np.asarray(inp["ctx"], f32)[b]
    xc = np.ascontiguousarray(np.concatenate([ctx.T, x.T], axis=1))
    cv = np.zeros((128, 16), f32)
    cv[:, 0:8] = colvec(np.asarray(inp["c"], f32)[b])
    cv[:, 8:16] = colvec(np.asarray(inp["c_ctx"], f32))
    return {"xc": xc, "cv": cv}


def build(dbg=None):
    dbg = dbg or set()
    nc = bass.Bass("TRN2", target_bir_lowering=False)
    dram_in = lambda n, s, dt=F32: nc.dram_tensor(n, list(s), dt, kind="ExternalInput").ap()
    xc_d = dram_in("xc", [D, TT])
    cv_d = dram_in("cv", [128, 16])
    adaw_d = dram_in("adaw", [2, 128, 8, 6 * D])
    vecs_d = dram_in("vecs", [128, NV])
    rowv_d = dram_in("rowv", [H, 640])
    whp_d = dram_in("whp", [128, H, 8, 768])
    wgp_d = dram_in("wgp", [128, 8, 16])
    wmo_d = dram_in("wmo", [128, 8, D])
    wfo_d = dram_in("wfo", [128, 8, D])
    upp_d = dram_in("upp", [2, 128, NF, 8, 256])
    dnp_d = dram_in("dnp", [2, 128, NF, D])
    c32_d = dram_in("c32", [128, NC32])
    cbf_d = dram_in("cbf", [128, NCBF], BF16)
    dft2_d = dram_in("dft2", [128, 2, 2, 8, 1024], BF16)
    out_d = nc.dram_tensor("out", [D, T], F32, kind="ExternalOutput").ap()
    yt_d = nc.dram_tensor("yt_scr", [128, 8, T], BF16, kind="ExternalOutput").ap()
    dbg_out = {}

    xc_v = xc_d.rearrange("(k p) t -> p k t", p=128)
    out_v = out_d.rearrange("(k p) t -> p k t", p=128)

    with ExitStack() as st:
        S = Sched(nc, st)

        uid = [0]

        def sb(name, shape, dt, stack=st):
            uid[0] += 1
            return stack.enter_context(nc.sbuf_tensor("sb%d_%s" % (uid[0], name), list(shape), dt))

        XT = sb("XT", [128, 8, T], F32)
        XTf = XT[:].rearrange("p k t -> p (k t)")
        XTB = [Buf("XT%d" % i) for i in range(4)]
        vecs = sb("vecs", [128, NV], F32); VECS = Buf("vecs")
        c32 = sb("c32", [128, NC32], F32); C32B = Buf("c32")
        cbf = sb("cbf", [128, NCBF], BF16); CBFB = Buf("cbf")
        cv = sb("cv", [128, 16], F32); CVB = Buf("cv")
        scv = sb("scv", [128, 16], F32); SCVB = Buf("scv")
        mod = sb("mod", [128, 2, 48], F32); MODB = Buf("mod")
        cmod = sb("cmod", [128, 16], F32); CMODB = Buf("cmod")
        der = sb("der", [128, 2, 4, 8], F32); DERB = Buf("der")
        cder = sb("cder", [128, 8], F32); CDERB = Buf("cder")
        ident = cbf[:, 0:128]
        ones = cbf[:, 128:256]
        dftd = cbf[:, 256:512]

        PS = []
        psbs = [st.enter_context(nc.psum_tensor("psb%d" % i, [128, 1024], BF16)) for i in range(2)]
        NPS = 6
        for i in range(NPS):
            t = st.enter_context(nc.psum_tensor("ps%d" % i, [128, 512], F32))
            PS.append((t, Buf("ps%d" % i, excl=True)))
        ps_i = [0]

        def ps_next():
            r = PS[ps_i[0] % NPS]
            ps_i[0] += 1
            return r

        def vcol(name, i=0, n=1):
            return vecs[:, VOFF[name] + i:VOFF[name] + i + n]

        def stop(name):
            if name in dbg:
                S.barrier()
                S.stopped = True

        def dump(name, ap_sb, shape, dt, bufs):
            if name not in dbg:
                return
            d = nc.dram_tensor("dbg_" + name, list(shape), dt, kind="ExternalOutput").ap()
            B = Buf("dbg_" + name)
            S.dma("sp", d, ap_sb, reads=bufs, writes=[B])
            dbg_out[name] = B

        try:
            S.dma("sp", vecs[:], vecs_d[:, :], writes=[VECS])
            S.dma("sp", cv[:], cv_d[:, :], writes=[CVB])
            S.dma("sp", c32[:], c32_d[:, :], writes=[C32B])
            S.dma("sp", cbf[:], cbf_d[:, :], writes=[CBFB])

            scvb = sb("scvb", [128, 16], BF16); SCVBB = Buf("scvb")
            S.op("act", lambda e: e.activation(out=scv[:], in_=cv[:], func=AF.Silu), reads=[CVB], writes=[SCVB])
            S.op("dve", lambda e: e.tensor_copy(scvb[:], scv[:]), reads=[SCVB], writes=[SCVBB])
            ada_state = {"next_dma": 0, "next_pe": 0, "bufs": None}
            ada_items = [(l, nb) for l in range(2) for nb in range(24)]

            def ada_dma(n):
                ada, ADAB, modrow, MRB = ada_state["bufs"]
                if n >= len(ada_items):
                    return
                l, nb = ada_items[n]
                S.dma("pool", ada[n % 2][:], adaw_d[l, :, :, nb * 256:(nb + 1) * 256], writes=[ADAB[n % 2]])

            def ada_pe(n):
                ada, ADAB, modrow, MRB = ada_state["bufs"]
                l, nb = ada_items[n]
                bi = n % 2
                pt, PB = ps_next()

                def mm(e, pt=pt, bi=bi):
                    last = None
                    for kk in range(8):
                        last = e.matmul(pt[0:2, 0:256], V(scvb[:, kk:kk + 1], [[8, 2]]), ada[bi][:, kk, :],
                                        start=(kk == 0), stop=(kk == 7))
                    return last
                S.op("pe", mm, reads=[ADAB[bi], SCVBB], writes=[PB])
                mr, MB_ = modrow[n % 2], MRB[n % 2]
                S.op("act", lambda e, pt=pt, mr=mr: e.copy(mr[:, :], pt[0:2, 0:256]), reads=[PB], writes=[MB_])
                pt2, PB2 = ps_next()

                def mmT(e, pt2=pt2, mr=mr):
                    last = None
                    for c4 in range(2):
                        last = e.matmul(pt2[:, c4 * 2:c4 * 2 + 2], mr[:, c4 * 128:(c4 + 1) * 128],
                                        c32[0:2, C32["id8"]:C32["id8"] + 2], start=True, stop=True)
                    return last
                S.op("pe", mmT, reads=[MB_, C32B], writes=[PB2])
                S.op("dve", lambda e, pt2=pt2, l=l, nb=nb: e.tensor_tensor(
                    out=mod[:, l, nb * 2:nb * 2 + 2], in0=V(pt2[:, 0:1], [[2, 2]]),
                    in1=vcol("adab%d" % l, nb * 2, 2), op=ALU.add), reads=[PB2, VECS], writes=[MODB])
                if l == 0 and nb < 8:
                    S.op("dve", lambda e, pt2=pt2, nb=nb: e.tensor_tensor(
                        out=cmod[:, nb * 2:nb * 2 + 2], in0=V(pt2[:, 1:2], [[2, 2]]),
                        in1=vcol("adab0", nb * 2, 2), op=ALU.add), reads=[PB2, VECS], writes=[CMODB])

            def ada_more(k):
                for _ in range(k):
                    n = ada_state["next_pe"]
                    if n >= len(ada_items):
                        return
                    ada_pe(n)
                    ada_state["next_pe"] = n + 1
                    ada_dma(n + 2)
                if ada_state["next_pe"] == len(ada_items) and not ada_state.get("done"):
                    ada_state["done"] = True
                    for l in range(2):
                        S.op("dve", lambda e, l=l: e.tensor_tensor(
                            out=der[:, l, 1, :], in0=mod[:, l, 16:24], in1=vcol("postmix", 8 * l, 8), op=ALU.mult),
                            reads=[MODB, VECS], writes=[DERB])
                        S.op("dve", lambda e, l=l: e.scalar_tensor_tensor(
                            out=der[:, l, 2, :], in0=mod[:, l, 32:40], scalar=1.0, in1=vcol("preffn", 8 * l, 8),
                            op0=ALU.add, op1=ALU.mult), reads=[MODB, VECS], writes=[DERB])
                        S.op("dve", lambda e, l=l: e.tensor_tensor(
                            out=der[:, l, 3, :], in0=mod[:, l, 40:48], in1=vcol("postffn", 8 * l, 8), op=ALU.mult),
                            reads=[MODB, VECS], writes=[DERB])
                    S.op("dve", lambda e: e.scalar_tensor_tensor(
                        out=der[:, 1, 0, :], in0=mod[:, 1, 8:16], scalar=1.0, in1=vcol("premix", 8, 8),
                        op0=ALU.add, op1=ALU.mult), reads=[MODB, VECS], writes=[DERB])


            def rstd_from_sq(sq, SQB, nb, sd, SDB, rstd, RSB, ndiv=float(D)):
                pt, PB = ps_next()

                def mm(e):
                    last = None
                    for k in range(8):
                        last = e.matmul(pt[:, 0:nb], ones, sq[:, k, 0:nb], start=(k == 0), stop=(k == 7))
                    return last
                S.op("pe", mm, reads=[SQB, CBFB], writes=[PB])
                S.op("act", lambda e: e.activation(out=sd[:, 0:nb], in_=pt[:, 0:nb], func=AF.Ln,
                                                   bias=EPS, scale=1.0 / ndiv), reads=[PB], writes=[SDB])
                S.op("act", lambda e: e.activation(out=rstd[:, 0:nb], in_=sd[:, 0:nb], func=AF.Exp, scale=-0.5),
                     reads=[SDB], writes=[RSB])

            with ExitStack() as ph:
                ada = [sb("ada%d" % i, [128, 8, 256], BF16, ph) for i in range(2)]
                ADAB = [Buf("ada%d" % i) for i in range(2)]
                modrow = [sb("modrow%d" % i, [2, 256], F32, ph) for i in range(2)]
                MRB = [Buf("modrow%d" % i) for i in range(2)]
                ada_state["bufs"] = (ada, ADAB, modrow, MRB)
                for n in range(2):
                    ada_dma(n)
                ada_more(8)
                S.op("dve", lambda e: e.scalar_tensor_tensor(
                    out=der[:, 0, 0, :], in0=mod[:, 0, 8:16], scalar=1.0, in1=vcol("premix", 0, 8),
                    op0=ALU.add, op1=ALU.mult), reads=[MODB, VECS], writes=[DERB])
                S.op("dve", lambda e: e.scalar_tensor_tensor(
                    out=cder[:], in0=cmod[:, 8:16], scalar=1.0, in1=vcol("premix", 0, 8),
                    op0=ALU.add, op1=ALU.mult), reads=[CMODB, VECS], writes=[CDERB])
                hxT = sb("hxT", [128, 8, TT], BF16, ph)
                blocks = [(0, 256)] + [(256 + 512 * i, 512) for i in range(4)]
                HXB = [Buf("hx%d" % i) for i in range(5)]

                with ExitStack() as p1:
                    xb = [sb("xb%d" % i, [128, 8, 512], F32, p1) for i in range(2)]
                    XBB = [Buf("xb%d" % i) for i in range(2)]
                    sqs = [sb("sq1_%d" % i, [128, 8, 512], BF16, p1) for i in range(2)]
                    SQBS = [Buf("sq1_%d" % i) for i in range(2)]
                    sds = [sb("sd1_%d" % i, [128, 512], F32, p1) for i in range(2)]
                    SDBS = [Buf("sd1_%d" % i) for i in range(2)]
                    rstds = [sb("rstd1_%d" % i, [128, 512], F32, p1) for i in range(2)]
                    RSBS = [Buf("rstd1_%d" % i) for i in range(2)]
                    tmp = [sb("tmp1_%d" % i, [128, 512], F32, p1) for i in range(4)]
                    TMPB = [Buf("tmp1_%d" % i) for i in range(4)]
                    for bi, (t0, nb) in enumerate(blocks):
                        x_ = xb[bi % 2]; XB_ = XBB[bi % 2]
                        sq, SQB, sd, SDB, rstd, RSB = sqs[bi % 2], SQBS[bi % 2], sds[bi % 2], SDBS[bi % 2], rstds[bi % 2], RSBS[bi % 2]
                        S.dma("sp", x_[:, :, 0:nb], xc_v[:, :, t0:t0 + nb], writes=[XB_])
                        S.op("act", lambda e, x_=x_, nb=nb, sq=sq: e.activation(out=sq[:, :, 0:nb], in_=x_[:, :, 0:nb],
                                                                         func=AF.Square), reads=[XB_], writes=[SQB])
                        rstd_from_sq(sq, SQB, nb, sd, SDB, rstd, RSB)
                        for k in range(8):
                            tm = tmp[k % 4]; TB = TMPB[k % 4]
                            if bi == 0:
                                a_col, b_col, AB, BB = cder[:, k:k + 1], cmod[:, k:k + 1], CDERB, CMODB
                            else:
                                a_col, b_col, AB, BB = der[:, 0, 0, k:k + 1], mod[:, 0, k:k + 1], DERB, MODB
                            S.op("dve", lambda e, x_=x_, k=k, nb=nb, tm=tm, a_col=a_col, rstd=rstd: e.scalar_tensor_tensor(
                                out=tm[:, 0:nb], in0=x_[:, k, 0:nb], scalar=a_col, in1=rstd[:, 0:nb],
                                op0=ALU.mult, op1=ALU.mult), reads=[XB_, RSB, AB], writes=[TB])
                            if k % 2 == 0:
                                S.op("act", lambda e, k=k, nb=nb, t0=t0, tm=tm, b_col=b_col: e.activation(
                                    out=hxT[:, k, t0:t0 + nb], in_=tm[:, 0:nb], func=AF.Identity, bias=b_col, scale=1.0),
                                    reads=[TB, BB], writes=[HXB[bi]])
                            else:
                                S.op("dve", lambda e, k=k, nb=nb, t0=t0, tm=tm, b_col=b_col: e.tensor_scalar(
                                    out=hxT[:, k, t0:t0 + nb], in0=tm[:, 0:nb], scalar1=b_col, scalar2=None, op0=ALU.add),
                                    reads=[TB, BB], writes=[HXB[bi]])
                    S.barrier()
                dump("hxT", hxT[:], [128, 8, TT], BF16, HXB)

                stop("stop1")

                RW = [XTf[0:8, i * TT:(i + 1) * TT] for i in range(7)]
                RB = [Buf("row%d" % i) for i in range(7)]
                ucng = sb("ucng", [128, NCH, 16], F32, ph); UCB = Buf("ucng")
                tots = sb("tots", [8, 4], F32, ph); TOTB = Buf("tots")
                rsf = vecs[0:8, VOFF["rsf"]:VOFF["rsf"] + 1]
                rsb = vecs[0:8, VOFF["rsb"]:VOFF["rsb"] + 1]
                with ExitStack() as p2:
                    wg = sb("wg", [128, 8, 16], BF16, ph); WGB = Buf("wg")
                    S.rec = []
                    S.dma("pool", wg[:], wgp_d[:, :, :], writes=[WGB])
                    gblocks = [(i * 512, min(512, TT - i * 512)) for i in range(5)]
                    for (t0, nb) in gblocks:
                        bsel = [HXB[0], HXB[1]] if t0 == 0 else ([HXB[(t0 - 256) // 512 + 1]] + ([HXB[(t0 - 256) // 512 + 2]] if t0 + nb > 256 + ((t0 - 256) // 512 + 1) * 512 else []))
                        for gi in range(2):
                            pt, PB = ps_next()

                            def mm(e, pt=pt, t0=t0, nb=nb, gi=gi):
                                last = None
                                for k in range(8):
                                    last = e.matmul(pt[0:8, 0:nb], wg[:, k, gi * 8:gi * 8 + 8], hxT[:, k, t0:t0 + nb],
                                                    start=(k == 0), stop=(k == 7))
                                return last
                            S.op("pe", mm, reads=[WGB] + HXB, writes=[PB])
                            bcol = vecs[0:8, VOFF["gbi" if gi == 0 else "gbf"]:VOFF["gbi" if gi == 0 else "gbf"] + 1]
                            S.op("act", lambda e, pt=pt, t0=t0, nb=nb, gi=gi, bcol=bcol: e.activation(
                                out=RW[gi][:, t0:t0 + nb], in_=pt[0:8, 0:nb], func=AF.Identity, bias=bcol, scale=1.0),
                                reads=[PB, VECS], writes=[RB[gi]])
                    S.op("act", lambda e: e.activation(out=RW[1], in_=RW[1], func=AF.Exp, scale=-1.0),
                         reads=[RB[1]], writes=[RB[1]])
                    S.op("act", lambda e: e.activation(out=RW[1], in_=RW[1], func=AF.Ln, bias=1.0, scale=1.0),
                         reads=[RB[1]], writes=[RB[1]])
                    S.op("pool", lambda e: e.memset(RW[3], 0.0), writes=[RB[3]])
                    for (a, b) in ((0, TC), (TC, TT)):
                        S.op("dve", lambda e, a=a, b=b: e.tensor_tensor_scan(
                            out=RW[2][:, a:b], data0=RW[1][:, a:b], data1=RW[3][:, a:b], initial=0.0,
                            op0=ALU.add, op1=ALU.add), reads=[RB[1], RB[3]], writes=[RB[2]])
                    S.op("dve", lambda e: e.tensor_copy(tots[:, 0:1], RW[2][:, TC - 1:TC]), reads=[RB[2]], writes=[TOTB])
                    S.op("dve", lambda e: e.tensor_tensor(out=tots[:, 1:2], in0=RW[2][:, TC - 1:TC], in1=RW[2][:, TT - 1:TT],
                                                          op=ALU.add), reads=[RB[2]], writes=[TOTB])
                    S.op("dve", lambda e: e.tensor_copy(RW[4][:, 0:TC], RW[2][:, 0:TC]), reads=[RB[2]], writes=[RB[4]])
                    S.op("dve", lambda e: e.tensor_scalar(out=RW[4][:, TC:TT], in0=RW[2][:, TC:TT], scalar1=tots[:, 0:1],
                                                          scalar2=None, op0=ALU.add), reads=[RB[2], TOTB], writes=[RB[4]])
                    S.op("dve", lambda e: e.tensor_tensor(out=RW[5], in0=RW[1], in1=RW[2], op=ALU.subtract),
                         reads=[RB[1], RB[2]], writes=[RB[5]])
                    S.op("dve", lambda e: e.tensor_scalar(out=RW[5][:, 0:TC], in0=RW[5][:, 0:TC], scalar1=tots[:, 0:1],
                                                          scalar2=None, op0=ALU.add), reads=[RB[5], TOTB], writes=[RB[5]])
                    S.op("dve", lambda e: e.tensor_scalar(out=RW[5][:, TC:TT], in0=RW[5][:, TC:TT], scalar1=tots[:, 1:2],
                                                          scalar2=None, op0=ALU.add), reads=[RB[5], TOTB], writes=[RB[5]])
                    S.op("dve", lambda e: e.tensor_scalar(out=RW[4], in0=RW[4], scalar1=rsf, scalar2=None, op0=ALU.mult),
                         reads=[RB[4], VECS], writes=[RB[4]])
                    S.op("dve", lambda e: e.scalar_tensor_tensor(out=RW[4], in0=RW[5], scalar=rsb, in1=RW[4],
                                                                 op0=ALU.mult, op1=ALU.add),
                         reads=[RB[5], RB[4], VECS], writes=[RB[4]])
                    S.op("dve", lambda e: e.tensor_tensor(out=RW[0], in0=RW[0], in1=RW[4], op=ALU.add),
                         reads=[RB[0], RB[4]], writes=[RB[0]])
                    S.op("dve", lambda e: e.tensor_tensor_scan(out=RW[5], data0=RW[0], data1=RW[0], initial=0.0,
                                                               op0=ALU.max, op1=ALU.max), reads=[RB[0]], writes=[RB[5]])
                    cur, CURB = RW[0], RB[0]
                    pp = 0
                    sh = 1
                    while sh < TT - TC:
                        nxt, NXTB = RW[2 + pp], RB[2 + pp]
                        for (a, b) in ((0, TC), (TC, TT)):
                            n = b - a
                            if sh < n:
                                S.op("dve", lambda e, a=a, b=b, sh=sh, cur=cur, nxt=nxt: e.tensor_tensor(
                                    out=nxt[:, a:b - sh], in0=cur[:, a:b - sh], in1=cur[:, a + sh:b], op=ALU.max),
                                    reads=[CURB], writes=[NXTB])
                                S.op("pool", lambda e, a=a, b=b, sh=sh, cur=cur, nxt=nxt: e.tensor_copy(
                                    nxt[:, b - sh:b], cur[:, b - sh:b]), reads=[CURB], writes=[NXTB])
                            else:
                                S.op("pool", lambda e, a=a, b=b, cur=cur, nxt=nxt: e.tensor_copy(
                                    nxt[:, a:b], cur[:, a:b]), reads=[CURB], writes=[NXTB])
                        cur, CURB = nxt, NXTB
                        pp ^= 1
                        sh *= 2
                    sm, SMB = cur, CURB
                    S.op("dve", lambda e: e.tensor_scalar(out=sm[:, 0:TC], in0=sm[:, 0:TC], scalar1=0.0, scalar2=None,
                                                          op0=ALU.max), reads=[SMB], writes=[SMB])
                    S.op("dve", lambda e: e.tensor_copy(tots[:, 2:3], sm[:, 0:1]), reads=[SMB], writes=[TOTB])
                    S.op("dve", lambda e: e.tensor_scalar(out=sm[:, TC:TT], in0=sm[:, TC:TT], scalar1=tots[:, 2:3],
                                                          scalar2=None, op0=ALU.max), reads=[SMB, TOTB], writes=[SMB])
                    S.op("dve", lambda e: e.tensor_scalar(out=RW[5], in0=RW[5], scalar1=rsf, scalar2=None, op0=ALU.mult),
                         reads=[RB[5], VECS], writes=[RB[5]])
                    S.op("dve", lambda e: e.scalar_tensor_tensor(out=RW[5], in0=sm, scalar=rsb, in1=RW[5],
                                                                 op0=ALU.mult, op1=ALU.add),
                         reads=[SMB, RB[5], VECS], writes=[RB[5]])
                    S.op("dve", lambda e: e.tensor_tensor(out=RW[4], in0=RW[4], in1=RW[5], op=ALU.subtract),
                         reads=[RB[4], RB[5]], writes=[RB[4]])
                    pt, PB = ps_next()

                    def mmT(e, pt=pt):
                        last = None
                        for c in range(NCH):
                            e.matmul(pt[:, c * 16:c * 16 + 8], RW[0][:, c * 128:(c + 1) * 128],
                                     c32[0:8, C32["id8"]:C32["id8"] + 8], start=True, stop=True)
                            last = e.matmul(pt[:, c * 16 + 8:c * 16 + 16], RW[4][:, c * 128:(c + 1) * 128],
                                            c32[0:8, C32["id8"]:C32["id8"] + 8], start=True, stop=True)
                        return last
                    S.op("pe", mmT, reads=[RB[0], RB[4], C32B], writes=[PB])
                    S.op("dve", lambda e, pt=pt: e.tensor_copy(ucng[:].rearrange("p c j -> p (c j)"), pt[:, 0:NCH * 16]),
                         reads=[PB], writes=[UCB])
                    S.op("dve", lambda e: e.tensor_copy(RW[0], RW[5]), reads=[RB[5], RB[0]], writes=[RB[0]])
                    S.barrier()
                gate_rec = S.rec
                S.rec = None
                if "stop2a" in dbg or "ucng" in dbg:
                    S.replay(gate_rec, 10 ** 6)
                dump("ucng", ucng[:], [128, NCH, 16], F32, [UCB])
                stop("stop2a")
                MROW, MROWB = RW[0], RB[0]

                MbD = [XTf[:, TT + d * 2 * TT:2 * TT + d * 2 * TT] for d in range(2)]
                zzD = [XTf[:, 2 * TT + d * 2 * TT:3 * TT + d * 2 * TT] for d in range(2)]
                MBB = [Buf("Mb%d" % d) for d in range(2)]
                ZB = [Buf("zz%d" % d) for d in range(2)]
                Hh = XTf[:, 5 * TT:5 * TT + 4096]; HHB = Buf("Hh")
                Mb3D = [m.rearrange("p (c t) -> p c t", t=128) for m in MbD]
                zz3D = [z.rearrange("p (c t) -> p c t", t=128) for z in zzD]
                Hh3 = Hh.rearrange("p (c e) -> p c e", e=256)
                with ExitStack() as p3:
                    wh = [sb("wh%d" % i, [128, 8, 768], BF16, p3) for i in range(1)]
                    WHB = [Buf("wh%d" % i) for i in range(1)]
                    rowb = sb("rowb", [128, 640], F32, p3); ROWB = Buf("rowb")
                    QT = sb("QT", [128, TT], BF16, p3); QTB = Buf("QT")
                    KT = sb("KT", [128, TT], BF16, p3); KTB = Buf("KT")
                    KV = sb("KV", [128, NCH, 385], BF16, p3); KVB = Buf("KV")
                    Osig = sb("Osig", [128, 16, 256], BF16, p3); OSB = Buf("Osig")
                    otmp = [sb("otmp%d" % i, [128, 256], F32, p3) for i in range(2)]
                    OTB = [Buf("otmp%d" % i) for i in range(2)]
                    QW = [sb("QW%d" % d, [128, TT], BF16, p3) for d in range(2)]
                    QWB = [Buf("QW%d" % d) for d in range(2)]
                    Dj = [sb("Dj%d" % d, [128, NCH, 128], BF16, p3) for d in range(2)]
                    DJB = [Buf("Dj%d" % d) for d in range(2)]
                    KS = [sb("KS%d" % d, [128, NCH, 128], BF16, p3) for d in range(2)]
                    KSB = [Buf("KS%d" % d) for d in range(2)]
                    Cst = [[sb("Cst%d_%d" % (d, i), [128, 257], F32, p3) for i in range(2)] for d in range(2)]
                    CSTB = [[Buf("Cst%d_%d" % (d, i)) for i in range(2)] for d in range(2)]
                    Cbf = [[sb("Cbf%d_%d" % (d, i), [128, 257], BF16, p3) for i in range(2)] for d in range(2)]
                    CBFB2 = [[Buf("Cbf%d_%d" % (d, i)) for i in range(2)] for d in range(2)]
                    sm18 = [sb("sm18_%d" % d, [128, 6, NCH], F32, p3) for d in range(2)]
                    SM18 = [Buf("sm18_%d" % d) for d in range(2)]
                    Sp = [sb("Sp%d" % i, [128, 128], BF16, p3) for i in range(4)]
                    SPB = [Buf("Sp%d" % i) for i in range(4)]
                    dsm = sb("dsm", [128, 4, 4], F32, p3); DSMB = [Buf("dsm%d" % i) for i in range(4)]
                    ssq = sb("ssq", [128, 3, 16], F32, p3); SSQB = Buf("ssq")
                    yh, YHB = Osig, OSB
                    ytb = [sb("ytb%d" % i, [128, 512], BF16, p3) for i in range(2)]
                    YTBB = [Buf("ytb%d" % i) for i in range(2)]
                    PSBH = [Buf("psb0", excl=True), Buf("psb1", excl=True)]
                    YTD = Buf("ytd")
                    qscale = float(DK) ** -0.5
                    dctr = [0]
                    spctr = [0]
                    S.dma("pool", wh[0][:], whp_d[:, 0, :, :], writes=[WHB[0]])
                    S.op("pool", lambda e: e.memset(KV[:, :, 384:385], 1.0), writes=[KVB])
                    junk = sb("junk", [128, 256], BF16, p3); JUNKB = Buf("junk")
                    pending_readout = []
                    pending_tr = []
                    ro_thunks = []

                    def readout(h):
                        for c in range(16):
                            S.op("act", lambda e, c=c: e.activation(out=junk[:, :], in_=Hh3[:, c, :], func=AF.Square,
                                                                    accum_out=ssq[:, 0, c:c + 1]),
                                 reads=[HHB], writes=[JUNKB, SSQB])
                        S.op("act", lambda e: e.activation(out=ssq[:, 1, :], in_=ssq[:, 0, :], func=AF.Sqrt, bias=EPS,
                                                           scale=1.0 / DV), reads=[SSQB], writes=[SSQB])
                        S.op("dve", lambda e: e.reciprocal(out=ssq[:, 2, :], in_=ssq[:, 1, :]), reads=[SSQB], writes=[SSQB])
                        for c in range(16):
                            ro_thunks.append(lambda c=c: S.op("dve", lambda e: e.scalar_tensor_tensor(
                                out=yh[:, c, :], in0=Hh3[:, c, :], scalar=ssq[:, 2, c:c + 1], in1=Osig[:, c, :],
                                op0=ALU.mult, op1=ALU.mult), reads=[HHB, SSQB, OSB], writes=[OSB]))
                        pending_tr.append(h)

                    def readout_tr(h):
                        tctr = 0
                        for i in range(2):
                            for cg in range(4):
                                hb = tctr % 2
                                tctr += 1

                                def tr(e, i=i, cg=cg, hb=hb):
                                    last = None
                                    for q in range(4):
                                        last = e.transpose(psbs[hb][:, q * 128:(q + 1) * 128],
                                                           yh[:, cg * 4 + q, i * 128:(i + 1) * 128], ident)
                                    return last
                                S.op("pe", tr, reads=[YHB, CBFB], writes=[PSBH[hb]])
                                S.op("act", lambda e, hb=hb: e.copy(ytb[hb][:, :], psbs[hb][:, 0:512]),
                                     reads=[PSBH[hb]], writes=[YTBB[hb]])
                                S.dma("sp", yt_d[:, 2 * h + i, cg * 512:(cg + 1) * 512], ytb[hb][:, :],
                                      reads=[YTBB[hb]], writes=[YTD])

                    orders = [list(range(NCH)), [1, 0] + list(range(NCH - 1, 1, -1))]
                    mcols = [C32["maskf"], C32["maskb"]]
                    for h in range(H):
                        whh, WHH = wh[0], WHB[0]
                        ptick = [0, 0]

                        def proj_tick(ptick=ptick):
                            ptick[0] += 1
                            if ptick[0] % 4 == 0 and ptick[1] < 10:
                                ptick[1] += 1
                                ada_more(1)
                        S.dma("sp", rowb[:], bass.AP(rowv_d.tensor, rowv_d[h:h + 1, :].offset, [[0, 128], [1, 640]]),
                              writes=[ROWB])
                        def emit_mb_prep(h=h):
                            for d in range(2):
                                j = d * 4 + h
                                Mb, Mb3 = MbD[d], Mb3D[d]
                                RN, RC, AL, WS, EE, T18 = [sm18[d][:, i, :] for i in range(6)]
                                SMB = SM18[d]
                                for (t0, nb) in gblocks:
                                    pt, PB = ps_next()
                                    S.op("pe", lambda e, pt=pt, t0=t0, nb=nb, j=j: e.matmul(
                                        pt[:, 0:nb], c32[0:8, C32["sel"] + j * 128:C32["sel"] + (j + 1) * 128],
                                        MROW[:, t0:t0 + nb], start=True, stop=True), reads=[C32B, MROWB], writes=[PB])
                                    S.op("act", lambda e, pt=pt, t0=t0, nb=nb, Mb=Mb: e.copy(Mb[:, t0:t0 + nb], pt[:, 0:nb]),
                                         reads=[PB], writes=[MBB[d]])
                                ucj = ucng[:, :, j]
                                ngj = ucng[:, :, 8 + j]
                                if d == 0:
                                    S.op("dve", lambda e, RN=RN, Mb=Mb: e.tensor_copy(RN, V(Mb[:, 127:128], [[128, NCH]])),
                                         reads=[MBB[d]], writes=[SMB])
                                    S.op("pool", lambda e, RC=RC: e.memset(RC[:, 0:1], 0.0), writes=[SMB])
                                    S.op("dve", lambda e, RN=RN, RC=RC: e.tensor_copy(RC[:, 1:NCH], RN[:, 0:NCH - 1]),
                                         reads=[SMB], writes=[SMB])
                                else:
                                    S.op("dve", lambda e, RN=RN, Mb=Mb: e.tensor_copy(RN, V(Mb[:, 0:1], [[128, NCH]])),
                                         reads=[MBB[d]], writes=[SMB])
                                    S.op("pool", lambda e, RC=RC: e.memset(RC[:, 1:2], 0.0), writes=[SMB])
                                    S.op("dve", lambda e, RN=RN, RC=RC: e.tensor_copy(RC[:, 0:1], RN[:, 1:2]), reads=[SMB], writes=[SMB])
                                    S.op("dve", lambda e, RN=RN, RC=RC: e.tensor_copy(RC[:, 2:17], RN[:, 3:18]), reads=[SMB], writes=[SMB])
                                    S.op("dve", lambda e, RN=RN, RC=RC: e.tensor_copy(RC[:, 17:18], RN[:, 0:1]), reads=[SMB], writes=[SMB])
                                S.op("dve", lambda e, AL=AL, RC=RC, RN=RN: e.tensor_tensor(out=AL, in0=RC, in1=RN, op=ALU.subtract),
                                     reads=[SMB], writes=[SMB])
                                S.op("dve", lambda e, WS=WS, ucj=ucj, RN=RN: e.tensor_tensor(out=WS, in0=ucj, in1=RN, op=ALU.subtract),
                                     reads=[SMB, UCB], writes=[SMB])
                                S.op("act", lambda e, AL=AL: e.activation(out=AL, in_=AL, func=AF.Exp), reads=[SMB], writes=[SMB])
                                S.op("act", lambda e, WS=WS: e.activation(out=WS, in_=WS, func=AF.Exp), reads=[SMB], writes=[SMB])
                                S.op("act", lambda e, EE=EE, ngj=ngj: e.activation(out=EE, in_=ngj, func=AF.Exp), reads=[UCB], writes=[SMB])

                        if h > 0:
                            emit_mb_prep()
                        for (t0, nb) in gblocks:
                            for qi in range(2):
                                pt, PB = ps_next()

                                def mm(e, pt=pt, t0=t0, nb=nb, qi=qi, whh=whh):
                                    last = None
                                    for k in range(8):
                                        last = e.matmul(pt[:, 0:nb], whh[:, k, qi * 128:(qi + 1) * 128], hxT[:, k, t0:t0 + nb],
                                                        start=(k == 0), stop=(k == 7))
                                    return last
                                S.op("pe", mm, reads=[WHH] + HXB, writes=[PB])
                                if qi == 0:
                                    S.op("dve", lambda e, pt=pt, t0=t0, nb=nb, h=h: e.tensor_scalar(
                                        out=QT[:, t0:t0 + nb], in0=pt[:, 0:nb], scalar1=vcol("bq", h), scalar2=qscale,
                                        op0=ALU.add, op1=ALU.mult), reads=[PB, VECS], writes=[QTB])
                                else:
                                    S.op("act", lambda e, pt=pt, t0=t0, nb=nb, h=h: e.activation(
                                        out=KT[:, t0:t0 + nb], in_=pt[:, 0:nb], func=AF.Identity, bias=vcol("bk", h),
                                        scale=1.0), reads=[PB, VECS], writes=[KTB])
                                if h == 0:
                                    S.replay(gate_rec, 3)
                                proj_tick()
                        while pending_readout:
                            readout(pending_readout.pop(0))
                        thunks = []
                        for d in range(2):
                            j = d * 4 + h
                            Mb3, zz, zz3 = Mb3D[d], zzD[d], zz3D[d]
                            RN, RC, AL, WS, EE, T18 = [sm18[d][:, i, :] for i in range(6)]
                            ucj = ucng[:, :, j]
                            thunks.append(lambda d=d, zz3=zz3, Mb3=Mb3, RC=RC: S.op("dve", lambda e: e.tensor_tensor(
                                out=zz3, in0=Mb3, in1=V(RC[:, 0:1], [[1, NCH], [0, 128]]), op=ALU.subtract),
                                reads=[MBB[d], SM18[d]], writes=[ZB[d]]))
                            thunks.append(lambda d=d, zz=zz: S.op("act", lambda e: e.activation(
                                out=zz, in_=zz, func=AF.Exp, scale=-1.0), reads=[ZB[d]], writes=[ZB[d]]))
                            thunks.append(lambda d=d, zz=zz: S.op("dve", lambda e: e.tensor_tensor(
                                out=QW[d][:, :], in0=QT[:, :], in1=zz, op=ALU.mult), reads=[QTB, ZB[d]], writes=[QWB[d]]))
                            thunks.append(lambda d=d, zz3=zz3, Mb3=Mb3, ucj=ucj: S.op("dve", lambda e: e.tensor_tensor(
                                out=zz3, in0=Mb3, in1=V(ucj[:, 0:1], [[16, NCH], [0, 128]]), op=ALU.subtract),
                                reads=[MBB[d], UCB, ZB[d]], writes=[ZB[d]]))
                            thunks.append(lambda d=d, zz3=zz3: S.op("dve", lambda e: e.tensor_tensor(
                                out=zz3, in0=zz3, in1=V(c32[:, mcols[d]:mcols[d] + 1], [[0, NCH], [1, 128]]), op=ALU.add),
                                reads=[ZB[d], C32B], writes=[ZB[d]]))
                            thunks.append(lambda d=d, zz=zz: S.op("act", lambda e: e.activation(
                                out=Dj[d][:].rearrange("p c t -> p (c t)"), in_=zz, func=AF.Exp, scale=-1.0),
                                reads=[ZB[d]], writes=[DJB[d]]))
                        thunks = [t for pair in zip(thunks[0:6], thunks[6:12]) for t in pair]
                        for c in range(NCH):
                            pt, PB = ps_next()

                            def mm(e, pt=pt, c=c, whh=whh):
                                last = None
                                for k in range(8):
                                    last = e.matmul(pt[:, 0:384], hxT[:, k, c * 128:(c + 1) * 128], whh[:, k, 128:512],
                                                    start=(k == 0), stop=(k == 7))
                                return last
                            S.op("pe", mm, reads=[WHH] + HXB, writes=[PB])
                            S.op("dve", lambda e, pt=pt, c=c: e.tensor_tensor(
                                out=KV[:, c, 0:384], in0=pt[:, 0:384], in1=rowb[:, 0:384], op=ALU.add),
                                reads=[PB, ROWB], writes=[KVB])
                            if h == 0:
                                S.replay(gate_rec, 3)
                            elif ro_thunks:
                                ro_thunks.pop(0)()
                            elif thunks:
                                thunks.pop(0)()
                            proj_tick()
                        while ro_thunks:
                            ro_thunks.pop(0)()
                        while pending_tr:
                            readout_tr(pending_tr.pop(0))
                        for c in range(2, NCH):
                            pt2, PB2 = ps_next()

                            def mm2(e, pt2=pt2, c=c, whh=whh):
                                last = None
                                for k in range(8):
                                    last = e.matmul(pt2[:, 0:256], hxT[:, k, c * 128:(c + 1) * 128], whh[:, k, 512:768],
                                                    start=(k == 0), stop=(k == 7))
                                return last
                            S.op("pe", mm2, reads=[WHH] + HXB, writes=[PB2])
                            ot, OB_ = otmp[c % 2], OTB[c % 2]
                            S.op("dve", lambda e, pt2=pt2, ot=ot: e.tensor_tensor(
                                out=ot[:, :], in0=pt2[:, 0:256], in1=rowb[:, 384:640], op=ALU.add),
                                reads=[PB2, ROWB], writes=[OB_])
                            S.op("act", lambda e, ot=ot, c=c: e.activation(out=Osig[:, c - 2, :], in_=ot[:, :],
                                                                           func=AF.Sigmoid), reads=[OB_], writes=[OSB])
                            if h == 0:
                                S.replay(gate_rec, 3)
                            elif thunks:
                                thunks.pop(0)()
                            proj_tick()
                        if h == 0:
                            S.replay(gate_rec, 10 ** 6)
                            emit_mb_prep()
                        while thunks:
                            thunks.pop(0)()
                        for d in range(2):
                            WS = sm18[d][:, 3, :]
                            S.op("dve", lambda e, d=d, WS=WS: e.tensor_tensor(
                                out=KS[d][:], in0=KV[:, :, 0:128], in1=V(WS[:, 0:1], [[1, NCH], [0, 128]]), op=ALU.mult),
                                reads=[KVB, SM18[d]], writes=[KSB[d]])
                        if h + 1 < H:
                            S.dma("pool", wh[0][:], whp_d[:, h + 1, :, :], writes=[WHB[0]])
                        while ptick[1] < 10:
                            ptick[1] += 1
                            ada_more(1)
                        if h == 0:
                            stop("stop2p")
                        touched = set()
                        cur = [0, 0]
                        for idx in range(NCH):
                            work = []
                            for d in range(2):
                                c = orders[d][idx]
                                AL, EE = sm18[d][:, 2, :], sm18[d][:, 4, :]
                                it = {"d": d, "c": c, "AL": AL, "EE": EE}
                                if c >= 2:
                                    pts, PSB_ = ps_next()
                                    S.op("pe", lambda e, pts=pts, c=c: e.matmul(
                                        pts[:, 0:128], KT[:, c * 128:(c + 1) * 128], QT[:, c * 128:(c + 1) * 128],
                                        start=True, stop=True), reads=[KTB, QTB], writes=[PSB_])
                                    it["pts"], it["PSB"] = pts, PSB_
                                if idx < NCH - 1:
                                    ptu, PUB = ps_next()
                                    S.op("pe", lambda e, ptu=ptu, c=c, d=d: e.matmul(
                                        ptu[:, 0:257], KS[d][:, c, :], KV[:, c, 128:385], start=True, stop=True),
                                        reads=[KSB[d], KVB], writes=[PUB])
                                    it["ptu"], it["PUB"] = ptu, PUB
                                work.append(it)
                            for it in work:
                                d, c = it["d"], it["c"]
                                if c >= 2:
                                    pts, PSB_ = it["pts"], it["PSB"]
                                    si = spctr[0] % 4
                                    spctr[0] += 1
                                    sp_, SB_ = Sp[si], SPB[si]
                                    S.op("dve", lambda e, pts=pts, c=c, sp_=sp_, d=d: e.tensor_tensor(
                                        out=sp_[:, :], in0=pts[:, 0:128], in1=Dj[d][:, c, :], op=ALU.mult),
                                        reads=[PSB_, DJB[d]], writes=[SB_])
                                    pto, POB = ps_next()
                                    cb_, CB_ = Cbf[d][cur[d]], CBFB2[d][cur[d]]

                                    def mmo(e, pto=pto, c=c, sp_=sp_, d=d, cb_=cb_):
                                        e.matmul(pto[:, 0:257], sp_[:, :], KV[:, c, 128:385], start=True, stop=False)
                                        return e.matmul(pto[:, 0:257], QW[d][:, c * 128:(c + 1) * 128], cb_[:, :],
                                                        start=False, stop=True)
                                    S.op("pe", mmo, reads=[SB_, KVB, QWB[d], CB_], writes=[POB])
                                    it["pto"], it["POB"] = pto, POB
                            for it in work:
                                d, c = it["d"], it["c"]
                                if idx < NCH - 1:
                                    ptu, PUB = it["ptu"], it["PUB"]
                                    co, cn = cur[d], 1 - cur[d]
                                    if idx == 0:
                                        S.op("dve", lambda e, ptu=ptu, d=d, cn=cn: e.tensor_copy(Cst[d][cn][:, :], ptu[:, 0:257]),
                                             reads=[PUB], writes=[CSTB[d][cn]])
                                    else:
                                        S.op("dve", lambda e, ptu=ptu, d=d, co=co, cn=cn, c=c, AL=it["AL"]: e.scalar_tensor_tensor(
                                            out=Cst[d][cn][:, :], in0=Cst[d][co][:, :], scalar=AL[:, c:c + 1], in1=ptu[:, 0:257],
                                            op0=ALU.mult, op1=ALU.add), reads=[PUB, CSTB[d][co], SM18[d]], writes=[CSTB[d][cn]])
                                    S.op("act", lambda e, d=d, cn=cn: e.copy(Cbf[d][cn][:, :], Cst[d][cn][:, :]),
                                         reads=[CSTB[d][cn]], writes=[CBFB2[d][cn]])
                                    cur[d] = cn
                                if c >= 2:
                                    pto, POB = it["pto"], it["POB"]
                                    di = dctr[0] % 4
                                    dctr[0] += 1
                                    dd_, DB_ = dsm[:, di, :], DSMB[di]
                                    EE = it["EE"]
                                    S.op("act", lambda e, pto=pto, dd_=dd_: e.activation(out=dd_[:, 0:1], in_=pto[:, 256:257],
                                                                                         func=AF.Abs), reads=[POB], writes=[DB_])
                                    S.op("dve", lambda e, dd_=dd_, c=c, EE=EE: e.tensor_tensor(
                                        out=dd_[:, 1:2], in0=dd_[:, 0:1], in1=EE[:, c:c + 1], op=ALU.max),
                                        reads=[DB_, SM18[d]], writes=[DB_])
                                    S.op("dve", lambda e, dd_=dd_: e.reciprocal(out=dd_[:, 2:3], in_=dd_[:, 1:2]),
                                         reads=[DB_], writes=[DB_])
                                    if c not in touched:
                                        touched.add(c)
                                        S.op("dve", lambda e, pto=pto, dd_=dd_, c=c: e.tensor_scalar(
                                            out=Hh3[:, c - 2, :], in0=pto[:, 0:256], scalar1=dd_[:, 2:3], scalar2=None,
                                            op0=ALU.mult), reads=[POB, DB_], writes=[HHB])
                                    else:
                                        S.op("dve", lambda e, pto=pto, dd_=dd_, c=c: e.scalar_tensor_tensor(
                                            out=Hh3[:, c - 2, :], in0=pto[:, 0:256], scalar=dd_[:, 2:3], in1=Hh3[:, c - 2, :],
                                            op0=ALU.mult, op1=ALU.add), reads=[POB, DB_, HHB], writes=[HHB])
                        if h == 0:
                            stop("stop2o")
                        pending_readout.append(h)
                        if h == 0:
                            stop("stop2h")
                    while pending_readout:
                        readout(pending_readout.pop(0))
                    while ro_thunks:
                        ro_thunks.pop(0)()
                    while pending_tr:
                        readout_tr(pending_tr.pop(0))
                    stop("stop2z")
                    S.barrier()

            def mk_pnr(ph_, tag, shared=None, nbuf=1):
                sets = []
                for i in range(nbuf):
                    yo_ = sb("yo%s%d" % (tag, i), [128, 8, 512], F32, ph_)
                    YOBS_ = [Buf("yo%s%d_%d" % (tag, i, j)) for j in range(8)]
                    if shared is None:
                        sq_ = sb("sqo%s%d" % (tag, i), [128, 8, 512], BF16, ph_); SQB_ = Buf("sqo%s%d" % (tag, i))
                        sd_ = sb("sdo%s%d" % (tag, i), [128, 512], F32, ph_); SDB_ = Buf("sdo%s%d" % (tag, i))
                        rstd_ = sb("rso%s%d" % (tag, i), [128, 512], F32, ph_); RSB_ = Buf("rso%s%d" % (tag, i))
                    else:
                        sq_, SQB_, sd_, SDB_, rstd_, RSB_ = shared
                    sets.append((yo_, YOBS_, sq_, SQB_, sd_, SDB_, rstd_, RSB_))

                def part1(blk, wsb, WBs, rhs_fn, rhs_bufs_fn, nk, bias_name=None, split_tail=0):
                    yo, YOBS, sq, SQB, sd, SDB, rstd, RSB = sets[blk % nbuf]
                    for dc in range(8):
                        pt, PB = ps_next()
                        segs = [(0, nk)]
                        if dc == 0 and split_tail:
                            segs = [(0, nk - split_tail), (nk - split_tail, nk)]
                        for (k0, k1) in segs:
                            def mm(e, pt=pt, dc=dc, k0=k0, k1=k1):
                                last = None
                                for k in range(k0, k1):
                                    last = e.matmul(pt[:, :], wsb[:, k, dc * 128:(dc + 1) * 128], rhs_fn(k),
                                                    start=(k == 0), stop=(k == nk - 1))
                                return last
                            S.op("pe", mm, reads=WBs + rhs_bufs_fn(k0, k1), writes=[PB])
                        YOB = YOBS[dc]
                        if bias_name is None:
                            S.op("dve", lambda e, pt=pt, dc=dc: e.tensor_copy(yo[:, dc, :], pt[:, :]), reads=[PB], writes=[YOB])
                            S.op("act", lambda e, pt=pt, dc=dc: e.activation(out=sq[:, dc, :], in_=pt[:, :], func=AF.Square),
                                 reads=[PB], writes=[SQB])
                        else:
                            S.op("dve", lambda e, pt=pt, dc=dc: e.tensor_scalar(
                                out=yo[:, dc, :], in0=pt[:, :], scalar1=vcol(bias_name, dc), scalar2=None, op0=ALU.add),
                                reads=[PB, VECS], writes=[YOB])
                            S.op("act", lambda e, pt=pt, dc=dc: e.activation(out=sq[:, dc, :], in_=pt[:, :], func=AF.Square,
                                                                             bias=vcol(bias_name, dc), scale=1.0),
                                 reads=[PB, VECS], writes=[SQB])

                def part2(blk, gate_idx, layer, xr, XRB):
                    yo, YOBS, sq, SQB, sd, SDB, rstd, RSB = sets[blk % nbuf]
                    rstd_from_sq(sq, SQB, 512, sd, SDB, rstd, RSB)
                    for dc in range(8):
                        YOB = YOBS[dc]
                        S.op("dve", lambda e, dc=dc: e.scalar_tensor_tensor(
                            out=yo[:, dc, :], in0=yo[:, dc, :], scalar=der[:, layer, gate_idx, dc:dc + 1], in1=rstd[:, :],
                            op0=ALU.mult, op1=ALU.mult), reads=[YOB, DERB, RSB], writes=[YOB])
                        S.op("dve", lambda e, dc=dc: e.tensor_tensor(
                            out=XT[:, dc, blk * 512:(blk + 1) * 512], in0=yo[:, dc, :], in1=xr(dc), op=ALU.add),
                            reads=[YOB] + XRB, writes=[XTB[blk]])

                def run(blk, wsb, WBs, rhs_fn, rhs_bufs, nk, gate_idx, layer, xr, XRB, bias_name=None):
                    part1(blk, wsb, WBs, rhs_fn, lambda k0, k1: rhs_bufs, nk, bias_name)
                    part2(blk, gate_idx, layer, xr, XRB)
                run.part1 = part1
                run.part2 = part2
                return run

            with ExitStack() as ph:
                wmo = sb("wmo", [128, 8, D], BF16, ph); WMOB = Buf("wmo")
                S.dma("pool", wmo[:], wmo_d[:, :, :], writes=[WMOB])
                for k in range(8):
                    S.op("dve", lambda e, k=k: e.tensor_scalar(out=wmo[:, k, :], in0=wmo[:, k, :], scalar1=vcol("ngc", k),
                                                                scalar2=None, op0=ALU.mult), reads=[WMOB, VECS], writes=[WMOB])
                ytl = [sb("ytl%d" % i, [128, 8, 512], BF16, ph) for i in range(2)]
                YTLB = [Buf("ytl%d" % i) for i in range(2)]
                xrs = [sb("xrs%d" % i, [128, 8, 512], F32, ph) for i in range(2)]
                XRSB = [Buf("xrs%d" % i) for i in range(2)]

                pnr = mk_pnr(ph, "3", nbuf=2)
                for blk in range(4):
                    yb, YB_ = ytl[blk % 2], YTLB[blk % 2]
                    xb_, XB_ = xrs[blk % 2], XRSB[blk % 2]
                    S.dma("sp", yb[:], yt_d[:, :, blk * 512:(blk + 1) * 512], reads=[YTD], writes=[YB_])
                    pnr.part1(blk, wmo, [WMOB], lambda k, yb=yb: yb[:, k, :], lambda k0, k1, YB_=YB_: [YB_], 8)
                    if blk > 0:
                        pb = blk - 1
                        pnr.part2(pb, 1, 0, lambda dc, xq=xrs[pb % 2]: xq[:, dc, :], [XRSB[pb % 2]])
                    S.dma("sp", xb_[:], xc_v[:, :, TC + blk * 512:TC + (blk + 1) * 512], writes=[XB_])
                pnr.part2(3, 1, 0, lambda dc, xq=xrs[1]: xq[:, dc, :], [XRSB[1]])
                S.barrier()
            dump("x1", XT[:], [128, 8, T], F32, XTB)
            stop("stop3")


            def pre_norm_sq(blk, sq, SQB):
                S.op("act", lambda e: e.activation(out=sq[:, :, :], in_=XT[:, :, blk * 512:(blk + 1) * 512],
                                                   func=AF.Square), reads=[XTB[blk]], writes=[SQB])

            def pre_norm_rest(blk, layer, a_idx, b_off, sq, SQB, sd, SDB, rstd, RSB, tmp, TMPB, dst_fn, DSTB_fn):
                rstd_from_sq(sq, SQB, 512, sd, SDB, rstd, RSB)
                for k in range(8):
                    tm, TB = tmp[k % 2], TMPB[k % 2]
                    S.op("dve", lambda e, k=k, tm=tm: e.scalar_tensor_tensor(
                        out=tm[:, :], in0=XT[:, k, blk * 512:(blk + 1) * 512], scalar=der[:, layer, a_idx, k:k + 1],
                        in1=rstd[:, :], op0=ALU.mult, op1=ALU.mult), reads=[XTB[blk], RSB, DERB], writes=[TB])
                    if k % 2 == 0:
                        S.op("act", lambda e, k=k, tm=tm: e.activation(
                            out=dst_fn(k), in_=tm[:, :], func=AF.Identity, bias=mod[:, layer, b_off + k:b_off + k + 1],
                            scale=1.0), reads=[TB, MODB], writes=DSTB_fn(k))
                    else:
                        S.op("dve", lambda e, k=k, tm=tm: e.tensor_scalar(
                            out=dst_fn(k), in0=tm[:, :], scalar1=mod[:, layer, b_off + k:b_off + k + 1], scalar2=None,
                            op0=ALU.add), reads=[TB, MODB], writes=DSTB_fn(k))

            def pre_norm_block(blk, layer, a_idx, b_off, sq, SQB, sd, SDB, rstd, RSB, tmp, TMPB, dst_fn, DSTB_fn):
                pre_norm_sq(blk, sq, SQB)
                pre_norm_rest(blk, layer, a_idx, b_off, sq, SQB, sd, SDB, rstd, RSB, tmp, TMPB, dst_fn, DSTB_fn)

            OUTB = Buf("outd")

            def ffn(l):
                with ExitStack() as ph:
                    wdn = sb("wdn", [128, NF, D], BF16, ph)
                    WDNB = [Buf("wdn%d_%d" % (l, i)) for i in range(2)]
                    h2T = [sb("h2T%d" % i, [128, 8, 512], BF16, ph) for i in range(2)]
                    H2B = [Buf("h2T%d_%d" % (l, i)) for i in range(2)]
                    aT = sb("aT", [128, NF, 512], BF16, ph); ATBS = [Buf("aT%d_%d" % (l, i)) for i in range(NF)]
                    wup = [sb("wup%d" % i, [128, 8, 256], BF16, ph) for i in range(4)]
                    WUPB = [Buf("wup%d_%d" % (l, i)) for i in range(4)]
                    sq = sb("sqf", [128, 8, 512], BF16, ph); SQB = Buf("sqf%d" % l)
                    sd = sb("sdf", [128, 512], F32, ph); SDB = Buf("sdf%d" % l)
                    rstd = sb("rsf", [128, 512], F32, ph); RSB = Buf("rsf%d" % l)
                    gc = [sb("gc%d" % i, [128, 512], F32, ph) for i in range(2)]
                    GCB = [Buf("gc%d_%d" % (l, i)) for i in range(2)]
                    tmp, TMPB = gc, GCB
                    sg = [sb("sg%d" % i, [128, 512], F32, ph) for i in range(2)]
                    SGB = [Buf("sg%d_%d" % (l, i)) for i in range(2)]
                    pnr = mk_pnr(ph, "f", shared=(sq, SQB, sd, SDB, rstd, RSB))
                    wctr = 0
                    for blk in range(4):
                        hb, HB_ = h2T[blk % 2], H2B[blk % 2]
                        if blk == 0:
                            pre_norm_block(0, l, 2, 24, sq, SQB, sd, SDB, rstd, RSB, tmp, TMPB,
                                           lambda k, hb=hb: hb[:, k, :], lambda k, HB_=HB_: [HB_])
                        for f in range(NF):
                            if f == 2 and blk > 0:
                                pnr.part2(blk - 1, 3, l, lambda dc, b_=blk - 1: XT[:, dc, b_ * 512:(b_ + 1) * 512], [XTB[blk - 1]])
                                if l == 1:
                                    b_ = blk - 1
                                    S.dma("sp", out_v[:, :, b_ * 512:(b_ + 1) * 512], XT[:, :, b_ * 512:(b_ + 1) * 512],
                                          reads=[XTB[b_]], writes=[OUTB])
                            if f == 9 and blk < 3:
                                pre_norm_sq(blk + 1, sq, SQB)
                            if f == 12 and blk < 3:
                                hn, HN_ = h2T[(blk + 1) % 2], H2B[(blk + 1) % 2]
                                pre_norm_rest(blk + 1, l, 2, 24, sq, SQB, sd, SDB, rstd, RSB, tmp, TMPB,
                                              lambda k, hn=hn: hn[:, k, :], lambda k, HN_=HN_: [HN_])
                            wb, WB_ = wup[wctr % 4], WUPB[wctr % 4]
                            wctr += 1
                            S.dma("pool", wb[:], upp_d[l, :, f, :, :], writes=[WB_])
                            if blk == 0 and f in (12, 17):
                                hf = 0 if f == 12 else 1
                                S.dma("pool", wdn[:, hf * 11:(hf + 1) * 11, :], dnp_d[l, :, hf * 11:(hf + 1) * 11, :],
                                      writes=[WDNB[hf]])
                            if True:
                                ptu, PUB = ps_next()
                                ptg, PGB = ps_next()

                                def mmu(e, ptu=ptu, wb=wb, hb=hb):
                                    last = None
                                    for k in range(8):
                                        last = e.matmul(ptu[:, :], wb[:, k, 0:128], hb[:, k, :], start=(k == 0), stop=(k == 7))
                                    return last

                                def mmg(e, ptg=ptg, wb=wb, hb=hb):
                                    last = None
                                    for k in range(8):
                                        last = e.matmul(ptg[:, :], wb[:, k, 128:256], hb[:, k, :], start=(k == 0), stop=(k == 7))
                                    return last
                                S.op("pe", mmg, reads=[WB_, HB_], writes=[PGB])
                                S.op("pe", mmu, reads=[WB_, HB_], writes=[PUB])
                                g_, GB_ = gc[f % 2], GCB[f % 2]
                                s_, SB_ = sg[f % 2], SGB[f % 2]
                                w0 = vcol("convw", (l * 3 + 0) * 22 + f)
                                w1 = vcol("convw", (l * 3 + 1) * 22 + f)
                                w2 = vcol("convw", (l * 3 + 2) * 22 + f)
                                cb_ = vcol("convb", l * 22 + f)
                                S.op("act", lambda e, ptg=ptg, g_=g_, w1=w1, cb_=cb_: e.activation(
                                    out=g_[:, :], in_=ptg[:, :], func=AF.Identity, bias=cb_, scale=w1),
                                    reads=[PGB, VECS], writes=[GB_])
                                g3 = g_[:, :].rearrange("p (r c) -> p r c", c=64)
                                p3 = ptg[:, :].rearrange("p (r c) -> p r c", c=64)
                                S.op("dve", lambda e, g3=g3, p3=p3, w0=w0: e.scalar_tensor_tensor(
                                    out=g3[:, :, 1:64], in0=p3[:, :, 0:63], scalar=w0, in1=g3[:, :, 1:64],
                                    op0=ALU.mult, op1=ALU.add), reads=[PGB, GB_, VECS], writes=[GB_])
                                S.op("dve", lambda e, g3=g3, p3=p3, w2=w2: e.scalar_tensor_tensor(
                                    out=g3[:, :, 0:63], in0=p3[:, :, 1:64], scalar=w2, in1=g3[:, :, 0:63],
                                    op0=ALU.mult, op1=ALU.add), reads=[PGB, GB_, VECS], writes=[GB_])
                                S.op("act", lambda e, g_=g_, s_=s_: e.activation(out=s_[:, :], in_=g_[:, :], func=AF.Silu),
                                     reads=[GB_], writes=[SB_])
                                S.op("dve", lambda e, s_=s_, ptu=ptu, f=f: e.tensor_tensor(
                                    out=aT[:, f, :], in0=s_[:, :], in1=ptu[:, :], op=ALU.mult),
                                    reads=[SB_, PUB], writes=[ATBS[f]])
                        pnr.part1(blk, wdn, WDNB, lambda k: aT[:, k, :], lambda k0, k1: ATBS[k0:k1], NF, split_tail=3)
                    pnr.part2(3, 3, l, lambda dc: XT[:, dc, 3 * 512:4 * 512], [XTB[3]])
                    S.barrier()

            ffn(0)
            dump("x2", XT[:], [128, 8, T], F32, XTB)
            stop("stop4")

            with ExitStack() as ph:
                hT = sb("hT", [128, 8, T], BF16, ph)
                HTB = [Buf("hT%d" % g) for g in range(8)]
                wfo = sb("wfo", [128, 8, D], BF16, ph); WFOB = Buf("wfo")
                S.dma("pool", wfo[:], wfo_d[:, :, :], writes=[WFOB])
                with ExitStack() as p5:
                    dft = sb("dft", [128, 2, 2, 8, 1024], BF16, p5)
                    DFTB = [[Buf("dft%d%d" % (a_, b_)) for b_ in range(2)] for a_ in range(2)]
                    for a_ in range(2):
                        for b_ in range(2):
                            S.dma("sp", dft[:, a_, b_, :, :], dft2_d[:, a_, b_, :, :], writes=[DFTB[a_][b_]])
                    PQ = [sb("PQ%d" % i, [128, 2, 8, 256], BF16, p5) for i in range(2)]
                    PQB = [Buf("PQ%d" % i) for i in range(2)]
                    Esb = [sb("Esb%d" % i, [128, 512], F32, p5) for i in range(2)]
                    ESB = [Buf("Esb%d" % i) for i in range(2)]
                    sd = sb("sd5", [128, 512], F32, p5); SDB = Buf("sd5")
                    rstd4 = PQ[0][:].rearrange("p a i c -> p (a i c)").bitcast(F32).rearrange("p (b t) -> p b t", t=512)
                    RSB4 = [PQB[0]] * 4
                    for blk in range(4):
                        k0 = 4 + 2 * (blk % 2)
                        sq = hT[:, k0:k0 + 2, :].rearrange("p a t -> p (a t)").rearrange("p (k t) -> p k t", t=512)
                        SQW = [HTB[k0], HTB[k0 + 1]]
                        S.op("act", lambda e, blk=blk, sq=sq: e.activation(out=sq, in_=XT[:, :, blk * 512:(blk + 1) * 512],
                                                                          func=AF.Square), reads=[XTB[blk]], writes=SQW)
                        pt, PB = ps_next()

                        def mmss(e, pt=pt, sq=sq):
                            last = None
                            for k in range(8):
                                last = e.matmul(pt[:, :], ones, sq[:, k, :], start=(k == 0), stop=(k == 7))
                            return last
                        S.op("pe", mmss, reads=SQW + [CBFB], writes=[PB])
                        S.op("act", lambda e, pt=pt: e.activation(out=sd[:, :], in_=pt[:, :], func=AF.Ln, bias=EPS,
                                                                  scale=1.0 / D), reads=[PB], writes=[SDB])
                        S.op("act", lambda e, blk=blk: e.activation(out=rstd4[:, blk, :], in_=sd[:, :], func=AF.Exp,
                                                                   scale=-0.5), reads=[SDB], writes=[RSB4[blk]])
                    tctr5 = [0]
                    pn_thunks = []

                    def pn_pair(k, blk):
                        tm, TB = Esb[tctr5[0] % 2], ESB[tctr5[0] % 2]
                        tctr5[0] += 1
                        S.op("dve", lambda e: e.scalar_tensor_tensor(
                            out=tm[:, :], in0=XT[:, k, blk * 512:(blk + 1) * 512], scalar=der[:, 1, 0, k:k + 1],
                            in1=rstd4[:, blk, :], op0=ALU.mult, op1=ALU.mult),
                            reads=[XTB[blk], RSB4[blk], DERB], writes=[TB])
                        if tctr5[0] % 2 == 0:
                            S.op("act", lambda e: e.activation(
                                out=hT[:, k, blk * 512:(blk + 1) * 512], in_=tm[:, :], func=AF.Identity,
                                bias=mod[:, 1, k:k + 1], scale=1.0), reads=[TB, MODB], writes=[HTB[k]])
                        else:
                            S.op("dve", lambda e: e.tensor_scalar(
                                out=hT[:, k, blk * 512:(blk + 1) * 512], in0=tm[:, :], scalar1=mod[:, 1, k:k + 1],
                                scalar2=None, op0=ALU.add), reads=[TB, MODB], writes=[HTB[k]])
                    for blk in range(4):
                        pn_pair(0, blk)
                    for k in range(1, 8):
                        for blk in range(4):
                            pn_thunks.append(lambda k=k, blk=blk: pn_pair(k, blk))
                    ectr = 0
                    for g in range(8):
                        pq, PQB_ = PQ[1], PQB[1]
                        for par in range(2):
                            for ip in range(4):
                                pt, PB = ps_next()

                                def mm0(e, pt=pt, g=g, par=par, ip=ip):
                                    last = None
                                    for q in range(2):
                                        i = 2 * ip + q
                                        t0 = 2 * i * 128 + par
                                        last = e.matmul(pt[:, q * 256:(q + 1) * 256], V(hT[:, g, t0:t0 + 1], [[2, 128]]),
                                                        dftd, start=True, stop=True)
                                    return last
                                S.op("pe", mm0, reads=[HTB[g], CBFB], writes=[PB])
                                if pn_thunks and ip % 2 == 1:
                                    pn_thunks.pop(0)()
                                dst = pq[:, par, 2 * ip:2 * ip + 2, :].rearrange("p a b -> p (a b)")
                                if ip % 2 == 0:
                                    S.op("act", lambda e, pt=pt, dst=dst: e.copy(dst, pt[:, :]), reads=[PB], writes=[PQB_])
                                else:
                                    S.op("dve", lambda e, pt=pt, dst=dst: e.tensor_copy(dst, pt[:, :]), reads=[PB], writes=[PQB_])
                        for kb in range(2):
                            pte, PEB = ps_next()
                            pto, POB = ps_next()
                            for par, ptx, PXB in ((0, pte, PEB), (1, pto, POB)):
                                def mm1(e, ptx=ptx, par=par, kb=kb, pq=pq):
                                    last = None
                                    for i in range(8):
                                        e.matmul(ptx[:, :], pq[:, par, i, 0:128], dft[:, 0, par, i, kb * 512:(kb + 1) * 512],
                                                 start=(i == 0), stop=False)
                                        last = e.matmul(ptx[:, :], pq[:, par, i, 128:256],
                                                        dft[:, 1, par, i, kb * 512:(kb + 1) * 512], start=False, stop=(i == 7))
                                    return last
                                S.op("pe", mm1, reads=[PQB_, DFTB[0][par], DFTB[1][par]], writes=[PXB])
                            es, ESB_ = Esb[ectr % 2], ESB[ectr % 2]
                            ectr += 1
                            S.op("act", lambda e, es=es, pte=pte: e.copy(es[:, :], pte[:, :]), reads=[PEB], writes=[ESB_])
                            S.op("dve", lambda e, es=es, pto=pto, g=g, kb=kb: e.tensor_tensor(
                                out=hT[:, g, kb * 512:(kb + 1) * 512], in0=es[:, :], in1=pto[:, :], op=ALU.add),
                                reads=[ESB_, POB], writes=[HTB[g]])
                            S.op("dve", lambda e, es=es, pto=pto, g=g, kb=kb: e.tensor_tensor(
                                out=hT[:, g, 1024 + kb * 512:1024 + (kb + 1) * 512], in0=es[:, :], in1=pto[:, :],
                                op=ALU.subtract), reads=[ESB_, POB], writes=[HTB[g]])
                    S.barrier()
                dump("yfftT", hT[:], [128, 8, T], BF16, HTB)
                with ExitStack() as p6:
                    pnr = mk_pnr(p6, "6", nbuf=2)
                    for blk in range(4):
                        pnr.part1(blk, wfo, [WFOB], lambda k, blk=blk: hT[:, k, blk * 512:(blk + 1) * 512],
                                  lambda k0, k1: HTB, 8, bias_name="fb")
                        if blk > 0:
                            pb = blk - 1
                            pnr.part2(pb, 1, 1, lambda dc, pb=pb: XT[:, dc, pb * 512:(pb + 1) * 512], [XTB[pb]])
                    pnr.part2(3, 1, 1, lambda dc: XT[:, dc, 3 * 512:4 * 512], [XTB[3]])
                    S.barrier()
            dump("x3", XT[:], [128, 8, T], F32, XTB)
            stop("stop6")

            ffn(1)
            for blk in range(3, 4):
                S.dma("sp", out_v[:, :, blk * 512:(blk + 1) * 512], XT[:, :, blk * 512:(blk + 1) * 512],
                      reads=[XTB[blk]], writes=[OUTB])
            dbg_out["__out"] = OUTB
        except _Stop:
            pass
        S.stopped = False
        S.finish(list(dbg_out.values()))
    return nc, dbg_out


_CACHE = {}


def kernel(**inputs):
    sh = host_shared(inputs)
    in_maps = []
    for b in range(8):
        m = dict(sh)
        m.update(host_percore(inputs, b))
        in_maps.append(m)
    if "nc" not in _CACHE:
        _CACHE["nc"] = build()[0]
    res = run_bass_kernel_spmd(_CACHE["nc"], in_maps, core_ids=list(range(8)))
    out = np.stack([np.ascontiguousarray(res.results[b]["out"].T) for b in range(8)])
    return out.astype(np.float32)
```

```python
from contextlib import ExitStack
import numpy as np
import ml_dtypes
import concourse.bass as bass
import concourse.mybir as mybir
from concourse.bass_utils import run_bass_kernel_spmd

F32 = mybir.dt.float32
BF16 = mybir.dt.bfloat16
AF = mybir.ActivationFunctionType
ALU = mybir.AluOpType
AX = mybir.AxisListType

D = 1024
T = 2048
TC = 256
TT = T + TC
NCH = TT // 128
H = 4
DK = 128
DV = 256
FF = 2816
NF = FF // 128
EPS = 1e-6
BIG = 30000.0


class Buf:
    __slots__ = ("name", "w", "r", "dsem", "dcount", "excl")

    def __init__(self, name, excl=False):
        self.name = name
        self.excl = excl
        self.w = None
        self.r = {}
        self.dsem = None
        self.dcount = 0


class Sched:
    def __init__(self, nc, stack):
        self.nc = nc
        self.stack = stack
        self.eng = {}
        self.sems = {}
        for name, h in (("pe", nc.tensor), ("act", nc.scalar), ("dve", nc.vector),
                        ("pool", nc.gpsimd), ("sp", nc.sync)):
            sem = stack.enter_context(nc.semaphore("s_" + name))
            self.eng[name] = {"h": h, "sem": sem, "count": 0, "waited": {}}
            self.sems[name] = sem
        self.dma_bufs = {}
        self.nsem = 5
        self.nops = {k: 0 for k in self.eng}
        self.stopped = False
        self.rec = None

    def _wait(self, ename, tok):
        key, val = tok
        if key in self.dma_bufs:
            val = max(val, self.dma_bufs[key].dcount)
        if key == "pe" and ename == "pe":
            return
        e = self.eng[ename]
        if e["waited"].get(key, 0) >= val:
            return
        e["waited"][key] = val
        e["h"].wait_ge(self.sems[key], val)

    def _deps(self, ename, reads, writes):
        for b in reads:
            if b.w is not None:
                self._wait(ename, b.w)
            if b.excl:
                for k, v in b.r.items():
                    if k != ename:
                        self._wait(ename, (k, v))
        for b in writes:
            if b.w is not None:
                self._wait(ename, b.w)
            for k, v in b.r.items():
                self._wait(ename, (k, v))

    def _mark(self, tok, reads, writes):
        k, v = tok
        for b in reads:
            if b.r.get(k, 0) < v:
                b.r[k] = v
        for b in writes:
            b.w = tok
            b.r = {}

    def op(self, ename, fn, reads=(), writes=()):
        if self.stopped:
            return
        if self.rec is not None:
            self.rec.append(("op", ename, fn, list(reads), list(writes), {}))
            return
        e = self.eng[ename]
        self._deps(ename, reads, writes)
        inst = fn(e["h"])
        e["count"] += 1
        self.nops[ename] += 1
        inst.then_inc(e["sem"], 1)
        self._mark((ename, e["count"]), reads, writes)

    def dma(self, qname, out_ap, in_ap, reads=(), writes=(), **kw):
        if self.stopped:
            return
        if self.rec is not None:
            self.rec.append(("dma", qname, (out_ap, in_ap), list(reads), list(writes), kw))
            return
        e = self.eng[qname]
        self._deps(qname, reads, writes)
        dst = writes[0]
        if dst.dsem is None:
            dst.dsem = "d_" + dst.name
            self.sems[dst.dsem] = self.stack.enter_context(self.nc.semaphore(dst.dsem))
            self.dma_bufs[dst.dsem] = dst
            self.nsem += 1
        dst.dcount += 16
        e["h"].dma_start(out=out_ap, in_=in_ap, **kw).then_inc(self.sems[dst.dsem], 16)
        self._mark((dst.dsem, dst.dcount), reads, writes)

    def replay(self, rec, n):
        for _ in range(n):
            if not rec:
                return
            kind, en, x, r, w, kw = rec.pop(0)
            if kind == "barrier":
                self.barrier()
            elif kind == "op":
                self.op(en, x, r, w)
            else:
                self.dma(en, x[0], x[1], r, w, **kw)

    def barrier(self):
        if self.stopped:
            return
        if self.rec is not None:
            self.rec.append(("barrier", None, None, [], [], {}))
            return
        toks = [(n, e["count"]) for n, e in self.eng.items() if e["count"] > 0]
        toks += [(k, b.dcount) for k, b in self.dma_bufs.items()]
        for n in self.eng:
            for t in toks:
                if not (t[0] == "pe" and n == "pe"):
                    self._wait(n, t)

    def finish(self, out_bufs):
        for b in out_bufs:
            if b.w is not None:
                self._wait("sp", b.w)


class _Stop(Exception):
    pass


def V(base, dims):
    return bass.AP(base.tensor, base.offset, [base.ap[0]] + [list(d) for d in dims])


VOFF = {}
_o = 0
for _n, _w in (("adab0", 48), ("adab1", 48), ("premix", 16), ("postmix", 16), ("preffn", 16),
               ("postffn", 16), ("bq", 4), ("bk", 4), ("fb", 8), ("convw", 132), ("convb", 44),
               ("gbi", 1), ("gbf", 1), ("rsf", 1), ("rsb", 1), ("ngc", 8)):
    VOFF[_n] = _o
    _o += _w
NV = _o

C32 = {"maskf": 0, "maskb": 128, "id8": 256, "sel": 264}
NC32 = 264 + 8 * 128
CBF = {"ident": 0, "ones": 128, "dftd": 256}
NCBF = 512


def pchunk(w):
    K, N = w.shape
    return np.ascontiguousarray(w.reshape(K // 128, 128, N).transpose(1, 0, 2))


def colvec(v):
    return np.ascontiguousarray(v.reshape(-1, 128).T)


def host_shared(inp):
    f32 = np.float32
    sh = {}
    ada_w = np.asarray(inp["ada_w"], f32)
    sh["adaw"] = np.ascontiguousarray(ada_w.reshape(2, 8, 128, 6 * D).transpose(0, 2, 1, 3))
    vecs = np.zeros((128, NV), f32)
    ada_b = np.asarray(inp["ada_b"], f32)
    for l in range(2):
        vecs[:, VOFF["adab%d" % l]:VOFF["adab%d" % l] + 48] = colvec(ada_b[l])
    for nm, key in (("premix", "pre_mix_g"), ("postmix", "post_mix_g"), ("preffn", "pre_ffn_g"),
                    ("postffn", "post_ffn_g")):
        a = np.asarray(inp[key], f32)
        for l in range(2):
            vecs[:, VOFF[nm] + 8 * l:VOFF[nm] + 8 * l + 8] = colvec(a[l])
    mb = np.asarray(inp["m_in_b"], f32)[0]
    vecs[:, VOFF["bq"]:VOFF["bq"] + 4] = colvec(mb[0:512])
    vecs[:, VOFF["bk"]:VOFF["bk"] + 4] = colvec(mb[512:1024])
    vecs[:, VOFF["fb"]:VOFF["fb"] + 8] = colvec(np.asarray(inp["f_out_b"], f32)[0])
    cw = np.asarray(inp["ffn_conv_w"], f32)
    cb = np.asarray(inp["ffn_conv_b"], f32)
    for l in range(2):
        for j in range(3):
            o = VOFF["convw"] + (l * 3 + j) * 22
            vecs[:, o:o + 22] = colvec(cw[l, j])
        o = VOFF["convb"] + l * 22
        vecs[:, o:o + 22] = colvec(cb[l])
    gperm = [0, 1, 2, 3, 8, 9, 10, 11, 4, 5, 6, 7, 12, 13, 14, 15]
    gb = mb[3072:3088][gperm]
    vecs[0:8, VOFF["gbi"]] = gb[0:8]
    vecs[0:8, VOFF["gbf"]] = gb[8:16]
    vecs[0:4, VOFF["rsf"]] = 1.0
    vecs[4:8, VOFF["rsb"]] = 1.0
    vecs[:, VOFF["ngc"]:VOFF["ngc"] + 8] = colvec(np.asarray(inp["m_norm_g"], f32)[0])
    sh["vecs"] = vecs
    ng = np.asarray(inp["m_norm_g"], f32)[0]
    rv = np.zeros((H, 640), f32)
    for h in range(H):
        rv[h, 0:128] = mb[512 + h * 128:512 + (h + 1) * 128]
        rv[h, 128:384] = mb[1024 + h * 256:1024 + (h + 1) * 256]
        rv[h, 384:640] = mb[2048 + h * 256:2048 + (h + 1) * 256]
    sh["rowv"] = rv
    miw = np.asarray(inp["m_in_w"], f32)[0]
    whp = np.zeros((128, H, 8, 768), f32)
    for h in range(H):
        cols = np.concatenate([np.arange(h * 128, (h + 1) * 128), 512 + np.arange(h * 128, (h + 1) * 128),
                               1024 + np.arange(h * 256, (h + 1) * 256),
                               2048 + np.arange(h * 256, (h + 1) * 256)])
        whp[:, h] = pchunk(miw[:, cols])
    sh["whp"] = whp
    sh["wgp"] = pchunk(miw[:, 3072 + np.array(gperm)])
    sh["wmo"] = pchunk(np.asarray(inp["m_out_w"], f32)[0])
    sh["wfo"] = pchunk(np.asarray(inp["f_out_w"], f32)[0])
    up = np.asarray(inp["ffn_up_w"], f32)
    upp = np.zeros((2, 128, NF, 8, 256), f32)
    for l in range(2):
        pc = pchunk(up[l])
        upp[l, :, :, :, 0:128] = pc[:, :, 0:FF].reshape(128, 8, NF, 128).transpose(0, 2, 1, 3)
        upp[l, :, :, :, 128:256] = pc[:, :, FF:2 * FF].reshape(128, 8, NF, 128).transpose(0, 2, 1, 3)
    sh["upp"] = upp
    dn = np.asarray(inp["ffn_down_w"], f32)
    sh["dnp"] = np.stack([pchunk(dn[l]) for l in range(2)])
    c32 = np.zeros((128, NC32), f32)
    s_i = np.arange(128)[:, None]
    t_i = np.arange(128)[None, :]
    c32[:, C32["maskf"]:C32["maskf"] + 128] = np.where(s_i <= t_i, 0.0, BIG)
    c32[:, C32["maskb"]:C32["maskb"] + 128] = np.where(s_i >= t_i, 0.0, BIG)
    c32[0:8, C32["id8"]:C32["id8"] + 8] = np.eye(8)
    sel = np.zeros((8, 8, 128), f32)
    for j in range(8):
        sel[j, j, :] = 1.0
    c32[0:8, C32["sel"]:] = sel.reshape(8, 8 * 128)
    sh["c32"] = c32
    cbf = np.zeros((128, NCBF), f32)
    cbf[:, 0:128] = np.eye(128)
    cbf[:, 128:256] = 1.0
    dd = np.arange(128)
    ang = 2.0 * np.pi * np.outer(dd, dd) / 128.0
    cbf[:, 256:384] = np.cos(ang) / 512.0
    cbf[:, 384:512] = np.sin(ang) / 512.0
    sh["cbf"] = cbf.astype(ml_dtypes.bfloat16)
    p_i = np.arange(128)[:, None, None, None]
    par = np.arange(2)[None, :, None, None]
    ii = np.arange(8)[None, None, :, None]
    k1 = np.arange(1024)[None, None, None, :]
    tt = 2 * (ii * 128 + p_i) + par
    ph = (tt * k1) % 2048
    a2 = 2.0 * np.pi * ph.astype(np.float64) / 2048.0
    dft2 = np.stack([np.cos(a2), -np.sin(a2)], axis=1).astype(f32)
    sh["dft2"] = dft2.astype(ml_dtypes.bfloat16)
    return sh


def host_percore(inp, b):
    f32 = np.float32
    x = np.asarray(inp["x"], f32)[b]
    ctx = np.asarray(inp["ctx"], f32)[b]
    xc = np.ascontiguousarray(np.concatenate([ctx.T, x.T], axis=1))
    cv = np.zeros((128, 16), f32)
    cv[:, 0:8] = colvec(np.asarray(inp["c"], f32)[b])
    cv[:, 8:16] = colvec(np.asarray(inp["c_ctx"], f32))
    return {"xc": xc, "cv": cv}


def build(dbg=None):
    dbg = dbg or set()
    nc = bass.Bass("TRN2", target_bir_lowering=False)
    dram_in = lambda n, s, dt=F32: nc.dram_tensor(n, list(s), dt, kind="ExternalInput").ap()
    xc_d = dram_in("xc", [D, TT])
    cv_d = dram_in("cv", [128, 16])
    adaw_d = dram_in("adaw", [2, 128, 8, 6 * D])
    vecs_d = dram_in("vecs", [128, NV])
    rowv_d = dram_in("rowv", [H, 640])
    whp_d = dram_in("whp", [128, H, 8, 768])
    wgp_d = dram_in("wgp", [128, 8, 16])
    wmo_d = dram_in("wmo", [128, 8, D])
    wfo_d = dram_in("wfo", [128, 8, D])
    upp_d = dram_in("upp", [2, 128, NF, 8, 256])
    dnp_d = dram_in("dnp", [2, 128, NF, D])
    c32_d = dram_in("c32", [128, NC32])
    cbf_d = dram_in("cbf", [128, NCBF], BF16)
    dft2_d = dram_in("dft2", [128, 2, 2, 8, 1024], BF16)
    out_d = nc.dram_tensor("out", [D, T], F32, kind="ExternalOutput").ap()
    yt_d = nc.dram_tensor("yt_scr", [128, 8, T], BF16, kind="ExternalOutput").ap()
    dbg_out = {}

    xc_v = xc_d.rearrange("(k p) t -> p k t", p=128)
    out_v = out_d.rearrange("(k p) t -> p k t", p=128)

    with ExitStack() as st:
        S = Sched(nc, st)

        uid = [0]

        def sb(name, shape, dt, stack=st):
            uid[0] += 1
            return stack.enter_context(nc.sbuf_tensor("sb%d_%s" % (uid[0], name), list(shape), dt))

        XT = sb("XT", [128, 8, T], F32)
        XTf = XT[:].rearrange("p k t -> p (k t)")
        XTB = [Buf("XT%d" % i) for i in range(4)]
        vecs = sb("vecs", [128, NV], F32); VECS = Buf("vecs")
        c32 = sb("c32", [128, NC32], F32); C32B = Buf("c32")
        cbf = sb("cbf", [128, NCBF], BF16); CBFB = Buf("cbf")
        cv = sb("cv", [128, 16], F32); CVB = Buf("cv")
        scv = sb("scv", [128, 16], F32); SCVB = Buf("scv")
        mod = sb("mod", [128, 2, 48], F32); MODB = Buf("mod")
        cmod = sb("cmod", [128, 16], F32); CMODB = Buf("cmod")
        der = sb("der", [128, 2, 4, 8], F32); DERB = Buf("der")
        cder = sb("cder", [128, 8], F32); CDERB = Buf("cder")
        ident = cbf[:, 0:128]
        ones = cbf[:, 128:256]
        dftd = cbf[:, 256:512]

        PS = []
        psbs = [st.enter_context(nc.psum_tensor("psb%d" % i, [128, 1024], BF16)) for i in range(2)]
        NPS = 6
        for i in range(NPS):
            t = st.enter_context(nc.psum_tensor("ps%d" % i, [128, 512], F32))
            PS.append((t, Buf("ps%d" % i, excl=True)))
        ps_i = [0]

        def ps_next():
            r = PS[ps_i[0] % NPS]
            ps_i[0] += 1
            return r

        def vcol(name, i=0, n=1):
            return vecs[:, VOFF[name] + i:VOFF[name] + i + n]

        def stop(name):
            if name in dbg:
                S.barrier()
                S.stopped = True

        def dump(name, ap_sb, shape, dt, bufs):
            if name not in dbg:
                return
            d = nc.dram_tensor("dbg_" + name, list(shape), dt, kind="ExternalOutput").ap()
            B = Buf("dbg_" + name)
            S.dma("sp", d, ap_sb, reads=bufs, writes=[B])
            dbg_out[name] = B

        try:
            S.dma("sp", vecs[:], vecs_d[:, :], writes=[VECS])
            S.dma("sp", cv[:], cv_d[:, :], writes=[CVB])
            S.dma("sp", c32[:], c32_d[:, :], writes=[C32B])
            S.dma("sp", cbf[:], cbf_d[:, :], writes=[CBFB])

            scvb = sb("scvb", [128, 16], BF16); SCVBB = Buf("scvb")
            S.op("act", lambda e: e.activation(out=scv[:], in_=cv[:], func=AF.Silu), reads=[CVB], writes=[SCVB])
            S.op("dve", lambda e: e.tensor_copy(scvb[:], scv[:]), reads=[SCVB], writes=[SCVBB])
            ada_state = {"next_dma": 0, "next_pe": 0, "bufs": None}
            ada_items = [(l, nb) for l in range(2) for nb in range(24)]

            def ada_dma(n):
                ada, ADAB, modrow, MRB = ada_state["bufs"]
                if n >= len(ada_items):
                    return
                l, nb = ada_items[n]
                S.dma("pool", ada[n % 2][:], adaw_d[l, :, :, nb * 256:(nb + 1) * 256], writes=[ADAB[n % 2]])

            def ada_pe(n):
                ada, ADAB, modrow, MRB = ada_state["bufs"]
                l, nb = ada_items[n]
                bi = n % 2
                pt, PB = ps_next()

                def mm(e, pt=pt, bi=bi):
                    last = None
                    for kk in range(8):
                        last = e.matmul(pt[0:2, 0:256], V(scvb[:, kk:kk + 1], [[8, 2]]), ada[bi][:, kk, :],
                                        start=(kk == 0), stop=(kk == 7))
                    return last
                S.op("pe", mm, reads=[ADAB[bi], SCVBB], writes=[PB])
                mr, MB_ = modrow[n % 2], MRB[n % 2]
                S.op("act", lambda e, pt=pt, mr=mr: e.copy(mr[:, :], pt[0:2, 0:256]), reads=[PB], writes=[MB_])
                pt2, PB2 = ps_next()

                def mmT(e, pt2=pt2, mr=mr):
                    last = None
                    for c4 in range(2):
                        last = e.matmul(pt2[:, c4 * 2:c4 * 2 + 2], mr[:, c4 * 128:(c4 + 1) * 128],
                                        c32[0:2, C32["id8"]:C32["id8"] + 2], start=True, stop=True)
                    return last
                S.op("pe", mmT, reads=[MB_, C32B], writes=[PB2])
                S.op("dve", lambda e, pt2=pt2, l=l, nb=nb: e.tensor_tensor(
                    out=mod[:, l, nb * 2:nb * 2 + 2], in0=V(pt2[:, 0:1], [[2, 2]]),
                    in1=vcol("adab%d" % l, nb * 2, 2), op=ALU.add), reads=[PB2, VECS], writes=[MODB])
                if l == 0 and nb < 8:
                    S.op("dve", lambda e, pt2=pt2, nb=nb: e.tensor_tensor(
                        out=cmod[:, nb * 2:nb * 2 + 2], in0=V(pt2[:, 1:2], [[2, 2]]),
                        in1=vcol("adab0", nb * 2, 2), op=ALU.add), reads=[PB2, VECS], writes=[CMODB])

            def ada_more(k):
                for _ in range(k):
                    n = ada_state["next_pe"]
                    if n >= len(ada_items):
                        return
                    ada_pe(n)
                    ada_state["next_pe"] = n + 1
                    ada_dma(n + 2)
                if ada_state["next_pe"] == len(ada_items) and not ada_state.get("done"):
                    ada_state["done"] = True
                    for l in range(2):
                        S.op("dve", lambda e, l=l: e.tensor_tensor(
                            out=der[:, l, 1, :], in0=mod[:, l, 16:24], in1=vcol("postmix", 8 * l, 8), op=ALU.mult),
                            reads=[MODB, VECS], writes=[DERB])
                        S.op("dve", lambda e, l=l: e.scalar_tensor_tensor(
                            out=der[:, l, 2, :], in0=mod[:, l, 32:40], scalar=1.0, in1=vcol("preffn", 8 * l, 8),
                            op0=ALU.add, op1=ALU.mult), reads=[MODB, VECS], writes=[DERB])
                        S.op("dve", lambda e, l=l: e.tensor_tensor(
                            out=der[:, l, 3, :], in0=mod[:, l, 40:48], in1=vcol("postffn", 8 * l, 8), op=ALU.mult),
                            reads=[MODB, VECS], writes=[DERB])
                    S.op("dve", lambda e: e.scalar_tensor_tensor(
                        out=der[:, 1, 0, :], in0=mod[:, 1, 8:16], scalar=1.0, in1=vcol("premix", 8, 8),
                        op0=ALU.add, op1=ALU.mult), reads=[MODB, VECS], writes=[DERB])


            def rstd_from_sq(sq, SQB, nb, sd, SDB, rstd, RSB, ndiv=float(D)):
                pt, PB = ps_next()

                def mm(e):
                    last = None
                    for k in range(8):
                        last = e.matmul(pt[:, 0:nb], ones, sq[:, k, 0:nb], start=(k == 0), stop=(k == 7))
                    return last
                S.op("pe", mm, reads=[SQB, CBFB], writes=[PB])
                S.op("act", lambda e: e.activation(out=sd[:, 0:nb], in_=pt[:, 0:nb], func=AF.Ln,
                                                   bias=EPS, scale=1.0 / ndiv), reads=[PB], writes=[SDB])
                S.op("act", lambda e: e.activation(out=rstd[:, 0:nb], in_=sd[:, 0:nb], func=AF.Exp, scale=-0.5),
                     reads=[SDB], writes=[RSB])

            with ExitStack() as ph:
                ada = [sb("ada%d" % i, [128, 8, 256], BF16, ph) for i in range(2)]
                ADAB = [Buf("ada%d" % i) for i in range(2)]
                modrow = [sb("modrow%d" % i, [2, 256], F32, ph) for i in range(2)]
                MRB = [Buf("modrow%d" % i) for i in range(2)]
                ada_state["bufs"] = (ada, ADAB, modrow, MRB)
                for n in range(2):
                    ada_dma(n)
                ada_more(8)
                S.op("dve", lambda e: e.scalar_tensor_tensor(
                    out=der[:, 0, 0, :], in0=mod[:, 0, 8:16], scalar=1.0, in1=vcol("premix", 0, 8),
                    op0=ALU.add, op1=ALU.mult), reads=[MODB, VECS], writes=[DERB])
                S.op("dve", lambda e: e.scalar_tensor_tensor(
                    out=cder[:], in0=cmod[:, 8:16], scalar=1.0, in1=vcol("premix", 0, 8),
                    op0=ALU.add, op1=ALU.mult), reads=[CMODB, VECS], writes=[CDERB])
                hxT = sb("hxT", [128, 8, TT], BF16, ph)
                blocks = [(0, 256)] + [(256 + 512 * i, 512) for i in range(4)]
                HXB = [Buf("hx%d" % i) for i in range(5)]

                with ExitStack() as p1:
                    xb = [sb("xb%d" % i, [128, 8, 512], F32, p1) for i in range(2)]
                    XBB = [Buf("xb%d" % i) for i in range(2)]
                    sqs = [sb("sq1_%d" % i, [128, 8, 512], BF16, p1) for i in range(2)]
                    SQBS = [Buf("sq1_%d" % i) for i in range(2)]
                    sds = [sb("sd1_%d" % i, [128, 512], F32, p1) for i in range(2)]
                    SDBS = [Buf("sd1_%d" % i) for i in range(2)]
                    rstds = [sb("rstd1_%d" % i, [128, 512], F32, p1) for i in range(2)]
                    RSBS = [Buf("rstd1_%d" % i) for i in range(2)]
                    tmp = [sb("tmp1_%d" % i, [128, 512], F32, p1) for i in range(4)]
                    TMPB = [Buf("tmp1_%d" % i) for i in range(4)]
                    for bi, (t0, nb) in enumerate(blocks):
                        x_ = xb[bi % 2]; XB_ = XBB[bi % 2]
                        sq, SQB, sd, SDB, rstd, RSB = sqs[bi % 2], SQBS[bi % 2], sds[bi % 2], SDBS[bi % 2], rstds[bi % 2], RSBS[bi % 2]
                        S.dma("sp", x_[:, :, 0:nb], xc_v[:, :, t0:t0 + nb], writes=[XB_])
                        S.op("act", lambda e, x_=x_, nb=nb, sq=sq: e.activation(out=sq[:, :, 0:nb], in_=x_[:, :, 0:nb],
                                                                         func=AF.Square), reads=[XB_], writes=[SQB])
                        rstd_from_sq(sq, SQB, nb, sd, SDB, rstd, RSB)
                        for k in range(8):
                            tm = tmp[k % 4]; TB = TMPB[k % 4]
                            if bi == 0:
                                a_col, b_col, AB, BB = cder[:, k:k + 1], cmod[:, k:k + 1], CDERB, CMODB
                            else:
                                a_col, b_col, AB, BB = der[:, 0, 0, k:k + 1], mod[:, 0, k:k + 1], DERB, MODB
                            S.op("dve", lambda e, x_=x_, k=k, nb=nb, tm=tm, a_col=a_col, rstd=rstd: e.scalar_tensor_tensor(
                                out=tm[:, 0:nb], in0=x_[:, k, 0:nb], scalar=a_col, in1=rstd[:, 0:nb],
                                op0=ALU.mult, op1=ALU.mult), reads=[XB_, RSB, AB], writes=[TB])
                            if k % 2 == 0:
                                S.op("act", lambda e, k=k, nb=nb, t0=t0, tm=tm, b_col=b_col: e.activation(
                                    out=hxT[:, k, t0:t0 + nb], in_=tm[:, 0:nb], func=AF.Identity, bias=b_col, scale=1.0),
                                    reads=[TB, BB], writes=[HXB[bi]])
                            else:
                                S.op("dve", lambda e, k=k, nb=nb, t0=t0, tm=tm, b_col=b_col: e.tensor_scalar(
                                    out=hxT[:, k, t0:t0 + nb], in0=tm[:, 0:nb], scalar1=b_col, scalar2=None, op0=ALU.add),
                                    reads=[TB, BB], writes=[HXB[bi]])
                    S.barrier()
                dump("hxT", hxT[:], [128, 8, TT], BF16, HXB)

                stop("stop1")

                RW = [XTf[0:8, i * TT:(i + 1) * TT] for i in range(7)]
                RB = [Buf("row%d" % i) for i in range(7)]
                ucng = sb("ucng", [128, NCH, 16], F32, ph); UCB = Buf("ucng")
                tots = sb("tots", [8, 4], F32, ph); TOTB = Buf("tots")
                rsf = vecs[0:8, VOFF["rsf"]:VOFF["rsf"] + 1]
                rsb = vecs[0:8, VOFF["rsb"]:VOFF["rsb"] + 1]
                with ExitStack() as p2:
                    wg = sb("wg", [128, 8, 16], BF16, ph); WGB = Buf("wg")
                    S.rec = []
                    S.dma("pool", wg[:], wgp_d[:, :, :], writes=[WGB])
                    gblocks = [(i * 512, min(512, TT - i * 512)) for i in range(5)]
                    for (t0, nb) in gblocks:
                        bsel = [HXB[0], HXB[1]] if t0 == 0 else ([HXB[(t0 - 256) // 512 + 1]] + ([HXB[(t0 - 256) // 512 + 2]] if t0 + nb > 256 + ((t0 - 256) // 512 + 1) * 512 else []))
                        for gi in range(2):
                            pt, PB = ps_next()

                            def mm(e, pt=pt, t0=t0, nb=nb, gi=gi):
                                last = None
                                for k in range(8):
                                    last = e.matmul(pt[0:8, 0:nb], wg[:, k, gi * 8:gi * 8 + 8], hxT[:, k, t0:t0 + nb],
                                                    start=(k == 0), stop=(k == 7))
                                return last
                            S.op("pe", mm, reads=[WGB] + HXB, writes=[PB])
                            bcol = vecs[0:8, VOFF["gbi" if gi == 0 else "gbf"]:VOFF["gbi" if gi == 0 else "gbf"] + 1]
                            S.op("act", lambda e, pt=pt, t0=t0, nb=nb, gi=gi, bcol=bcol: e.activation(
                                out=RW[gi][:, t0:t0 + nb], in_=pt[0:8, 0:nb], func=AF.Identity, bias=bcol, scale=1.0),
                                reads=[PB, VECS], writes=[RB[gi]])
                    S.op("act", lambda e: e.activation(out=RW[1], in_=RW[1], func=AF.Exp, scale=-1.0),
                         reads=[RB[1]], writes=[RB[1]])
                    S.op("act", lambda e: e.activation(out=RW[1], in_=RW[1], func=AF.Ln, bias=1.0, scale=1.0),
                         reads=[RB[1]], writes=[RB[1]])
                    S.op("pool", lambda e: e.memset(RW[3], 0.0), writes=[RB[3]])
                    for (a, b) in ((0, TC), (TC, TT)):
                        S.op("dve", lambda e, a=a, b=b: e.tensor_tensor_scan(
                            out=RW[2][:, a:b], data0=RW[1][:, a:b], data1=RW[3][:, a:b], initial=0.0,
                            op0=ALU.add, op1=ALU.add), reads=[RB[1], RB[3]], writes=[RB[2]])
                    S.op("dve", lambda e: e.tensor_copy(tots[:, 0:1], RW[2][:, TC - 1:TC]), reads=[RB[2]], writes=[TOTB])
                    S.op("dve", lambda e: e.tensor_tensor(out=tots[:, 1:2], in0=RW[2][:, TC - 1:TC], in1=RW[2][:, TT - 1:TT],
                                                          op=ALU.add), reads=[RB[2]], writes=[TOTB])
                    S.op("dve", lambda e: e.tensor_copy(RW[4][:, 0:TC], RW[2][:, 0:TC]), reads=[RB[2]], writes=[RB[4]])
                    S.op("dve", lambda e: e.tensor_scalar(out=RW[4][:, TC:TT], in0=RW[2][:, TC:TT], scalar1=tots[:, 0:1],
                                                          scalar2=None, op0=ALU.add), reads=[RB[2], TOTB], writes=[RB[4]])
                    S.op("dve", lambda e: e.tensor_tensor(out=RW[5], in0=RW[1], in1=RW[2], op=ALU.subtract),
                         reads=[RB[1], RB[2]], writes=[RB[5]])
                    S.op("dve", lambda e: e.tensor_scalar(out=RW[5][:, 0:TC], in0=RW[5][:, 0:TC], scalar1=tots[:, 0:1],
                                                          scalar2=None, op0=ALU.add), reads=[RB[5], TOTB], writes=[RB[5]])
                    S.op("dve", lambda e: e.tensor_scalar(out=RW[5][:, TC:TT], in0=RW[5][:, TC:TT], scalar1=tots[:, 1:2],
                                                          scalar2=None, op0=ALU.add), reads=[RB[5], TOTB], writes=[RB[5]])
                    S.op("dve", lambda e: e.tensor_scalar(out=RW[4], in0=RW[4], scalar1=rsf, scalar2=None, op0=ALU.mult),
                         reads=[RB[4], VECS], writes=[RB[4]])
                    S.op("dve", lambda e: e.scalar_tensor_tensor(out=RW[4], in0=RW[5], scalar=rsb, in1=RW[4],
                                                                 op0=ALU.mult, op1=ALU.add),
                         reads=[RB[5], RB[4], VECS], writes=[RB[4]])
                    S.op("dve", lambda e: e.tensor_tensor(out=RW[0], in0=RW[0], in1=RW[4], op=ALU.add),
                         reads=[RB[0], RB[4]], writes=[RB[0]])
                    S.op("dve", lambda e: e.tensor_tensor_scan(out=RW[5], data0=RW[0], data1=RW[0], initial=0.0,
                                                               op0=ALU.max, op1=ALU.max), reads=[RB[0]], writes=[RB[5]])
                    cur, CURB = RW[0], RB[0]
                    pp = 0
                    sh = 1
                    while sh < TT - TC:
                        nxt, NXTB = RW[2 + pp], RB[2 + pp]
                        for (a, b) in ((0, TC), (TC, TT)):
                            n = b - a
                            if sh < n:
                                S.op("dve", lambda e, a=a, b=b, sh=sh, cur=cur, nxt=nxt: e.tensor_tensor(
                                    out=nxt[:, a:b - sh], in0=cur[:, a:b - sh], in1=cur[:, a + sh:b], op=ALU.max),
                                    reads=[CURB], writes=[NXTB])
                                S.op("pool", lambda e, a=a, b=b, sh=sh, cur=cur, nxt=nxt: e.tensor_copy(
                                    nxt[:, b - sh:b], cur[:, b - sh:b]), reads=[CURB], writes=[NXTB])
                            else:
                                S.op("pool", lambda e, a=a, b=b, cur=cur, nxt=nxt: e.tensor_copy(
                                    nxt[:, a:b], cur[:, a:b]), reads=[CURB], writes=[NXTB])
                        cur, CURB = nxt, NXTB
                        pp ^= 1
                        sh *= 2
                    sm, SMB = cur, CURB
                    S.op("dve", lambda e: e.tensor_scalar(out=sm[:, 0:TC], in0=sm[:, 0:TC], scalar1=0.0, scalar2=None,
                                                          op0=ALU.max), reads=[SMB], writes=[SMB])
                    S.op("dve", lambda e: e.tensor_copy(tots[:, 2:3], sm[:, 0:1]), reads=[SMB], writes=[TOTB])
                    S.op("dve", lambda e: e.tensor_scalar(out=sm[:, TC:TT], in0=sm[:, TC:TT], scalar1=tots[:, 2:3],
                                                          scalar2=None, op0=ALU.max), reads=[SMB, TOTB], writes=[SMB])
                    S.op("dve", lambda e: e.tensor_scalar(out=RW[5], in0=RW[5], scalar1=rsf, scalar2=None, op0=ALU.mult),
                         reads=[RB[5], VECS], writes=[RB[5]])
                    S.op("dve", lambda e: e.scalar_tensor_tensor(out=RW[5], in0=sm, scalar=rsb, in1=RW[5],
                                                                 op0=ALU.mult, op1=ALU.add),
                         reads=[SMB, RB[5], VECS], writes=[RB[5]])
                    S.op("dve", lambda e: e.tensor_tensor(out=RW[4], in0=RW[4], in1=RW[5], op=ALU.subtract),
                         reads=[RB[4], RB[5]], writes=[RB[4]])
                    pt, PB = ps_next()

                    def mmT(e, pt=pt):
                        last = None
                        for c in range(NCH):
                            e.matmul(pt[:, c * 16:c * 16 + 8], RW[0][:, c * 128:(c + 1) * 128],
                                     c32[0:8, C32["id8"]:C32["id8"] + 8], start=True, stop=True)
                            last = e.matmul(pt[:, c * 16 + 8:c * 16 + 16], RW[4][:, c * 128:(c + 1) * 128],
                                            c32[0:8, C32["id8"]:C32["id8"] + 8], start=True, stop=True)
                        return last
                    S.op("pe", mmT, reads=[RB[0], RB[4], C32B], writes=[PB])
                    S.op("dve", lambda e, pt=pt: e.tensor_copy(ucng[:].rearrange("p c j -> p (c j)"), pt[:, 0:NCH * 16]),
                         reads=[PB], writes=[UCB])
                    S.op("dve", lambda e: e.tensor_copy(RW[0], RW[5]), reads=[RB[5], RB[0]], writes=[RB[0]])
                    S.barrier()
                gate_rec = S.rec
                S.rec = None
                if "stop2a" in dbg or "ucng" in dbg:
                    S.replay(gate_rec, 10 ** 6)
                dump("ucng", ucng[:], [128, NCH, 16], F32, [UCB])
                stop("stop2a")
                MROW, MROWB = RW[0], RB[0]

                MbD = [XTf[:, TT + d * 2 * TT:2 * TT + d * 2 * TT] for d in range(2)]
                zzD = [XTf[:, 2 * TT + d * 2 * TT:3 * TT + d * 2 * TT] for d in range(2)]
                MBB = [Buf("Mb%d" % d) for d in range(2)]
                ZB = [Buf("zz%d" % d) for d in range(2)]
                Hh = XTf[:, 5 * TT:5 * TT + 4096]; HHB = Buf("Hh")
                Mb3D = [m.rearrange("p (c t) -> p c t", t=128) for m in MbD]
                zz3D = [z.rearrange("p (c t) -> p c t", t=128) for z in zzD]
                Hh3 = Hh.rearrange("p (c e) -> p c e", e=256)
                with ExitStack() as p3:
                    wh = [sb("wh%d" % i, [128, 8, 768], BF16, p3) for i in range(1)]
                    WHB = [Buf("wh%d" % i) for i in range(1)]
                    rowb = sb("rowb", [128, 640], F32, p3); ROWB = Buf("rowb")
                    QT = sb("QT", [128, TT], BF16, p3); QTB = Buf("QT")
                    KT = sb("KT", [128, TT], BF16, p3); KTB = Buf("KT")
                    KV = sb("KV", [128, NCH, 385], BF16, p3); KVB = Buf("KV")
                    Osig = sb("Osig", [128, 16, 256], BF16, p3); OSB = Buf("Osig")
                    otmp = [sb("otmp%d" % i, [128, 256], F32, p3) for i in range(2)]
                    OTB = [Buf("otmp%d" % i) for i in range(2)]
                    QW = [sb("QW%d" % d, [128, TT], BF16, p3) for d in range(2)]
                    QWB = [Buf("QW%d" % d) for d in range(2)]
                    Dj = [sb("Dj%d" % d, [128, NCH, 128], BF16, p3) for d in range(2)]
                    DJB = [Buf("Dj%d" % d) for d in range(2)]
                    KS = [sb("KS%d" % d, [128, NCH, 128], BF16, p3) for d in range(2)]
                    KSB = [Buf("KS%d" % d) for d in range(2)]
                    Cst = [[sb("Cst%d_%d" % (d, i), [128, 257], F32, p3) for i in range(2)] for d in range(2)]
                    CSTB = [[Buf("Cst%d_%d" % (d, i)) for i in range(2)] for d in range(2)]
                    Cbf = [[sb("Cbf%d_%d" % (d, i), [128, 257], BF16, p3) for i in range(2)] for d in range(2)]
                    CBFB2 = [[Buf("Cbf%d_%d" % (d, i)) for i in range(2)] for d in range(2)]
                    sm18 = [sb("sm18_%d" % d, [128, 6, NCH], F32, p3) for d in range(2)]
                    SM18 = [Buf("sm18_%d" % d) for d in range(2)]
                    Sp = [sb("Sp%d" % i, [128, 128], BF16, p3) for i in range(4)]
                    SPB = [Buf("Sp%d" % i) for i in range(4)]
                    dsm = sb("dsm", [128, 4, 4], F32, p3); DSMB = [Buf("dsm%d" % i) for i in range(4)]
                    ssq = sb("ssq", [128, 3, 16], F32, p3); SSQB = Buf("ssq")
                    yh, YHB = Osig, OSB
                    ytb = [sb("ytb%d" % i, [128, 512], BF16, p3) for i in range(2)]
                    YTBB = [Buf("ytb%d" % i) for i in range(2)]
                    PSBH = [Buf("psb0", excl=True), Buf("psb1", excl=True)]
                    YTD = Buf("ytd")
                    qscale = float(DK) ** -0.5
                    dctr = [0]
                    spctr = [0]
                    S.dma("pool", wh[0][:], whp_d[:, 0, :, :], writes=[WHB[0]])
                    S.op("pool", lambda e: e.memset(KV[:, :, 384:385], 1.0), writes=[KVB])
                    junk = sb("junk", [128, 256], BF16, p3); JUNKB = Buf("junk")
                    pending_readout = []
                    pending_tr = []
                    ro_thunks = []

                    def readout(h):
                        for c in range(16):
                            S.op("act", lambda e, c=c: e.activation(out=junk[:, :], in_=Hh3[:, c, :], func=AF.Square,
                                                                    accum_out=ssq[:, 0, c:c + 1]),
                                 reads=[HHB], writes=[JUNKB, SSQB])
                        S.op("act", lambda e: e.activation(out=ssq[:, 1, :], in_=ssq[:, 0, :], func=AF.Sqrt, bias=EPS,
                                                           scale=1.0 / DV), reads=[SSQB], writes=[SSQB])
                        S.op("dve", lambda e: e.reciprocal(out=ssq[:, 2, :], in_=ssq[:, 1, :]), reads=[SSQB], writes=[SSQB])
                        for c in range(16):
                            ro_thunks.append(lambda c=c: S.op("dve", lambda e: e.scalar_tensor_tensor(
                                out=yh[:, c, :], in0=Hh3[:, c, :], scalar=ssq[:, 2, c:c + 1], in1=Osig[:, c, :],
                                op0=ALU.mult, op1=ALU.mult), reads=[HHB, SSQB, OSB], writes=[OSB]))
                        pending_tr.append(h)

                    def readout_tr(h):
                        tctr = 0
                        for i in range(2):
                            for cg in range(4):
                                hb = tctr % 2
                                tctr += 1

                                def tr(e, i=i, cg=cg, hb=hb):
                                    last = None
                                    for q in range(4):
                                        last = e.transpose(psbs[hb][:, q * 128:(q + 1) * 128],
                                                           yh[:, cg * 4 + q, i * 128:(i + 1) * 128], ident)
                                    return last
                                S.op("pe", tr, reads=[YHB, CBFB], writes=[PSBH[hb]])
                                S.op("act", lambda e, hb=hb: e.copy(ytb[hb][:, :], psbs[hb][:, 0:512]),
                                     reads=[PSBH[hb]], writes=[YTBB[hb]])
                                S.dma("sp", yt_d[:, 2 * h + i, cg * 512:(cg + 1) * 512], ytb[hb][:, :],
                                      reads=[YTBB[hb]], writes=[YTD])

                    orders = [list(range(NCH)), [1, 0] + list(range(NCH - 1, 1, -1))]
                    mcols = [C32["maskf"], C32["maskb"]]
                    for h in range(H):
                        whh, WHH = wh[0], WHB[0]
                        S.dma("sp", rowb[:], bass.AP(rowv_d.tensor, rowv_d[h:h + 1, :].offset, [[0, 128], [1, 640]]),
                              writes=[ROWB])
                        def emit_mb_prep(h=h):
                            for d in range(2):
                                j = d * 4 + h
                                Mb, Mb3 = MbD[d], Mb3D[d]
                                RN, RC, AL, WS, EE, T18 = [sm18[d][:, i, :] for i in range(6)]
                                SMB = SM18[d]
                                for (t0, nb) in gblocks:
                                    pt, PB = ps_next()
                                    S.op("pe", lambda e, pt=pt, t0=t0, nb=nb, j=j: e.matmul(
                                        pt[:, 0:nb], c32[0:8, C32["sel"] + j * 128:C32["sel"] + (j + 1) * 128],
                                        MROW[:, t0:t0 + nb], start=True, stop=True), reads=[C32B, MROWB], writes=[PB])
                                    S.op("act", lambda e, pt=pt, t0=t0, nb=nb, Mb=Mb: e.copy(Mb[:, t0:t0 + nb], pt[:, 0:nb]),
                                         reads=[PB], writes=[MBB[d]])
                                ucj = ucng[:, :, j]
                                ngj = ucng[:, :, 8 + j]
                                if d == 0:
                                    S.op("dve", lambda e, RN=RN, Mb=Mb: e.tensor_copy(RN, V(Mb[:, 127:128], [[128, NCH]])),
                                         reads=[MBB[d]], writes=[SMB])
                                    S.op("pool", lambda e, RC=RC: e.memset(RC[:, 0:1], 0.0), writes=[SMB])
                                    S.op("dve", lambda e, RN=RN, RC=RC: e.tensor_copy(RC[:, 1:NCH], RN[:, 0:NCH - 1]),
                                         reads=[SMB], writes=[SMB])
                                else:
                                    S.op("dve", lambda e, RN=RN, Mb=Mb: e.tensor_copy(RN, V(Mb[:, 0:1], [[128, NCH]])),
                                         reads=[MBB[d]], writes=[SMB])
                                    S.op("pool", lambda e, RC=RC: e.memset(RC[:, 1:2], 0.0), writes=[SMB])
                                    S.op("dve", lambda e, RN=RN, RC=RC: e.tensor_copy(RC[:, 0:1], RN[:, 1:2]), reads=[SMB], writes=[SMB])
                                    S.op("dve", lambda e, RN=RN, RC=RC: e.tensor_copy(RC[:, 2:17], RN[:, 3:18]), reads=[SMB], writes=[SMB])
                                    S.op("dve", lambda e, RN=RN, RC=RC: e.tensor_copy(RC[:, 17:18], RN[:, 0:1]), reads=[SMB], writes=[SMB])
                                S.op("dve", lambda e, AL=AL, RC=RC, RN=RN: e.tensor_tensor(out=AL, in0=RC, in1=RN, op=ALU.subtract),
                                     reads=[SMB], writes=[SMB])
                                S.op("dve", lambda e, WS=WS, ucj=ucj, RN=RN: e.tensor_tensor(out=WS, in0=ucj, in1=RN, op=ALU.subtract),
                                     reads=[SMB, UCB], writes=[SMB])
                                S.op("act", lambda e, AL=AL: e.activation(out=AL, in_=AL, func=AF.Exp), reads=[SMB], writes=[SMB])
                                S.op("act", lambda e, WS=WS: e.activation(out=WS, in_=WS, func=AF.Exp), reads=[SMB], writes=[SMB])
                                S.op("act", lambda e, EE=EE, ngj=ngj: e.activation(out=EE, in_=ngj, func=AF.Exp), reads=[UCB], writes=[SMB])

                        if h > 0:
                            emit_mb_prep()
                        for (t0, nb) in gblocks:
                            for qi in range(2):
                                pt, PB = ps_next()

                                def mm(e, pt=pt, t0=t0, nb=nb, qi=qi, whh=whh):
                                    last = None
                                    for k in range(8):
                                        last = e.matmul(pt[:, 0:nb], whh[:, k, qi * 128:(qi + 1) * 128], hxT[:, k, t0:t0 + nb],
                                                        start=(k == 0), stop=(k == 7))
                                    return last
                                S.op("pe", mm, reads=[WHH] + HXB, writes=[PB])
                                if qi == 0:
                                    S.op("dve", lambda e, pt=pt, t0=t0, nb=nb, h=h: e.tensor_scalar(
                                        out=QT[:, t0:t0 + nb], in0=pt[:, 0:nb], scalar1=vcol("bq", h), scalar2=qscale,
                                        op0=ALU.add, op1=ALU.mult), reads=[PB, VECS], writes=[QTB])
                                else:
                                    S.op("act", lambda e, pt=pt, t0=t0, nb=nb, h=h: e.activation(
                                        out=KT[:, t0:t0 + nb], in_=pt[:, 0:nb], func=AF.Identity, bias=vcol("bk", h),
                                        scale=1.0), reads=[PB, VECS], writes=[KTB])
                                if h == 0:
                                    S.replay(gate_rec, 3)
                        while pending_readout:
                            readout(pending_readout.pop(0))
                        thunks = []
                        for d in range(2):
                            j = d * 4 + h
                            Mb3, zz, zz3 = Mb3D[d], zzD[d], zz3D[d]
                            RN, RC, AL, WS, EE, T18 = [sm18[d][:, i, :] for i in range(6)]
                            ucj = ucng[:, :, j]
                            thunks.append(lambda d=d, zz3=zz3, Mb3=Mb3, RC=RC: S.op("dve", lambda e: e.tensor_tensor(
                                out=zz3, in0=Mb3, in1=V(RC[:, 0:1], [[1, NCH], [0, 128]]), op=ALU.subtract),
                                reads=[MBB[d], SM18[d]], writes=[ZB[d]]))
                            thunks.append(lambda d=d, zz=zz: S.op("act", lambda e: e.activation(
                                out=zz, in_=zz, func=AF.Exp, scale=-1.0), reads=[ZB[d]], writes=[ZB[d]]))
                            thunks.append(lambda d=d, zz=zz: S.op("dve", lambda e: e.tensor_tensor(
                                out=QW[d][:, :], in0=QT[:, :], in1=zz, op=ALU.mult), reads=[QTB, ZB[d]], writes=[QWB[d]]))
                            thunks.append(lambda d=d, zz3=zz3, Mb3=Mb3, ucj=ucj: S.op("dve", lambda e: e.tensor_tensor(
                                out=zz3, in0=Mb3, in1=V(ucj[:, 0:1], [[16, NCH], [0, 128]]), op=ALU.subtract),
                                reads=[MBB[d], UCB, ZB[d]], writes=[ZB[d]]))
                            thunks.append(lambda d=d, zz3=zz3: S.op("dve", lambda e: e.tensor_tensor(
                                out=zz3, in0=zz3, in1=V(c32[:, mcols[d]:mcols[d] + 1], [[0, NCH], [1, 128]]), op=ALU.add),
                                reads=[ZB[d], C32B], writes=[ZB[d]]))
                            thunks.append(lambda d=d, zz=zz: S.op("act", lambda e: e.activation(
                                out=Dj[d][:].rearrange("p c t -> p (c t)"), in_=zz, func=AF.Exp, scale=-1.0),
                                reads=[ZB[d]], writes=[DJB[d]]))
                        thunks = [t for pair in zip(thunks[0:6], thunks[6:12]) for t in pair]
                        for c in range(NCH):
                            pt, PB = ps_next()

                            def mm(e, pt=pt, c=c, whh=whh):
                                last = None
                                for k in range(8):
                                    last = e.matmul(pt[:, 0:384], hxT[:, k, c * 128:(c + 1) * 128], whh[:, k, 128:512],
                                                    start=(k == 0), stop=(k == 7))
                                return last
                            S.op("pe", mm, reads=[WHH] + HXB, writes=[PB])
                            S.op("dve", lambda e, pt=pt, c=c: e.tensor_tensor(
                                out=KV[:, c, 0:384], in0=pt[:, 0:384], in1=rowb[:, 0:384], op=ALU.add),
                                reads=[PB, ROWB], writes=[KVB])
                            if h == 0:
                                S.replay(gate_rec, 3)
                            elif ro_thunks:
                                ro_thunks.pop(0)()
                            elif thunks:
                                thunks.pop(0)()
                        while ro_thunks:
                            ro_thunks.pop(0)()
                        while pending_tr:
                            readout_tr(pending_tr.pop(0))
                        for c in range(2, NCH):
                            pt2, PB2 = ps_next()

                            def mm2(e, pt2=pt2, c=c, whh=whh):
                                last = None
                                for k in range(8):
                                    last = e.matmul(pt2[:, 0:256], hxT[:, k, c * 128:(c + 1) * 128], whh[:, k, 512:768],
                                                    start=(k == 0), stop=(k == 7))
                                return last
                            S.op("pe", mm2, reads=[WHH] + HXB, writes=[PB2])
                            ot, OB_ = otmp[c % 2], OTB[c % 2]
                            S.op("dve", lambda e, pt2=pt2, ot=ot: e.tensor_tensor(
                                out=ot[:, :], in0=pt2[:, 0:256], in1=rowb[:, 384:640], op=ALU.add),
                                reads=[PB2, ROWB], writes=[OB_])
                            S.op("act", lambda e, ot=ot, c=c: e.activation(out=Osig[:, c - 2, :], in_=ot[:, :],
                                                                           func=AF.Sigmoid), reads=[OB_], writes=[OSB])
                            if h == 0:
                                S.replay(gate_rec, 3)
                            elif thunks:
                                thunks.pop(0)()
                        if h == 0:
                            S.replay(gate_rec, 10 ** 6)
                            emit_mb_prep()
                        while thunks:
                            thunks.pop(0)()
                        for d in range(2):
                            WS = sm18[d][:, 3, :]
                            S.op("dve", lambda e, d=d, WS=WS: e.tensor_tensor(
                                out=KS[d][:], in0=KV[:, :, 0:128], in1=V(WS[:, 0:1], [[1, NCH], [0, 128]]), op=ALU.mult),
                                reads=[KVB, SM18[d]], writes=[KSB[d]])
                        if h + 1 < H:
                            S.dma("pool", wh[0][:], whp_d[:, h + 1, :, :], writes=[WHB[0]])
                        ada_more(1)
                        if h == 0:
                            stop("stop2p")
                        touched = set()
                        cur = [0, 0]
                        for idx in range(NCH):
                            if idx % 2 == 1:
                                ada_more(1)
                            work = []
                            for d in range(2):
                                c = orders[d][idx]
                                AL, EE = sm18[d][:, 2, :], sm18[d][:, 4, :]
                                it = {"d": d, "c": c, "AL": AL, "EE": EE}
                                if c >= 2:
                                    pts, PSB_ = ps_next()
                                    S.op("pe", lambda e, pts=pts, c=c: e.matmul(
                                        pts[:, 0:128], KT[:, c * 128:(c + 1) * 128], QT[:, c * 128:(c + 1) * 128],
                                        start=True, stop=True), reads=[KTB, QTB], writes=[PSB_])
                                    it["pts"], it["PSB"] = pts, PSB_
                                if idx < NCH - 1:
                                    ptu, PUB = ps_next()
                                    S.op("pe", lambda e, ptu=ptu, c=c, d=d: e.matmul(
                                        ptu[:, 0:257], KS[d][:, c, :], KV[:, c, 128:385], start=True, stop=True),
                                        reads=[KSB[d], KVB], writes=[PUB])
                                    it["ptu"], it["PUB"] = ptu, PUB
                                work.append(it)
                            for it in work:
                                d, c = it["d"], it["c"]
                                if c >= 2:
                                    pts, PSB_ = it["pts"], it["PSB"]
                                    si = spctr[0] % 4
                                    spctr[0] += 1
                                    sp_, SB_ = Sp[si], SPB[si]
                                    S.op("dve", lambda e, pts=pts, c=c, sp_=sp_, d=d: e.tensor_tensor(
                                        out=sp_[:, :], in0=pts[:, 0:128], in1=Dj[d][:, c, :], op=ALU.mult),
                                        reads=[PSB_, DJB[d]], writes=[SB_])
                                    pto, POB = ps_next()
                                    cb_, CB_ = Cbf[d][cur[d]], CBFB2[d][cur[d]]

                                    def mmo(e, pto=pto, c=c, sp_=sp_, d=d, cb_=cb_):
                                        e.matmul(pto[:, 0:257], sp_[:, :], KV[:, c, 128:385], start=True, stop=False)
                                        return e.matmul(pto[:, 0:257], QW[d][:, c * 128:(c + 1) * 128], cb_[:, :],
                                                        start=False, stop=True)
                                    S.op("pe", mmo, reads=[SB_, KVB, QWB[d], CB_], writes=[POB])
                                    it["pto"], it["POB"] = pto, POB
                            for it in work:
                                d, c = it["d"], it["c"]
                                if idx < NCH - 1:
                                    ptu, PUB = it["ptu"], it["PUB"]
                                    co, cn = cur[d], 1 - cur[d]
                                    if idx == 0:
                                        S.op("dve", lambda e, ptu=ptu, d=d, cn=cn: e.tensor_copy(Cst[d][cn][:, :], ptu[:, 0:257]),
                                             reads=[PUB], writes=[CSTB[d][cn]])
                                    else:
                                        S.op("dve", lambda e, ptu=ptu, d=d, co=co, cn=cn, c=c, AL=it["AL"]: e.scalar_tensor_tensor(
                                            out=Cst[d][cn][:, :], in0=Cst[d][co][:, :], scalar=AL[:, c:c + 1], in1=ptu[:, 0:257],
                                            op0=ALU.mult, op1=ALU.add), reads=[PUB, CSTB[d][co], SM18[d]], writes=[CSTB[d][cn]])
                                    S.op("act", lambda e, d=d, cn=cn: e.copy(Cbf[d][cn][:, :], Cst[d][cn][:, :]),
                                         reads=[CSTB[d][cn]], writes=[CBFB2[d][cn]])
                                    cur[d] = cn
                                if c >= 2:
                                    pto, POB = it["pto"], it["POB"]
                                    di = dctr[0] % 4
                                    dctr[0] += 1
                                    dd_, DB_ = dsm[:, di, :], DSMB[di]
                                    EE = it["EE"]
                                    S.op("act", lambda e, pto=pto, dd_=dd_: e.activation(out=dd_[:, 0:1], in_=pto[:, 256:257],
                                                                                         func=AF.Abs), reads=[POB], writes=[DB_])
                                    S.op("dve", lambda e, dd_=dd_, c=c, EE=EE: e.tensor_tensor(
                                        out=dd_[:, 1:2], in0=dd_[:, 0:1], in1=EE[:, c:c + 1], op=ALU.max),
                                        reads=[DB_, SM18[d]], writes=[DB_])
                                    S.op("dve", lambda e, dd_=dd_: e.reciprocal(out=dd_[:, 2:3], in_=dd_[:, 1:2]),
                                         reads=[DB_], writes=[DB_])
                                    if c not in touched:
                                        touched.add(c)
                                        S.op("dve", lambda e, pto=pto, dd_=dd_, c=c: e.tensor_scalar(
                                            out=Hh3[:, c - 2, :], in0=pto[:, 0:256], scalar1=dd_[:, 2:3], scalar2=None,
                                            op0=ALU.mult), reads=[POB, DB_], writes=[HHB])
                                    else:
                                        S.op("dve", lambda e, pto=pto, dd_=dd_, c=c: e.scalar_tensor_tensor(
                                            out=Hh3[:, c - 2, :], in0=pto[:, 0:256], scalar=dd_[:, 2:3], in1=Hh3[:, c - 2, :],
                                            op0=ALU.mult, op1=ALU.add), reads=[POB, DB_, HHB], writes=[HHB])
                        if h == 0:
                            stop("stop2o")
                        pending_readout.append(h)
                        if h == 0:
                            stop("stop2h")
                    while pending_readout:
                        readout(pending_readout.pop(0))
                    while ro_thunks:
                        ro_thunks.pop(0)()
                    while pending_tr:
                        readout_tr(pending_tr.pop(0))
                    stop("stop2z")
                    S.barrier()

            def mk_pnr(ph_, tag, shared=None, nbuf=1):
                sets = []
                for i in range(nbuf):
                    yo_ = sb("yo%s%d" % (tag, i), [128, 8, 512], F32, ph_)
                    YOBS_ = [Buf("yo%s%d_%d" % (tag, i, j)) for j in range(8)]
                    if shared is None:
                        sq_ = sb("sqo%s%d" % (tag, i), [128, 8, 512], BF16, ph_); SQB_ = Buf("sqo%s%d" % (tag, i))
                        sd_ = sb("sdo%s%d" % (tag, i), [128, 512], F32, ph_); SDB_ = Buf("sdo%s%d" % (tag, i))
                        rstd_ = sb("rso%s%d" % (tag, i), [128, 512], F32, ph_); RSB_ = Buf("rso%s%d" % (tag, i))
                    else:
                        sq_, SQB_, sd_, SDB_, rstd_, RSB_ = shared
                    sets.append((yo_, YOBS_, sq_, SQB_, sd_, SDB_, rstd_, RSB_))

                def part1(blk, wsb, WBs, rhs_fn, rhs_bufs_fn, nk, bias_name=None, split_tail=0):
                    yo, YOBS, sq, SQB, sd, SDB, rstd, RSB = sets[blk % nbuf]
                    for dc in range(8):
                        pt, PB = ps_next()
                        segs = [(0, nk)]
                        if dc == 0 and split_tail:
                            segs = [(0, nk - split_tail), (nk - split_tail, nk)]
                        for (k0, k1) in segs:
                            def mm(e, pt=pt, dc=dc, k0=k0, k1=k1):
                                last = None
                                for k in range(k0, k1):
                                    last = e.matmul(pt[:, :], wsb[:, k, dc * 128:(dc + 1) * 128], rhs_fn(k),
                                                    start=(k == 0), stop=(k == nk - 1))
                                return last
                            S.op("pe", mm, reads=WBs + rhs_bufs_fn(k0, k1), writes=[PB])
                        YOB = YOBS[dc]
                        if bias_name is None:
                            S.op("dve", lambda e, pt=pt, dc=dc: e.tensor_copy(yo[:, dc, :], pt[:, :]), reads=[PB], writes=[YOB])
                            S.op("act", lambda e, pt=pt, dc=dc: e.activation(out=sq[:, dc, :], in_=pt[:, :], func=AF.Square),
                                 reads=[PB], writes=[SQB])
                        else:
                            S.op("dve", lambda e, pt=pt, dc=dc: e.tensor_scalar(
                                out=yo[:, dc, :], in0=pt[:, :], scalar1=vcol(bias_name, dc), scalar2=None, op0=ALU.add),
                                reads=[PB, VECS], writes=[YOB])
                            S.op("act", lambda e, pt=pt, dc=dc: e.activation(out=sq[:, dc, :], in_=pt[:, :], func=AF.Square,
                                                                             bias=vcol(bias_name, dc), scale=1.0),
                                 reads=[PB, VECS], writes=[SQB])

                def part2(blk, gate_idx, layer, xr, XRB):
                    yo, YOBS, sq, SQB, sd, SDB, rstd, RSB = sets[blk % nbuf]
                    rstd_from_sq(sq, SQB, 512, sd, SDB, rstd, RSB)
                    for dc in range(8):
                        YOB = YOBS[dc]
                        S.op("dve", lambda e, dc=dc: e.scalar_tensor_tensor(
                            out=yo[:, dc, :], in0=yo[:, dc, :], scalar=der[:, layer, gate_idx, dc:dc + 1], in1=rstd[:, :],
                            op0=ALU.mult, op1=ALU.mult), reads=[YOB, DERB, RSB], writes=[YOB])
                        S.op("dve", lambda e, dc=dc: e.tensor_tensor(
                            out=XT[:, dc, blk * 512:(blk + 1) * 512], in0=yo[:, dc, :], in1=xr(dc), op=ALU.add),
                            reads=[YOB] + XRB, writes=[XTB[blk]])

                def run(blk, wsb, WBs, rhs_fn, rhs_bufs, nk, gate_idx, layer, xr, XRB, bias_name=None):
                    part1(blk, wsb, WBs, rhs_fn, lambda k0, k1: rhs_bufs, nk, bias_name)
                    part2(blk, gate_idx, layer, xr, XRB)
                run.part1 = part1
                run.part2 = part2
                return run

            with ExitStack() as ph:
                wmo = sb("wmo", [128, 8, D], BF16, ph); WMOB = Buf("wmo")
                S.dma("pool", wmo[:], wmo_d[:, :, :], writes=[WMOB])
                for k in range(8):
                    S.op("dve", lambda e, k=k: e.tensor_scalar(out=wmo[:, k, :], in0=wmo[:, k, :], scalar1=vcol("ngc", k),
                                                                scalar2=None, op0=ALU.mult), reads=[WMOB, VECS], writes=[WMOB])
                ytl = [sb("ytl%d" % i, [128, 8, 512], BF16, ph) for i in range(2)]
                YTLB = [Buf("ytl%d" % i) for i in range(2)]
                xrs = [sb("xrs%d" % i, [128, 8, 512], F32, ph) for i in range(2)]
                XRSB = [Buf("xrs%d" % i) for i in range(2)]

                pnr = mk_pnr(ph, "3", nbuf=2)
                for blk in range(4):
                    yb, YB_ = ytl[blk % 2], YTLB[blk % 2]
                    xb_, XB_ = xrs[blk % 2], XRSB[blk % 2]
                    S.dma("sp", yb[:], yt_d[:, :, blk * 512:(blk + 1) * 512], reads=[YTD], writes=[YB_])
                    pnr.part1(blk, wmo, [WMOB], lambda k, yb=yb: yb[:, k, :], lambda k0, k1, YB_=YB_: [YB_], 8)
                    if blk > 0:
                        pb = blk - 1
                        pnr.part2(pb, 1, 0, lambda dc, xq=xrs[pb % 2]: xq[:, dc, :], [XRSB[pb % 2]])
                    S.dma("sp", xb_[:], xc_v[:, :, TC + blk * 512:TC + (blk + 1) * 512], writes=[XB_])
                pnr.part2(3, 1, 0, lambda dc, xq=xrs[1]: xq[:, dc, :], [XRSB[1]])
                S.barrier()
            dump("x1", XT[:], [128, 8, T], F32, XTB)
            stop("stop3")


            def pre_norm_sq(blk, sq, SQB):
                S.op("act", lambda e: e.activation(out=sq[:, :, :], in_=XT[:, :, blk * 512:(blk + 1) * 512],
                                                   func=AF.Square), reads=[XTB[blk]], writes=[SQB])

            def pre_norm_rest(blk, layer, a_idx, b_off, sq, SQB, sd, SDB, rstd, RSB, tmp, TMPB, dst_fn, DSTB_fn):
                rstd_from_sq(sq, SQB, 512, sd, SDB, rstd, RSB)
                for k in range(8):
                    tm, TB = tmp[k % 2], TMPB[k % 2]
                    S.op("dve", lambda e, k=k, tm=tm: e.scalar_tensor_tensor(
                        out=tm[:, :], in0=XT[:, k, blk * 512:(blk + 1) * 512], scalar=der[:, layer, a_idx, k:k + 1],
                        in1=rstd[:, :], op0=ALU.mult, op1=ALU.mult), reads=[XTB[blk], RSB, DERB], writes=[TB])
                    if k % 2 == 0:
                        S.op("act", lambda e, k=k, tm=tm: e.activation(
                            out=dst_fn(k), in_=tm[:, :], func=AF.Identity, bias=mod[:, layer, b_off + k:b_off + k + 1],
                            scale=1.0), reads=[TB, MODB], writes=DSTB_fn(k))
                    else:
                        S.op("dve", lambda e, k=k, tm=tm: e.tensor_scalar(
                            out=dst_fn(k), in0=tm[:, :], scalar1=mod[:, layer, b_off + k:b_off + k + 1], scalar2=None,
                            op0=ALU.add), reads=[TB, MODB], writes=DSTB_fn(k))

            def pre_norm_block(blk, layer, a_idx, b_off, sq, SQB, sd, SDB, rstd, RSB, tmp, TMPB, dst_fn, DSTB_fn):
                pre_norm_sq(blk, sq, SQB)
                pre_norm_rest(blk, layer, a_idx, b_off, sq, SQB, sd, SDB, rstd, RSB, tmp, TMPB, dst_fn, DSTB_fn)

            OUTB = Buf("outd")

            def ffn(l):
                with ExitStack() as ph:
                    wdn = sb("wdn", [128, NF, D], BF16, ph)
                    WDNB = [Buf("wdn%d_%d" % (l, i)) for i in range(2)]
                    h2T = [sb("h2T%d" % i, [128, 8, 512], BF16, ph) for i in range(2)]
                    H2B = [Buf("h2T%d_%d" % (l, i)) for i in range(2)]
                    aT = sb("aT", [128, NF, 512], BF16, ph); ATBS = [Buf("aT%d_%d" % (l, i)) for i in range(NF)]
                    wup = [sb("wup%d" % i, [128, 8, 256], BF16, ph) for i in range(4)]
                    WUPB = [Buf("wup%d_%d" % (l, i)) for i in range(4)]
                    sq = sb("sqf", [128, 8, 512], BF16, ph); SQB = Buf("sqf%d" % l)
                    sd = sb("sdf", [128, 512], F32, ph); SDB = Buf("sdf%d" % l)
                    rstd = sb("rsf", [128, 512], F32, ph); RSB = Buf("rsf%d" % l)
                    gc = [sb("gc%d" % i, [128, 512], F32, ph) for i in range(2)]
                    GCB = [Buf("gc%d_%d" % (l, i)) for i in range(2)]
                    tmp, TMPB = gc, GCB
                    sg = [sb("sg%d" % i, [128, 512], F32, ph) for i in range(2)]
                    SGB = [Buf("sg%d_%d" % (l, i)) for i in range(2)]
                    pnr = mk_pnr(ph, "f", shared=(sq, SQB, sd, SDB, rstd, RSB))
                    wctr = 0
                    for blk in range(4):
                        hb, HB_ = h2T[blk % 2], H2B[blk % 2]
                        if blk == 0:
                            pre_norm_block(0, l, 2, 24, sq, SQB, sd, SDB, rstd, RSB, tmp, TMPB,
                                           lambda k, hb=hb: hb[:, k, :], lambda k, HB_=HB_: [HB_])
                        for f in range(NF):
                            if f == 2 and blk > 0:
                                pnr.part2(blk - 1, 3, l, lambda dc, b_=blk - 1: XT[:, dc, b_ * 512:(b_ + 1) * 512], [XTB[blk - 1]])
                                if l == 1:
                                    b_ = blk - 1
                                    S.dma("sp", out_v[:, :, b_ * 512:(b_ + 1) * 512], XT[:, :, b_ * 512:(b_ + 1) * 512],
                                          reads=[XTB[b_]], writes=[OUTB])
                            if f == 9 and blk < 3:
                                pre_norm_sq(blk + 1, sq, SQB)
                            if f == 12 and blk < 3:
                                hn, HN_ = h2T[(blk + 1) % 2], H2B[(blk + 1) % 2]
                                pre_norm_rest(blk + 1, l, 2, 24, sq, SQB, sd, SDB, rstd, RSB, tmp, TMPB,
                                              lambda k, hn=hn: hn[:, k, :], lambda k, HN_=HN_: [HN_])
                            wb, WB_ = wup[wctr % 4], WUPB[wctr % 4]
                            wctr += 1
                            S.dma("pool", wb[:], upp_d[l, :, f, :, :], writes=[WB_])
                            if blk == 0 and f in (12, 17):
                                hf = 0 if f == 12 else 1
                                S.dma("pool", wdn[:, hf * 11:(hf + 1) * 11, :], dnp_d[l, :, hf * 11:(hf + 1) * 11, :],
                                      writes=[WDNB[hf]])
                            if True:
                                ptu, PUB = ps_next()
                                ptg, PGB = ps_next()

                                def mmu(e, ptu=ptu, wb=wb, hb=hb):
                                    last = None
                                    for k in range(8):
                                        last = e.matmul(ptu[:, :], wb[:, k, 0:128], hb[:, k, :], start=(k == 0), stop=(k == 7))
                                    return last

                                def mmg(e, ptg=ptg, wb=wb, hb=hb):
                                    last = None
                                    for k in range(8):
                                        last = e.matmul(ptg[:, :], wb[:, k, 128:256], hb[:, k, :], start=(k == 0), stop=(k == 7))
                                    return last
                                S.op("pe", mmg, reads=[WB_, HB_], writes=[PGB])
                                S.op("pe", mmu, reads=[WB_, HB_], writes=[PUB])
                                g_, GB_ = gc[f % 2], GCB[f % 2]
                                s_, SB_ = sg[f % 2], SGB[f % 2]
                                w0 = vcol("convw", (l * 3 + 0) * 22 + f)
                                w1 = vcol("convw", (l * 3 + 1) * 22 + f)
                                w2 = vcol("convw", (l * 3 + 2) * 22 + f)
                                cb_ = vcol("convb", l * 22 + f)
                                S.op("act", lambda e, ptg=ptg, g_=g_, w1=w1, cb_=cb_: e.activation(
                                    out=g_[:, :], in_=ptg[:, :], func=AF.Identity, bias=cb_, scale=w1),
                                    reads=[PGB, VECS], writes=[GB_])
                                g3 = g_[:, :].rearrange("p (r c) -> p r c", c=64)
                                p3 = ptg[:, :].rearrange("p (r c) -> p r c", c=64)
                                S.op("dve", lambda e, g3=g3, p3=p3, w0=w0: e.scalar_tensor_tensor(
                                    out=g3[:, :, 1:64], in0=p3[:, :, 0:63], scalar=w0, in1=g3[:, :, 1:64],
                                    op0=ALU.mult, op1=ALU.add), reads=[PGB, GB_, VECS], writes=[GB_])
                                S.op("dve", lambda e, g3=g3, p3=p3, w2=w2: e.scalar_tensor_tensor(
                                    out=g3[:, :, 0:63], in0=p3[:, :, 1:64], scalar=w2, in1=g3[:, :, 0:63],
                                    op0=ALU.mult, op1=ALU.add), reads=[PGB, GB_, VECS], writes=[GB_])
                                S.op("act", lambda e, g_=g_, s_=s_: e.activation(out=s_[:, :], in_=g_[:, :], func=AF.Silu),
                                     reads=[GB_], writes=[SB_])
                                S.op("dve", lambda e, s_=s_, ptu=ptu, f=f: e.tensor_tensor(
                                    out=aT[:, f, :], in0=s_[:, :], in1=ptu[:, :], op=ALU.mult),
                                    reads=[SB_, PUB], writes=[ATBS[f]])
                        pnr.part1(blk, wdn, WDNB, lambda k: aT[:, k, :], lambda k0, k1: ATBS[k0:k1], NF, split_tail=3)
                    pnr.part2(3, 3, l, lambda dc: XT[:, dc, 3 * 512:4 * 512], [XTB[3]])
                    S.barrier()

            ffn(0)
            dump("x2", XT[:], [128, 8, T], F32, XTB)
            stop("stop4")

            with ExitStack() as ph:
                hT = sb("hT", [128, 8, T], BF16, ph)
                HTB = [Buf("hT%d" % g) for g in range(8)]
                wfo = sb("wfo", [128, 8, D], BF16, ph); WFOB = Buf("wfo")
                S.dma("pool", wfo[:], wfo_d[:, :, :], writes=[WFOB])
                with ExitStack() as p5:
                    dft = sb("dft", [128, 2, 2, 8, 1024], BF16, p5)
                    DFTB = [[Buf("dft%d%d" % (a_, b_)) for b_ in range(2)] for a_ in range(2)]
                    for a_ in range(2):
                        for b_ in range(2):
                            S.dma("sp", dft[:, a_, b_, :, :], dft2_d[:, a_, b_, :, :], writes=[DFTB[a_][b_]])
                    PQ = [sb("PQ%d" % i, [128, 2, 8, 256], BF16, p5) for i in range(2)]
                    PQB = [Buf("PQ%d" % i) for i in range(2)]
                    Esb = [sb("Esb%d" % i, [128, 512], F32, p5) for i in range(2)]
                    ESB = [Buf("Esb%d" % i) for i in range(2)]
                    sd = sb("sd5", [128, 512], F32, p5); SDB = Buf("sd5")
                    rstd4 = PQ[0][:].rearrange("p a i c -> p (a i c)").bitcast(F32).rearrange("p (b t) -> p b t", t=512)
                    RSB4 = [PQB[0]] * 4
                    for blk in range(4):
                        k0 = 4 + 2 * (blk % 2)
                        sq = hT[:, k0:k0 + 2, :].rearrange("p a t -> p (a t)").rearrange("p (k t) -> p k t", t=512)
                        SQW = [HTB[k0], HTB[k0 + 1]]
                        S.op("act", lambda e, blk=blk, sq=sq: e.activation(out=sq, in_=XT[:, :, blk * 512:(blk + 1) * 512],
                                                                          func=AF.Square), reads=[XTB[blk]], writes=SQW)
                        pt, PB = ps_next()

                        def mmss(e, pt=pt, sq=sq):
                            last = None
                            for k in range(8):
                                last = e.matmul(pt[:, :], ones, sq[:, k, :], start=(k == 0), stop=(k == 7))
                            return last
                        S.op("pe", mmss, reads=SQW + [CBFB], writes=[PB])
                        S.op("act", lambda e, pt=pt: e.activation(out=sd[:, :], in_=pt[:, :], func=AF.Ln, bias=EPS,
                                                                  scale=1.0 / D), reads=[PB], writes=[SDB])
                        S.op("act", lambda e, blk=blk: e.activation(out=rstd4[:, blk, :], in_=sd[:, :], func=AF.Exp,
                                                                   scale=-0.5), reads=[SDB], writes=[RSB4[blk]])
                    tctr5 = [0]
                    pn_thunks = []

                    def pn_pair(k, blk):
                        tm, TB = Esb[tctr5[0] % 2], ESB[tctr5[0] % 2]
                        tctr5[0] += 1
                        S.op("dve", lambda e: e.scalar_tensor_tensor(
                            out=tm[:, :], in0=XT[:, k, blk * 512:(blk + 1) * 512], scalar=der[:, 1, 0, k:k + 1],
                            in1=rstd4[:, blk, :], op0=ALU.mult, op1=ALU.mult),
                            reads=[XTB[blk], RSB4[blk], DERB], writes=[TB])
                        if tctr5[0] % 2 == 0:
                            S.op("act", lambda e: e.activation(
                                out=hT[:, k, blk * 512:(blk + 1) * 512], in_=tm[:, :], func=AF.Identity,
                                bias=mod[:, 1, k:k + 1], scale=1.0), reads=[TB, MODB], writes=[HTB[k]])
                        else:
                            S.op("dve", lambda e: e.tensor_scalar(
                                out=hT[:, k, blk * 512:(blk + 1) * 512], in0=tm[:, :], scalar1=mod[:, 1, k:k + 1],
                                scalar2=None, op0=ALU.add), reads=[TB, MODB], writes=[HTB[k]])
                    for blk in range(4):
                        pn_pair(0, blk)
                    for k in range(1, 8):
                        for blk in range(4):
                            pn_thunks.append(lambda k=k, blk=blk: pn_pair(k, blk))
                    ectr = 0
                    for g in range(8):
                        pq, PQB_ = (PQ[g % 2], PQB[g % 2]) if g >= 4 else (PQ[1], PQB[1])
                        for par in range(2):
                            for ip in range(4):
                                pt, PB = ps_next()

                                def mm0(e, pt=pt, g=g, par=par, ip=ip):
                                    last = None
                                    for q in range(2):
                                        i = 2 * ip + q
                                        t0 = 2 * i * 128 + par
                                        last = e.matmul(pt[:, q * 256:(q + 1) * 256], V(hT[:, g, t0:t0 + 1], [[2, 128]]),
                                                        dftd, start=True, stop=True)
                                    return last
                                S.op("pe", mm0, reads=[HTB[g], CBFB], writes=[PB])
                                if pn_thunks:
                                    pn_thunks.pop(0)()
                                dst = pq[:, par, 2 * ip:2 * ip + 2, :].rearrange("p a b -> p (a b)")
                                if ip % 2 == 0:
                                    S.op("act", lambda e, pt=pt, dst=dst: e.copy(dst, pt[:, :]), reads=[PB], writes=[PQB_])
                                else:
                                    S.op("dve", lambda e, pt=pt, dst=dst: e.tensor_copy(dst, pt[:, :]), reads=[PB], writes=[PQB_])
                        for kb in range(2):
                            pte, PEB = ps_next()
                            pto, POB = ps_next()
                            for par, ptx, PXB in ((0, pte, PEB), (1, pto, POB)):
                                def mm1(e, ptx=ptx, par=par, kb=kb, pq=pq):
                                    last = None
                                    for i in range(8):
                                        e.matmul(ptx[:, :], pq[:, par, i, 0:128], dft[:, 0, par, i, kb * 512:(kb + 1) * 512],
                                                 start=(i == 0), stop=False)
                                        last = e.matmul(ptx[:, :], pq[:, par, i, 128:256],
                                                        dft[:, 1, par, i, kb * 512:(kb + 1) * 512], start=False, stop=(i == 7))
                                    return last
                                S.op("pe", mm1, reads=[PQB_, DFTB[0][par], DFTB[1][par]], writes=[PXB])
                            es, ESB_ = Esb[ectr % 2], ESB[ectr % 2]
                            ectr += 1
                            S.op("act", lambda e, es=es, pte=pte: e.copy(es[:, :], pte[:, :]), reads=[PEB], writes=[ESB_])
                            S.op("dve", lambda e, es=es, pto=pto, g=g, kb=kb: e.tensor_tensor(
                                out=hT[:, g, kb * 512:(kb + 1) * 512], in0=es[:, :], in1=pto[:, :], op=ALU.add),
                                reads=[ESB_, POB], writes=[HTB[g]])
                            S.op("dve", lambda e, es=es, pto=pto, g=g, kb=kb: e.tensor_tensor(
                                out=hT[:, g, 1024 + kb * 512:1024 + (kb + 1) * 512], in0=es[:, :], in1=pto[:, :],
                                op=ALU.subtract), reads=[ESB_, POB], writes=[HTB[g]])
                    S.barrier()
                dump("yfftT", hT[:], [128, 8, T], BF16, HTB)
                with ExitStack() as p6:
                    pnr = mk_pnr(p6, "6", nbuf=2)
                    for blk in range(4):
                        pnr.part1(blk, wfo, [WFOB], lambda k, blk=blk: hT[:, k, blk * 512:(blk + 1) * 512],
                                  lambda k0, k1: HTB, 8, bias_name="fb")
                        if blk > 0:
                            pb = blk - 1
                            pnr.part2(pb, 1, 1, lambda dc, pb=pb: XT[:, dc, pb * 512:(pb + 1) * 512], [XTB[pb]])
                    pnr.part2(3, 1, 1, lambda dc: XT[:, dc, 3 * 512:4 * 512], [XTB[3]])
                    S.barrier()
            dump("x3", XT[:], [128, 8, T], F32, XTB)
            stop("stop6")

            ffn(1)
            for blk in range(3, 4):
                S.dma("sp", out_v[:, :, blk * 512:(blk + 1) * 512], XT[:, :, blk * 512:(blk + 1) * 512],
                      reads=[XTB[blk]], writes=[OUTB])
            dbg_out["__out"] = OUTB
        except _Stop:
            pass
        S.stopped = False
        S.finish(list(dbg_out.values()))
    return nc, dbg_out


_CACHE = {}


def kernel(**inputs):
    sh = host_shared(inputs)
    in_maps = []
    for b in range(8):
        m = dict(sh)
        m.update(host_percore(inputs, b))
        in_maps.append(m)
    if "nc" not in _CACHE:
        _CACHE["nc"] = build()[0]
    res = run_bass_kernel_spmd(_CACHE["nc"], in_maps, core_ids=list(range(8)))
    out = np.stack([np.ascontiguousarray(res.results[b]["out"].T) for b in range(8)])
    return out.astype(np.float32)
```
